# Optimizing a Trainium2 kernel written in Bass

```python
import math
import jax, jax.numpy as jnp
from jax import lax
import numpy as np

D_MODEL = 1024
BATCH = 4
SEQ = 8192
DEPTH = 4

D_MIX = D_MODEL
N_MIXERS = 4
GROUP_WIDTH = D_MIX // N_MIXERS
HEAD_DIM = 64
N_HEADS = GROUP_WIDTH // HEAD_DIM
ATTN_BLOCK = 128
SG_CHUNK = 128
SSM_CHUNK = 128
SSM_GROUPS = 2
SSM_HEADS_PER_GROUP = N_HEADS // SSM_GROUPS
SSM_STATE = 128
CONV_WIDTH = 4
SSM_XBC = GROUP_WIDTH + 2 * SSM_GROUPS * SSM_STATE
MLSTM_CHUNK = 128
MLSTM_M_INIT = -1e30
N_EXPERT_GROUPS = 4
EXPERTS_PER_GROUP = 8
N_EXPERTS = N_EXPERT_GROUPS * EXPERTS_PER_GROUP
TOP_K = 2
D_EXPERT = 512
MOE_BLOCK = 256
NORM_EPS = 1e-6
ATTN_COLS = 3 * GROUP_WIDTH + N_HEADS
SG_COLS = 2 * GROUP_WIDTH
SSM_COLS = GROUP_WIDTH + SSM_XBC + N_HEADS
MLSTM_COLS = 4 * GROUP_WIDTH + 2 * N_HEADS
D_IN_PROJ = ATTN_COLS + SG_COLS + SSM_COLS + MLSTM_COLS

kernel_name = 'hybrid_fox_gmlp_ssd_mlstm_hmoe'


def rms_norm(x, gain):
    xf = x.astype(jnp.float32)
    y = xf * lax.rsqrt(jnp.mean(xf * xf, axis=-1, keepdims=True) + NORM_EPS)
    return (y * gain.astype(jnp.float32)).astype(x.dtype)


def split_columns(x, widths):
    cuts = [int(i) for i in np.cumsum(widths)[:-1]]
    return jnp.split(x, cuts, axis=-1)


def causal_depthwise_conv(x, w, b):
    width, ch = w.shape
    y = lax.conv_general_dilated(x, w[:, None, :].astype(x.dtype), window_strides=(1,),
                                 padding=((width - 1, 0),),
                                 dimension_numbers=('NWC', 'WIO', 'NWC'),
                                 feature_group_count=ch)
    return y + b.astype(y.dtype)


def forgetting_attention(q, k, v, f_logit):
    Bb, S, H, Dh = q.shape
    L = ATTN_BLOCK
    qf = q.astype(jnp.float32) * (Dh ** -0.5)
    kf = k.astype(jnp.float32)
    vf = v.astype(jnp.float32)
    cum_logf = jnp.cumsum(jax.nn.log_sigmoid(f_logit.astype(jnp.float32)), axis=1).transpose(0, 2, 1)
    diag = jnp.tril(jnp.ones((L, L), bool))
    outs = []
    for blk in range(S // L):
        q0, q1 = blk * L, (blk + 1) * L
        logits = jnp.einsum('bqhd,bkhd->bhqk', qf[:, q0:q1], kf[:, :q1])
        logits = logits + cum_logf[:, :, q0:q1, None] - cum_logf[:, :, None, :q1]
        mask = jnp.concatenate([jnp.ones((L, q0), bool), diag], axis=1)
        probs = jax.nn.softmax(jnp.where(mask, logits, -jnp.inf), axis=-1)
        outs.append(jnp.einsum('bhqk,bkhd->bqhd', probs, vf[:, :q1]))
    return jnp.concatenate(outs, axis=1)


def spatial_gating(u, v, v_gain, w_s, b_s):
    Bb, S, H, Dh = u.shape
    L = SG_CHUNK
    u = jax.nn.gelu(u.astype(jnp.float32))
    v = rms_norm(jax.nn.gelu(v.astype(jnp.float32)), v_gain)
    causal = jnp.tril(jnp.ones((L, L), bool))
    w = jnp.where(causal[None], w_s.astype(jnp.float32), 0.0)
    vc = v.reshape(Bb, S // L, L, H, Dh)
    mixed = jnp.einsum('hts,bcshd->bcthd', w, vc) + b_s.astype(jnp.float32).T[None, None, :, :, None]
    return u * mixed.reshape(Bb, S, H, Dh)


def ssd_chunked(x, a, b, c):
    Bb, S, G, R, P = x.shape
    L = SSM_CHUNK
    nc = S // L
    x = x.reshape(Bb, nc, L, G, R, P)
    a = a.reshape(Bb, nc, L, G, R)
    b = b.reshape(Bb, nc, L, G, -1)
    c = c.reshape(Bb, nc, L, G, -1)
    a_cum = jnp.cumsum(a, axis=2)
    causal = jnp.tril(jnp.ones((L, L), bool))[None, None, :, :, None, None]
    seg = a_cum[:, :, :, None] - a_cum[:, :, None, :]
    decay = jnp.exp(jnp.where(causal, seg, -jnp.inf))
    scores = jnp.einsum('bctgn,bcsgn->bctsg', c, b)
    y_diag = jnp.einsum('bctsgr,bcsgrp->bctgrp', scores[..., None] * decay, x)
    decay_to_end = jnp.exp(a_cum[:, :, -1:] - a_cum)
    states = jnp.einsum('bcsgn,bcsgrp->bcgrpn', b, x * decay_to_end[..., None])
    chunk_decay = jnp.exp(a_cum[:, :, -1])

    def carry_state(h, inp):
        st, dc = inp
        return dc[..., None, None] * h + st, h

    h0 = jnp.zeros((Bb,) + states.shape[2:], jnp.float32)
    _, h_in = lax.scan(carry_state, h0, (jnp.moveaxis(states, 1, 0), jnp.moveaxis(chunk_decay, 1, 0)))
    h_in = jnp.moveaxis(h_in, 0, 1)
    y_off = jnp.einsum('bctgn,bcgrpn->bctgrp', c, h_in) * jnp.exp(a_cum)[..., None]
    return (y_diag + y_off).reshape(Bb, S, G, R, P)


def mamba2_mixer(z, xbc, dt_raw, conv_w, conv_b, dt_bias, a_log, d_skip):
    Bb, S, _ = xbc.shape
    G, R = SSM_GROUPS, SSM_HEADS_PER_GROUP
    xbc = jax.nn.silu(causal_depthwise_conv(xbc, conv_w, conv_b)).astype(jnp.float32)
    xs, bm, cm = jnp.split(xbc, [GROUP_WIDTH, GROUP_WIDTH + SSM_GROUPS * SSM_STATE], axis=-1)
    xs = xs.reshape(Bb, S, G, R, HEAD_DIM)
    bm = bm.reshape(Bb, S, G, SSM_STATE)
    cm = cm.reshape(Bb, S, G, SSM_STATE)
    dt = jax.nn.softplus(dt_raw.astype(jnp.float32) + dt_bias.astype(jnp.float32)).reshape(Bb, S, G, R)
    a = -jnp.exp(a_log.astype(jnp.float32)).reshape(G, R)
    y = ssd_chunked(xs * dt[..., None], dt * a, bm, cm)
    y = y + d_skip.astype(jnp.float32).reshape(G, R)[..., None] * xs
    y = y.reshape(Bb, S, N_HEADS, HEAD_DIM)
    return y * jax.nn.silu(z.astype(jnp.float32)).reshape(Bb, S, N_HEADS, HEAD_DIM)


def mlstm_chunkwise(q, k, v, i_pre, f_pre):
    Bb, S, H, Dh = q.shape
    L = MLSTM_CHUNK
    nc = S // L
    q = q.astype(jnp.float32).reshape(Bb, nc, L, H, Dh)
    k = (k.astype(jnp.float32) * (Dh ** -0.5)).reshape(Bb, nc, L, H, Dh)
    v = v.astype(jnp.float32).reshape(Bb, nc, L, H, Dh)
    log_f = jax.nn.log_sigmoid(f_pre.astype(jnp.float32)).reshape(Bb, nc, L, H)
    ig = i_pre.astype(jnp.float32).reshape(Bb, nc, L, H)
    a = jnp.cumsum(log_f, axis=2)
    a_end = a[:, :, -1]
    g = a_end[:, :, None] - a + ig

    def carry_state(carry, inp):
        C, n, m = carry
        kc, vc, gc, ac = inp
        m_new = jnp.maximum(ac + m, gc.max(axis=1))
        w_s = jnp.exp(gc - m_new[:, None, :])
        scale = jnp.exp(ac + m - m_new)
        C_new = scale[..., None, None] * C + jnp.einsum('blhd,blhe->bhde', vc * w_s[..., None], kc)
        n_new = scale[..., None] * n + jnp.einsum('blh,blhe->bhe', w_s, kc)
        return (C_new, n_new, m_new), (C, n, m)

    init = (jnp.zeros((Bb, H, Dh, Dh), jnp.float32), jnp.zeros((Bb, H, Dh), jnp.float32),
            jnp.full((Bb, H), MLSTM_M_INIT, jnp.float32))
    _, (C_in, n_in, m_in) = lax.scan(carry_state, init, (jnp.moveaxis(k, 1, 0), jnp.moveaxis(v, 1, 0),
                                                         jnp.moveaxis(g, 1, 0), jnp.moveaxis(a_end, 1, 0)))
    C_in = jnp.moveaxis(C_in, 0, 1)
    n_in = jnp.moveaxis(n_in, 0, 1)
    m_in = jnp.moveaxis(m_in, 0, 1)
    causal = jnp.tril(jnp.ones((L, L), bool))[None, None, :, :, None]
    log_d = a[:, :, :, None, :] - a[:, :, None, :, :] + ig[:, :, None, :, :]
    log_d = jnp.where(causal, log_d, -jnp.inf)
    log_inter = a + m_in[:, :, None, :]
    m_t = jnp.maximum(log_d.max(axis=3), log_inter)
    w = jnp.einsum('bcthd,bcshd->bctsh', q, k) * jnp.exp(log_d - m_t[:, :, :, None, :])
    inter = jnp.exp(log_inter - m_t)
    num = jnp.einsum('bctsh,bcshd->bcthd', w, v) + inter[..., None] * jnp.einsum('bchde,bcthe->bcthd', C_in, q)
    den = w.sum(axis=3) + inter * jnp.einsum('bche,bcthe->bcth', n_in, q)
    h = num / jnp.maximum(jnp.abs(den), jnp.exp(-m_t))[..., None]
    return h.reshape(Bb, S, H, Dh)


def hybrid_mixer(h, w_in, w_out, mix_gain, attn_f_bias, sg_w, sg_b, conv_w, conv_b,
                 dt_bias, a_log, d_skip, i_bias, f_bias):
    Bb, S, _ = h.shape
    heads = lambda t: t.reshape(Bb, S, N_HEADS, HEAD_DIM)
    gw = GROUP_WIDTH
    proj = h @ w_in
    (aq, ak, av, af, su, sv, mz, mxbc, mdt, lq, lk, lv, lo, li, lf) = split_columns(
        proj, [gw, gw, gw, N_HEADS, gw, gw, gw, SSM_XBC, N_HEADS, gw, gw, gw, gw, N_HEADS, N_HEADS])
    g_attn, g_sg, g_ssm, g_ml = [t.reshape(N_HEADS, HEAD_DIM) for t in jnp.split(mix_gain, N_MIXERS)]
    y_attn = rms_norm(forgetting_attention(heads(aq), heads(ak), heads(av),
                                           af.astype(jnp.float32) + attn_f_bias.astype(jnp.float32)), g_attn)
    y_sg = spatial_gating(heads(su), heads(sv), g_sg, sg_w, sg_b)
    y_ssm = rms_norm(mamba2_mixer(mz, mxbc, mdt, conv_w, conv_b, dt_bias, a_log, d_skip), g_ssm)
    y_ml = jax.nn.sigmoid(heads(lo).astype(jnp.float32)) * rms_norm(
        mlstm_chunkwise(heads(lq), heads(lk), heads(lv),
                        li.astype(jnp.float32) + i_bias.astype(jnp.float32),
                        lf.astype(jnp.float32) + f_bias.astype(jnp.float32)), g_ml)
    y = jnp.concatenate([y_attn, y_sg, y_ssm, y_ml], axis=2).reshape(Bb, S, D_MIX).astype(h.dtype)
    return y @ w_out


def hierarchical_moe(h, w_rg, b_rg, w_re, b_re, w_gate, w_up, w_down):
    Bb, S, D = h.shape
    tokens = h.reshape(-1, D)
    T = tokens.shape[0]
    rows = jnp.arange(T)
    g_logits = (tokens @ w_rg).astype(jnp.float32) + b_rg.astype(jnp.float32)
    g_sel = jnp.argmax(g_logits, axis=-1)
    g_prob = jax.nn.softmax(g_logits, axis=-1)[rows, g_sel]
    e_logits = ((tokens @ w_re).astype(jnp.float32) + b_re.astype(jnp.float32)).reshape(
        T, N_EXPERT_GROUPS, EXPERTS_PER_GROUP)[rows, g_sel]
    top_logit, top_local = lax.top_k(e_logits, TOP_K)
    gate = g_prob[:, None] * jax.nn.softmax(top_logit, axis=-1)
    expert = g_sel[:, None] * EXPERTS_PER_GROUP + top_local
    M = T * TOP_K
    e_flat = expert.reshape(-1).astype(jnp.int32)
    tok_flat = (jnp.arange(M) // TOP_K).astype(jnp.int32)
    w_flat = gate.reshape(-1)
    order = jnp.argsort(e_flat)
    e_sorted = e_flat[order]
    counts = jnp.bincount(e_flat, length=N_EXPERTS)
    starts = jnp.cumsum(counts) - counts
    padded = ((counts + MOE_BLOCK - 1) // MOE_BLOCK) * MOE_BLOCK
    p_ends = jnp.cumsum(padded)
    p_starts = p_ends - padded
    dest = p_starts[e_sorted] + (jnp.arange(M) - starts[e_sorted])
    n_blocks = -(-M // MOE_BLOCK) + N_EXPERTS
    P = n_blocks * MOE_BLOCK
    row_tok = jnp.full((P,), T, jnp.int32).at[dest].set(tok_flat[order])
    row_w = jnp.zeros((P,), jnp.float32).at[dest].set(w_flat[order])
    block_e = jnp.minimum(jnp.searchsorted(p_ends, jnp.arange(n_blocks) * MOE_BLOCK, side='right'),
                          N_EXPERTS - 1)
    tokens_pad = jnp.concatenate([tokens, jnp.zeros((1, D), tokens.dtype)], axis=0)
    xb = tokens_pad[row_tok].reshape(n_blocks, MOE_BLOCK, D)

    def expert_block(args):
        xe, e = args
        return (jax.nn.silu(xe @ w_gate[e]) * (xe @ w_up[e])) @ w_down[e]

    yb = lax.map(expert_block, (xb, block_e)).reshape(P, D)
    y = jnp.zeros((T + 1, D), jnp.float32).at[row_tok].add(yb.astype(jnp.float32) * row_w[:, None])
    return y[:T].reshape(Bb, S, D).astype(h.dtype)


def setup_inputs(seed: int = 0) -> dict:
    key = jax.random.key(seed)
    ks = jax.random.split(key, 32)
    nrm = lambda k, shape, s: s * jax.random.normal(k, shape, jnp.float32)
    x = nrm(ks[0], (BATCH, SEQ, D_MODEL), 1.0)
    c = nrm(ks[1], (BATCH, D_MODEL), 1.0)
    w_in = nrm(ks[2], (DEPTH, D_MODEL, D_IN_PROJ), D_MODEL ** -0.5)
    w_out = nrm(ks[3], (DEPTH, D_MIX, D_MODEL), D_MIX ** -0.5)
    w_mix_norm = 1.0 + nrm(ks[4], (DEPTH, D_MIX), 0.1)
    attn_f_bias = jnp.linspace(2.0, 6.0, N_HEADS)[None, :] + nrm(ks[5], (DEPTH, N_HEADS), 0.1)
    sg_w = nrm(ks[6], (DEPTH, N_HEADS, SG_CHUNK, SG_CHUNK), 0.5 * SG_CHUNK ** -0.5)
    sg_b = 1.0 + nrm(ks[7], (DEPTH, N_HEADS, SG_CHUNK), 0.1)
    ssm_conv_w = nrm(ks[8], (DEPTH, CONV_WIDTH, SSM_XBC), CONV_WIDTH ** -0.5)
    ssm_conv_b = nrm(ks[9], (DEPTH, SSM_XBC), 0.02)
    dt0 = jnp.exp(jax.random.uniform(ks[10], (DEPTH, N_HEADS), jnp.float32,
                                     minval=math.log(1e-3), maxval=math.log(1e-1)))
    ssm_dt_bias = dt0 + jnp.log(-jnp.expm1(-dt0))
    ssm_a_log = jnp.log(jax.random.uniform(ks[11], (DEPTH, N_HEADS), jnp.float32, minval=1.0, maxval=16.0))
    ssm_d = 1.0 + nrm(ks[12], (DEPTH, N_HEADS), 0.1)
    mlstm_i_bias = nrm(ks[13], (DEPTH, N_HEADS), 0.1)
    mlstm_f_bias = jnp.linspace(3.0, 6.0, N_HEADS)[None, :] + nrm(ks[14], (DEPTH, N_HEADS), 0.1)
    w_ada = nrm(ks[15], (DEPTH, D_MODEL, 6 * D_MODEL), 0.5 * D_MODEL ** -0.5)
    b_ada = nrm(ks[16], (DEPTH, 6 * D_MODEL), 0.02)
    w_norm1 = 1.0 + nrm(ks[17], (DEPTH, D_MODEL), 0.1)
    w_norm2 = 1.0 + nrm(ks[18], (DEPTH, D_MODEL), 0.1)
    w_router_group = nrm(ks[19], (DEPTH, D_MODEL, N_EXPERT_GROUPS), D_MODEL ** -0.5)
    b_router_group = nrm(ks[20], (DEPTH, N_EXPERT_GROUPS), 0.01)
    w_router_expert = nrm(ks[21], (DEPTH, D_MODEL, N_EXPERTS), D_MODEL ** -0.5)
    b_router_expert = nrm(ks[22], (DEPTH, N_EXPERTS), 0.01)
    w_expert_gate = nrm(ks[23], (DEPTH, N_EXPERTS, D_MODEL, D_EXPERT), D_MODEL ** -0.5)
    w_expert_up = nrm(ks[24], (DEPTH, N_EXPERTS, D_MODEL, D_EXPERT), D_MODEL ** -0.5)
    w_expert_down = nrm(ks[25], (DEPTH, N_EXPERTS, D_EXPERT, D_MODEL), D_EXPERT ** -0.5)
    w_norm_final = 1.0 + nrm(ks[26], (D_MODEL,), 0.1)
    return {'x': x, 'c': c, 'w_in': w_in, 'w_out': w_out, 'w_mix_norm': w_mix_norm,
            'attn_f_bias': attn_f_bias, 'sg_w': sg_w, 'sg_b': sg_b,
            'ssm_conv_w': ssm_conv_w, 'ssm_conv_b': ssm_conv_b, 'ssm_dt_bias': ssm_dt_bias,
            'ssm_a_log': ssm_a_log, 'ssm_d': ssm_d, 'mlstm_i_bias': mlstm_i_bias,
            'mlstm_f_bias': mlstm_f_bias, 'w_ada': w_ada, 'b_ada': b_ada,
            'w_norm1': w_norm1, 'w_norm2': w_norm2,
            'w_router_group': w_router_group, 'b_router_group': b_router_group,
            'w_router_expert': w_router_expert, 'b_router_expert': b_router_expert,
            'w_expert_gate': w_expert_gate, 'w_expert_up': w_expert_up,
            'w_expert_down': w_expert_down, 'w_norm_final': w_norm_final}


def reference(x, c, w_in, w_out, w_mix_norm, attn_f_bias, sg_w, sg_b, ssm_conv_w, ssm_conv_b,
              ssm_dt_bias, ssm_a_log, ssm_d, mlstm_i_bias, mlstm_f_bias, w_ada, b_ada,
              w_norm1, w_norm2, w_router_group, b_router_group, w_router_expert, b_router_expert,
              w_expert_gate, w_expert_up, w_expert_down, w_norm_final):
    cond = jax.nn.silu(c)
    for layer in range(DEPTH):
        mod = cond @ w_ada[layer] + b_ada[layer]
        shift1, scale1, gate1, shift2, scale2, gate2 = jnp.split(mod[:, None, :], 6, axis=-1)
        h = rms_norm(x, w_norm1[layer]) * (1.0 + scale1) + shift1
        x = x + gate1 * hybrid_mixer(h, w_in[layer], w_out[layer], w_mix_norm[layer], attn_f_bias[layer],
                                     sg_w[layer], sg_b[layer], ssm_conv_w[layer], ssm_conv_b[layer],
                                     ssm_dt_bias[layer], ssm_a_log[layer], ssm_d[layer],
                                     mlstm_i_bias[layer], mlstm_f_bias[layer])
        h = rms_norm(x, w_norm2[layer]) * (1.0 + scale2) + shift2
        x = x + gate2 * hierarchical_moe(h, w_router_group[layer], b_router_group[layer],
                                         w_router_expert[layer], b_router_expert[layer],
                                         w_expert_gate[layer], w_expert_up[layer], w_expert_down[layer])
    return rms_norm(x, w_norm_final)
```

```python
import numpy as np
from contextlib import ExitStack
import concourse.bass as bass
import concourse.mybir as mybir
from concourse.bass_utils import run_bass_kernel_spmd

F32 = mybir.dt.float32
BF16 = mybir.dt.bfloat16
I32 = mybir.dt.int32
ALU = mybir.AluOpType
AF = mybir.ActivationFunctionType
AX = mybir.AxisListType

D = 1024
DEPTH = 4
NCORES = 8
EPS = 1e-6
NEG = -1.0e30
SAME_ENGINE_SYNC = True
PIPE_G = 2


class Res:
    __slots__ = ("name", "w", "r", "excl")

    def __init__(self, name=""):
        self.name = name
        self.w = None
        self.r = []
        self.excl = False


class Prog:
    ENG = ("pe", "act", "dve", "pool", "sp")

    def __init__(self, nc, es, same_engine_sync=True):
        self.nc = nc
        self.es = es
        self.same = same_engine_sync
        self.ops = {e: [] for e in self.ENG}
        self.sem = {e: es.enter_context(nc.semaphore("s_" + e)) for e in self.ENG}
        self.cnt = {e: 0 for e in self.ENG}
        self.dsem = {}
        self.waited = {e: {} for e in self.ENG}
        self.nres = 0

    def res(self, name=""):
        self.nres += 1
        return Res(name or f"r{self.nres}")

    def _deps(self, eng, reads, writes):
        deps = []
        for r in reads:
            if r.w is not None:
                deps.append(r.w)
            if r.excl:
                deps.extend(t for t in r.r if not (t[0] == "e" and t[1] == eng))
        for w in writes:
            if w.w is not None:
                deps.append(w.w)
            deps.extend(w.r)
        waits = {}
        for (kind, key, val) in deps:
            if kind == "e":
                if key == eng and (not self.same or eng == "pe"):
                    continue
                sem = self.sem[key]
            else:
                sem = self.dsem[key][0]
            k = id(sem)
            if self.waited[eng].get(k, 0) >= val:
                continue
            if k not in waits or waits[k][1] < val:
                waits[k] = (sem, val)
        for k, (sem, val) in waits.items():
            self.waited[eng][k] = val
        return list(waits.values())

    def _mark(self, token, reads, writes):
        for r in reads:
            r.r.append(token)
        for w in writes:
            w.w = token
            w.r = []

    def op(self, eng, fn, reads=(), writes=()):
        waits = self._deps(eng, reads, writes)
        self.cnt[eng] += 1
        token = ("e", eng, self.cnt[eng])
        self._mark(token, reads, writes)
        self._emit_now(eng, waits, fn, (self.sem[eng], 1))

    def dma(self, eng, fn, key, reads=(), writes=()):
        if key not in self.dsem:
            self.dsem[key] = [self.es.enter_context(self.nc.semaphore("d_" + str(key))), 0]
        waits = self._deps(eng, reads, writes)
        self.dsem[key][1] += 16
        token = ("d", key, self.dsem[key][1])
        self._mark(token, reads, writes)
        self._emit_now(eng, waits, fn, (self.dsem[key][0], 16))

    def final_wait(self, eng, resources):
        waits = self._deps(eng, resources, resources)
        self._emit_now(eng, waits, None, None)

    ENGMAP = {"pe": "tensor", "act": "scalar", "dve": "vector", "pool": "gpsimd", "sp": "sync"}

    def _emit_now(self, e, waits, fn, inc):
        eng = getattr(self.nc, self.ENGMAP[e])
        for sem, val in waits:
            eng.wait_ge(sem, val)
        if fn is not None:
            ins = fn(eng)
            ins.then_inc(inc[0], inc[1])
        self.ninstr = getattr(self, "ninstr", 0) + 1 + len(waits)


C_AQ, C_AK, C_AV, C_AF = 0, 256, 512, 768
C_SU, C_SV = 772, 1028
C_SZ, C_XBC, C_DT = 1284, 1540, 2308
C_MQ, C_MK, C_MV, C_MO, C_MI, C_MF = 2312, 2568, 2824, 3080, 3336, 3340
TM_SEGS = [("su", C_SU), ("sv", C_SV), ("av", C_AV), ("mv", C_MV), ("mk", C_MK), ("sz", C_SZ), ("mo", C_MO)]
CM_SEGS = [("aq", C_AQ, 256), ("ak", C_AK, 256), ("xbc", C_XBC, 768), ("mq", C_MQ, 256), ("mkt", C_MK, 256)]
N_TM = 256 * len(TM_SEGS)
N_CM = 256 * 2 + 768 + 256 * 2
WCOLS = N_TM + N_CM + 16


class Builder:
    def __init__(self, S, nlayers, debug=False, phases=None):
        self.S = S
        self.L = nlayers
        self.debug = debug
        self.phases = phases
        self.NT = S // 128
        self.TBK = min(S, 2048)
        self.nc = bass.Bass("TRN2", target_bir_lowering=False)
        self.es = ExitStack()
        self.P = Prog(self.nc, self.es, same_engine_sync=SAME_ENGINE_SYNC)
        self.dbg_names = []

    def sb(self, name, shape, dt):
        return self.es.enter_context(self.nc.sbuf_tensor(name, shape, dt))

    def ps(self, name, shape, dt):
        return self.es.enter_context(self.nc.psum_tensor(name, shape, dt))

    def din(self, name, shape, dt=F32):
        return self.nc.dram_tensor(name, list(shape), dt, kind="ExternalInput").ap()

    def scr(self, name, shape, dt):
        if self.debug:
            self.dbg_names.append(name)
            return self.nc.dram_tensor(name, list(shape), dt, kind="ExternalOutput").ap()
        return self.nc.dram_tensor(name, list(shape), dt, kind="Internal").ap()

    def R(self, name=""):
        return self.P.res(name)

    def RL(self, name, n):
        return [self.P.res(f"{name}{i}") for i in range(n)]

    def RP(self, name=""):
        r = self.P.res(name)
        r.excl = True
        return r

    def RLP(self, name, n):
        return [self.RP(f"{name}{i}") for i in range(n)]

    def op(self, eng, fn, reads=(), writes=()):
        self.P.op(eng, fn, reads, writes)

    def dma(self, eng, fn, key, reads=(), writes=()):
        self.P.dma(eng, fn, key, reads, writes)

    def barrier(self):
        P = self.P
        allw = [(P.sem[e], P.cnt[e]) for e in P.ENG if P.cnt[e] > 0]
        allw += [(s, c) for (s, c) in P.dsem.values() if c > 0]
        for e in P.ENG:
            waits = []
            for sem, val in allw:
                if P.waited[e].get(id(sem), 0) < val:
                    waits.append((sem, val))
                    P.waited[e][id(sem)] = val
            P._emit_now(e, waits, None, None)

    class Phase:
        def __init__(self, b):
            self.b = b

        def __enter__(self):
            self.saved = self.b.es
            self.b.es_phase = ExitStack()
            self.b._alloc_es = self.b.es_phase
            return self

        def __exit__(self, *a):
            self.b.barrier()
            self.b.es_phase.close()
            self.b._alloc_es = self.b.es
            return False

    def phase(self):
        return Builder.Phase(self)

    def run_window(self, make_gen, n, G=2, skew=0):
        active = []
        nxt = 0
        first = True
        while nxt < n or active:
            while len(active) < G and nxt < n:
                g = make_gen(nxt)
                nxt += 1
                if first and skew > 0 and G > 1:
                    first = False
                    alive = True
                    for _ in range(skew):
                        try:
                            next(g)
                        except StopIteration:
                            alive = False
                            break
                    if alive:
                        active.append(g)
                else:
                    first = False
                    active.append(g)
            for g in list(active):
                try:
                    next(g)
                except StopIteration:
                    active.remove(g)

    def tsb(self, name, shape, dt):
        self._uid = getattr(self, "_uid", 0) + 1
        return self._alloc_es.enter_context(self.nc.sbuf_tensor(f"{name}_{self._uid}", shape, dt))

    def tps(self, name, shape, dt):
        self._uid = getattr(self, "_uid", 0) + 1
        esz = 2 if dt == BF16 else 4
        nfree = int(np.prod(shape[1:]))
        assert nfree * esz <= 2048, (name, shape)
        t = self._alloc_es.enter_context(self.nc.psum_tensor(f"{name}_{self._uid}", [128, 2048 // esz], dt))
        ap = t[0:shape[0], 0:nfree]
        if len(shape) == 3:
            ap = ap.rearrange("p (a b) -> p a b", b=shape[2])
        return ap

    def declare(self):
        S, L = self.S, self.L
        di = self.din
        self.x_in = di("x", [S, D])
        self.c_in = di("c", [1, D])
        self.w_in = di("w_in", [L, D, 3344])
        self.w_out = di("w_out", [L, D, D])
        self.w_mix_norm = di("w_mix_norm", [L, D])
        self.attn_f_bias = di("attn_f_bias", [L, 4])
        self.sg_w = di("sg_w", [L, 4, 128, 128])
        self.sg_b = di("sg_b", [L, 4, 128])
        self.ssm_conv_w = di("ssm_conv_w", [L, 4, 768])
        self.ssm_conv_b = di("ssm_conv_b", [L, 768])
        self.ssm_dt_bias = di("ssm_dt_bias", [L, 4])
        self.ssm_a_log = di("ssm_a_log", [L, 4])
        self.ssm_d = di("ssm_d", [L, 4])
        self.mlstm_i_bias = di("mlstm_i_bias", [L, 4])
        self.mlstm_f_bias = di("mlstm_f_bias", [L, 4])
        self.w_ada = di("w_ada", [L, D, 6 * D])
        self.b_ada = di("b_ada", [L, 6 * D])
        self.w_norm1 = di("w_norm1", [L, D])
        self.w_norm2 = di("w_norm2", [L, D])
        self.w_rg = di("w_router_group", [L, D, 4])
        self.b_rg = di("b_router_group", [L, 4])
        self.w_re = di("w_router_expert", [L, D, 32])
        self.b_re = di("b_router_expert", [L, 32])
        self.w_eg = di("w_expert_gate", [L * 32 * 128, 4096])
        self.w_eu = di("w_expert_up", [L * 32 * 128, 4096])
        self.w_ed = di("w_expert_down", [L * 32 * 128, 4096])
        self.w_norm_final = di("w_norm_final", [1, D])
        self.out = self.nc.dram_tensor("out", [S, D], F32, kind="ExternalOutput").ap()
        sc = self.scr
        self.X = sc("X", [S, D], F32)
        self.QT = sc("QT", [256, S], BF16)
        self.KT = sc("KT", [256, S], BF16)
        self.AV = sc("AV", [S, 256], BF16)
        self.GR = sc("GR", [16, S], F32)
        self.SU = sc("SU", [S, 256], BF16)
        self.SV = sc("SV", [S, 256], BF16)
        self.SZ = sc("SZ", [S, 256], BF16)
        self.XBC = sc("XBC", [768, S], BF16)
        self.XC = sc("XC", [768, S], BF16)
        self.MQT = sc("MQT", [256, S], BF16)
        self.MKT = sc("MKT", [256, S], BF16)
        self.MK = sc("MK", [S, 256], BF16)
        self.MV = sc("MV", [S, 256], BF16)
        self.MO = sc("MO", [S, 256], BF16)
        self.Y = sc("Y", [S, D], BF16)
        self.FQd = sc("FQd", [4, 3, S], BF16)
        self.FKd = sc("FKd", [4, 3, S], BF16)
        self.SROW = sc("SROW", [20, S], F32)
        self.MROW = sc("MROW", [24, S], F32)
        self.MSCL = sc("MSCL", [4, S // 128], F32)
        self.MB = 512 if S >= 2048 else 128
        self.LB = {512: 9, 128: 7}[self.MB]
        self.NB = (2 * S) // self.MB + 32
        self.HS = sc("HS", [S, D], BF16)
        self.XS = sc("XS", [self.NB * self.MB, D], BF16)
        self.YB = sc("YB", [self.NB * self.MB, D], F32)
        NT = self.NT
        self.rX = self.RL("X", NT)
        self.rY = [self.RL(f"Y{m}_", NT) for m in range(4)]
        self.rProj = self.RL("proj", max(1, S // 512))
        self.rXC = self.R("XC")

    def consts(self):
        nc = self.nc
        self.ones_f = self.sb("ones_f", [128, 512], F32)
        self.zeros_f = self.sb("zeros_f", [128, 128], F32)
        self.ident_f = self.sb("ident_f", [128, 128], F32)
        self.ident_b = self.sb("ident_b", [128, 128], BF16)
        self.maskb = self.sb("maskb", [128, 128], F32)
        self.mask01 = self.sb("mask01", [128, 128], BF16)
        self.mask01f = self.sb("mask01f", [128, 128], F32)
        self.ones_b = self.sb("ones_b", [128, 128], BF16)
        self.rC = self.R("consts")
        r = self.rC
        self.op("pool", lambda e: e.memset(self.ones_f[:], 1.0), writes=[r])
        self.op("pool", lambda e: e.memset(self.zeros_f[:], 0.0), writes=[r])
        self.op("pool", lambda e: e.memset(self.ones_b[:], 1.0), writes=[r])
        self.op("pool", lambda e: e.affine_select(out=self.ident_f[:], in_=self.ones_f[:, 0:128], pattern=[[-1, 128]],
                                                   compare_op=ALU.is_equal, fill=0.0, base=0, channel_multiplier=1),
                reads=[r], writes=[r])
        self.op("pool", lambda e: e.tensor_copy(out=self.ident_b[:], in_=self.ident_f[:]), reads=[r], writes=[r])
        self.op("pool", lambda e: e.affine_select(out=self.maskb[:], in_=self.zeros_f[:], pattern=[[1, 128]],
                                                   compare_op=ALU.is_ge, fill=NEG, base=0, channel_multiplier=-1),
                reads=[r], writes=[r])
        self.op("pool", lambda e: e.affine_select(out=self.mask01f[:], in_=self.ones_f[:, 0:128], pattern=[[1, 128]],
                                                   compare_op=ALU.is_ge, fill=0.0, base=0, channel_multiplier=-1),
                reads=[r], writes=[r])
        self.op("pool", lambda e: e.tensor_copy(out=self.mask01[:], in_=self.mask01f[:]), reads=[r], writes=[r])
        self.sel = self.sb("sel", [4, 4, 128], F32)
        self.seln = self.sb("seln", [4, 4, 128], F32)
        self.op("pool", lambda e: e.memset(self.sel[:], 1.0), writes=[r])
        for h in range(4):
            self.op("pool", lambda e, h=h: e.affine_select(out=self.sel[:, h, :], in_=self.sel[:, h, :], pattern=[[0, 128]],
                                                            compare_op=ALU.is_equal, fill=0.0, base=-h, channel_multiplier=1),
                    reads=[r], writes=[r])
        self.op("pool", lambda e: e.tensor_scalar(out=self.seln[:], in0=self.sel[:], scalar1=-1.0, scalar2=None, op0=ALU.mult),
                reads=[r], writes=[r])
        self.sel127 = self.sb("sel127", [128, 128], F32)
        self.op("pool", lambda e: e.affine_select(out=self.sel127[:], in_=self.ones_f[:, 0:128], pattern=[[0, 128]],
                                                   compare_op=ALU.is_equal, fill=0.0, base=-127, channel_multiplier=1),
                reads=[r], writes=[r])
        self.condT = self.sb("condT", [128, 8], F32)
        self.condB = self.sb("condB", [128, 8, 128], F32)
        self.dma("sp", lambda e: e.dma_start(out=self.condT[:], in_=self.c_in.rearrange("o (k p) -> p (o k)", p=128),
                                             allow_slow_non_contiguous=True), "cst", writes=[r])
        self.op("act", lambda e: e.activation(out=self.condT[:], in_=self.condT[:], func=AF.Silu), reads=[r], writes=[r])
        for kc in range(8):
            self.op("dve", lambda e, kc=kc: e.tensor_scalar(out=self.condB[:, kc, :], in0=self.ones_f[:, 0:128],
                                                            scalar1=self.condT[:, kc:kc + 1], scalar2=None, op0=ALU.mult),
                    reads=[r], writes=[r])
        self.UTs = self.sb("UTs", [128, 128], BF16)
        utf = self.sb("utf", [128, 128], F32)
        self.op("pool", lambda e: e.affine_select(out=utf[:], in_=self.ones_f[:, 0:128], pattern=[[1, 128]], compare_op=ALU.is_gt, fill=0.0, base=0, channel_multiplier=-1),
                reads=[r], writes=[r])
        self.op("pool", lambda e: e.tensor_copy(out=self.UTs[:], in_=utf[:]), reads=[r], writes=[r])
        self.GATES = self.sb("GATES", [128, self.NT, 2], F32)
        self.DEST = self.sb("DEST", [128, self.NT, 2], I32)
        self.IDXW = self.sb("IDXW", [128, self.NB], I32)
        self.rPLAN = self.R("plan")
        self.MOD = self.sb("MOD", [128, 6 * D], F32)
        self.rMOD = self.R("MOD")

    def adaln(self, l):
        r = self.rMOD
        with self.phase():
            st = [self.tsb("ada_st", [128, 8, 512], F32) for _ in range(2)]
            rst = self.RL("ada_st", 2)
            bb = [self.tsb("ada_b", [128, 512], F32) for _ in range(2)]
            rbb = self.RL("ada_b", 2)
            pm = [self.tps("ada_ps", [128, 512], F32) for _ in range(2)]
            rpm = self.RLP("ada_ps", 2)
            wn = self.tsb("ada_wn", [128, D], F32)
            rwn = self.R("ada_wn")
            for nb in range(12):
                i = nb % 2
                self.dma("sp", lambda e, nb=nb, i=i: e.dma_start(
                    out=st[i][:], in_=self.w_ada[l, :, nb * 512:(nb + 1) * 512].rearrange("(k p) n -> p k n", p=128)),
                    f"ada_st{i}", writes=[rst[i]])
                self.dma("sp", lambda e, nb=nb, i=i: e.dma_start(
                    out=bb[i][:], in_=self.b_ada[l:l + 1, nb * 512:(nb + 1) * 512].partition_broadcast(128)),
                    f"ada_b{i}", writes=[rbb[i]])
                for kc in range(8):
                    self.op("pe", lambda e, kc=kc, i=i: e.matmul(pm[i][:], lhsT=self.condB[:, kc, :], rhs=st[i][:, kc, :],
                                                                 start=(kc == 0), stop=(kc == 7)),
                            reads=[rst[i], self.rC], writes=[rpm[i]])
                self.op("dve", lambda e, nb=nb, i=i: e.tensor_tensor(out=self.MOD[:, nb * 512:(nb + 1) * 512], in0=pm[i][:],
                                                                     in1=bb[i][:], op=ALU.add),
                        reads=[rpm[i], rbb[i]], writes=[r])
            for j, wnorm in ((1, self.w_norm1), (4, self.w_norm2)):
                self.dma("sp", lambda e, wnorm=wnorm: e.dma_start(out=wn[:], in_=wnorm[l:l + 1, :].partition_broadcast(128)),
                         "ada_wn", writes=[rwn])
                self.op("dve", lambda e, j=j: e.scalar_tensor_tensor(out=self.MOD[:, j * D:(j + 1) * D], in0=self.MOD[:, j * D:(j + 1) * D],
                                                                     scalar=1.0, in1=wn[:], op0=ALU.add, op1=ALU.mult),
                        reads=[rwn], writes=[r])

    def head_rms(self, eng, src, n, gain, out, tmp, ss, reads, writes):
        rt = self.R("hr")
        self.op(eng, lambda e: e.tensor_tensor(out=tmp, in0=src, in1=src, op=ALU.mult), reads=reads, writes=[rt])
        self.op("dve", lambda e: e.tensor_reduce(out=ss, in_=tmp, axis=AX.X, op=ALU.add), reads=[rt], writes=[rt])
        self.op("act", lambda e: e.activation(out=ss, in_=ss, func=AF.Sqrt, bias=EPS, scale=1.0 / 64.0), reads=[rt], writes=[rt])
        self.op("dve", lambda e: e.reciprocal(out=ss, in_=ss), reads=[rt], writes=[rt])
        self.op(eng, lambda e: e.tensor_tensor(out=tmp, in0=src, in1=ss.unsqueeze(2).to_broadcast([128, n, 64]), op=ALU.mult),
                reads=[rt] + list(reads), writes=[rt])
        self.op(eng, lambda e: e.tensor_tensor(out=out, in0=tmp, in1=gain, op=ALU.mult), reads=[rt], writes=writes)

    def phase_a(self, l):
        S, NT = self.S, self.NT
        NB = S // 512
        with self.phase():
            W = self.tsb("W", [128, 8, WCOLS], BF16)
            rW = self.R("W")
            st = [self.tsb("wst", [128, 8, 256], F32) for _ in range(2)]
            rst = self.RL("wst", 2)
            pieces = []
            dst = 0
            for name, c0 in TM_SEGS:
                pieces.append((c0, 256, dst, 0.125 if name == "mk" else 1.0))
                dst += 256
            for name, c0, w in CM_SEGS:
                for o in range(0, w, 256):
                    pieces.append((c0 + o, 256, dst, 0.125 if name in ("aq", "mkt") else 1.0))
                    dst += 256
            for c0 in (C_AF, C_DT, C_MI, C_MF):
                pieces.append((c0, 4, dst, 1.0))
                dst += 4
            assert dst == WCOLS
            for pi, (c0, w, d0, scale) in enumerate(pieces):
                i = pi % 2
                self.dma("sp", lambda e, c0=c0, w=w, i=i: e.dma_start(
                    out=st[i][:, :, 0:w], in_=self.w_in[l, :, c0:c0 + w].rearrange("(k p) n -> p k n", p=128),
                    allow_slow_non_contiguous=(w < 64)), f"wst{i}", writes=[rst[i]])
                self.op("dve" if i == 0 else "pool", lambda e, w=w, d0=d0, i=i, scale=scale: e.tensor_scalar(
                    out=W[:, :, d0:d0 + w], in0=st[i][:, :, 0:w], scalar1=scale, scalar2=None, op0=ALU.mult),
                    reads=[rst[i]], writes=[rW])
            xt = [self.tsb("xt", [128, D], F32) for _ in range(2)]
            rxt = self.RL("xt", 2)
            tmp = [self.tsb("htmp", [128, D], F32) for _ in range(2)]
            rtmp = self.RL("htmp", 2)
            hb = [self.tsb("hb", [128, D], BF16) for _ in range(2)]
            rhb = self.RL("hb", 2)
            ss = [self.tsb("ss", [128, 1], F32) for _ in range(2)]
            hT = [self.tsb("hT", [128, 8, 512], BF16) for _ in range(2)]
            rhT = self.RL("hT", 2)
            pT = [self.tps("pT", [128, 4, 128], BF16) for _ in range(2)]
            rpT = self.RLP("pT", 2)
            NPM = 6
            pm = [self.tps("pm", [128, 512], F32) for _ in range(NPM)]
            rpm = self.RLP("pm", NPM)
            ost = [self.tsb("ost", [128, 512], BF16) for _ in range(NPM)]
            rost = self.RL("ost", NPM)
            gst = self.tsb("gst", [16, 512], F32)
            rgst = self.R("gst")
            tm_names = [n for n, _ in TM_SEGS]
            tm_dest = {"su": self.SU, "sv": self.SV, "av": self.AV, "mv": self.MV, "mk": self.MK, "sz": self.SZ, "mo": self.MO}
            tm_func = {"su": AF.Gelu_apprx_tanh, "sv": AF.Gelu_apprx_tanh, "sz": AF.Silu, "mo": AF.Sigmoid}
            cm_dest = [(self.QT, 0), (self.QT, 128), (self.KT, 0), (self.KT, 128)] + [(self.XBC, 128 * j) for j in range(6)] + \
                      [(self.MQT, 0), (self.MQT, 128), (self.MKT, 0), (self.MKT, 128)]
            npm = 0
            nost = 0
            ntile = 0

            hb8 = [self.tsb("hb8", [128, D], BF16) for _ in range(8)]
            rhb8 = self.RL("hb8", 8)

            def load_x(ti):
                i = ti % 2
                self.dma("sp", lambda e: e.dma_start(out=xt[i][:], in_=self.X[ti * 128:(ti + 1) * 128, :]), f"xt{i}", writes=[rxt[i]])

            def norm_block(tb):
                for tt in range(4):
                    ti = tb * 4 + tt
                    i = ti % 2
                    hbi = (tb % 2) * 4 + tt
                    if ti + 1 < NT:
                        load_x(ti + 1)
                    self.op("pool", lambda e, i=i: e.memset(ss[i][:], 0.0), writes=[rtmp[i]])
                    self.op("act", lambda e, i=i: e.activation(out=tmp[i][:], in_=xt[i][:], func=AF.Square, accum_out=ss[i][:]),
                            reads=[rxt[i]], writes=[rtmp[i]])
                    self.op("act", lambda e, i=i: e.activation(out=ss[i][:], in_=ss[i][:], func=AF.Sqrt, bias=EPS, scale=1.0 / D),
                            writes=[rtmp[i]])
                    self.op("dve", lambda e, i=i: e.reciprocal(out=ss[i][:], in_=ss[i][:]), writes=[rtmp[i]])
                    self.op("dve", lambda e, i=i: e.scalar_tensor_tensor(out=tmp[i][:], in0=xt[i][:], scalar=ss[i][:, 0:1],
                                                                         in1=self.MOD[:, D:2 * D], op0=ALU.mult, op1=ALU.mult),
                            reads=[rxt[i], self.rMOD], writes=[rtmp[i]])
                    self.op("pool", lambda e, i=i, hbi=hbi: e.tensor_tensor(out=hb8[hbi][:], in0=tmp[i][:], in1=self.MOD[:, 0:D], op=ALU.add),
                            reads=[rtmp[i], self.rMOD], writes=[rhb8[hbi]])

            def transp_block(tb):
                b = tb % 2
                for tt in range(4):
                    hbi = (tb % 2) * 4 + tt
                    for half in range(2):
                        for j in range(4):
                            kc = half * 4 + j
                            self.op("pe", lambda e, half=half, j=j, kc=kc, hbi=hbi: e.transpose(
                                out=pT[half][:, j, :], in_=hb8[hbi][:, kc * 128:(kc + 1) * 128], identity=self.ident_b[:]),
                                reads=[rhb8[hbi], self.rC], writes=[rpT[half]])
                        if half == 0:
                            self.op("act", lambda e, tt=tt, b=b: e.copy(out=hT[b][:, 0:4, tt * 128:(tt + 1) * 128], in_=pT[0][:]),
                                    reads=[rpT[0]], writes=[rhT[b]])
                        else:
                            self.op("dve", lambda e, tt=tt, b=b: e.tensor_copy(out=hT[b][:, 4:8, tt * 128:(tt + 1) * 128], in_=pT[1][:]),
                                    reads=[rpT[1]], writes=[rhT[b]])

            load_x(0)
            norm_block(0)
            transp_block(0)
            for tb in range(NB):
                b = tb % 2
                if tb + 1 < NB:
                    norm_block(tb + 1)
                for cb in range(4):
                    c0 = cb * 512
                    w = min(512, N_TM - c0)
                    for tt in range(4):
                        ti = tb * 4 + tt
                        p = npm % NPM
                        npm += 1
                        for kc in range(8):
                            self.op("pe", lambda e, p=p, kc=kc, tt=tt, b=b, c0=c0, w=w: e.matmul(
                                pm[p][:, 0:w], lhsT=hT[b][:, kc, tt * 128:(tt + 1) * 128], rhs=W[:, kc, c0:c0 + w],
                                start=(kc == 0), stop=(kc == 7)), reads=[rhT[b], rW], writes=[rpm[p]])
                        o = nost % NPM
                        nost += 1
                        for sgi in range(w // 256):
                            name = tm_names[(c0 + sgi * 256) // 256]
                            if name in tm_func:
                                self.op("act", lambda e, o=o, p=p, sgi=sgi, name=name: e.activation(
                                    out=ost[o][:, sgi * 256:(sgi + 1) * 256], in_=pm[p][:, sgi * 256:(sgi + 1) * 256], func=tm_func[name]),
                                    reads=[rpm[p]], writes=[rost[o]])
                            else:
                                self.op("dve", lambda e, o=o, p=p, sgi=sgi: e.tensor_copy(
                                    out=ost[o][:, sgi * 256:(sgi + 1) * 256], in_=pm[p][:, sgi * 256:(sgi + 1) * 256]),
                                    reads=[rpm[p]], writes=[rost[o]])
                            self.dma("sp", lambda e, o=o, sgi=sgi, name=name, ti=ti: e.dma_start(
                                out=tm_dest[name][ti * 128:(ti + 1) * 128, :], in_=ost[o][:, sgi * 256:(sgi + 1) * 256]),
                                f"ost{o}", reads=[rost[o]])
                for ch in range(15):
                    m = 128 if ch < 14 else 16
                    c0 = N_TM + ch * 128
                    p = npm % NPM
                    npm += 1
                    for kc in range(8):
                        self.op("pe", lambda e, p=p, kc=kc, b=b, c0=c0, m=m: e.matmul(
                            pm[p][0:m, :], lhsT=W[:, kc, c0:c0 + m], rhs=hT[b][:, kc, :], start=(kc == 0), stop=(kc == 7)),
                            reads=[rhT[b], rW], writes=[rpm[p]])
                    if ch < 14:
                        o = nost % NPM
                        nost += 1
                        self.op("dve" if ch % 2 == 0 else "act", lambda e, o=o, p=p, ch=ch: (
                            e.tensor_copy(out=ost[o][:], in_=pm[p][:]) if ch % 2 == 0 else e.copy(out=ost[o][:], in_=pm[p][:])),
                            reads=[rpm[p]], writes=[rost[o]])
                        dt_, r0 = cm_dest[ch]
                        self.dma("sp", lambda e, o=o, dt_=dt_, r0=r0, tb=tb: e.dma_start(
                            out=dt_[r0:r0 + 128, tb * 512:(tb + 1) * 512], in_=ost[o][:]), f"ost{o}", reads=[rost[o]])
                    else:
                        self.op("dve", lambda e, p=p: e.tensor_copy(out=gst[:], in_=pm[p][0:16, :]), reads=[rpm[p]], writes=[rgst])
                        self.dma("sp", lambda e, tb=tb: e.dma_start(out=self.GR[:, tb * 512:(tb + 1) * 512], in_=gst[:]),
                                 "gst", reads=[rgst])
                if tb + 1 < NB:
                    transp_block(tb + 1)

    def logsigmoid_rows(self, g, t1, t2, r):
        self.op("dve", lambda e: e.scalar_tensor_tensor(out=t1, in0=g, scalar=-1.0, in1=g, op0=ALU.mult, op1=ALU.max), reads=[r], writes=[r])
        self.op("act", lambda e: e.activation(out=t1, in_=t1, func=AF.Exp, scale=-1.0), reads=[r], writes=[r])
        self.op("act", lambda e: e.activation(out=t1, in_=t1, func=AF.Ln, bias=1.0), reads=[r], writes=[r])
        self.op("dve", lambda e: e.tensor_scalar_min(out=t2, in0=g, scalar1=0.0), reads=[r], writes=[r])
        self.op("dve", lambda e: e.tensor_tensor(out=g, in0=t2, in1=t1, op=ALU.subtract), reads=[r], writes=[r])

    def load_col(self, dst, src_row, key, r):
        self.dma("sp", lambda e: e.dma_start(out=dst, in_=src_row.rearrange("o h -> h o"), allow_slow_non_contiguous=True), key, writes=[r])

    def phase_attn(self, l):
        S, NT = self.S, self.NT
        NQ = S // 512
        TB = self.TBK
        with self.phase():
            g = self.tsb("ag", [4, TB], F32)
            t1 = self.tsb("at1", [4, TB], F32)
            t2 = self.tsb("at2", [4, TB], F32)
            fb = self.tsb("afb", [4, 1], F32)
            carry = self.tsb("acarry", [4, 1], F32)
            fq = self.tsb("afq", [4, 3, TB], BF16)
            fk = self.tsb("afk", [4, 3, TB], BF16)
            rg = self.R("agates")
            self.load_col(fb[:], self.attn_f_bias[l:l + 1, :], "ag", rg)
            self.op("pool", lambda e: e.memset(carry[:], 0.0), writes=[rg])
            ones_bc = self.ones_f[0:4, 0:1].to_broadcast([4, TB])
            for blk in range(S // TB):
                t0 = blk * TB
                self.dma("sp", lambda e, t0=t0: e.dma_start(out=g[:], in_=self.GR[0:4, t0:t0 + TB]), "ag", writes=[rg])
                self.op("dve", lambda e: e.tensor_scalar(out=g[:], in0=g[:], scalar1=fb[:, 0:1], scalar2=None, op0=ALU.add), reads=[rg], writes=[rg])
                self.logsigmoid_rows(g[:], t1[:], t2[:], rg)
                self.op("dve", lambda e: e.tensor_tensor_scan(out=t1[:], data0=ones_bc, data1=g[:], initial=carry[:, 0:1], op0=ALU.mult, op1=ALU.add),
                        reads=[rg, self.rC], writes=[rg])
                self.op("dve", lambda e: e.tensor_copy(out=carry[:], in_=t1[:, TB - 1:TB]), reads=[rg], writes=[rg])
                self.op("dve", lambda e: e.tensor_copy(out=fq[:, 0, :], in_=t1[:]), reads=[rg], writes=[rg])
                self.op("dve", lambda e: e.tensor_copy(out=t2[:], in_=fq[:, 0, :]), reads=[rg], writes=[rg])
                self.op("dve", lambda e: e.tensor_tensor(out=g[:], in0=t1[:], in1=t2[:], op=ALU.subtract), reads=[rg], writes=[rg])
                self.op("dve", lambda e: e.tensor_copy(out=fq[:, 1, :], in_=g[:]), reads=[rg], writes=[rg])
                self.op("dve", lambda e: e.tensor_copy(out=t2[:], in_=fq[:, 1, :]), reads=[rg], writes=[rg])
                self.op("dve", lambda e: e.tensor_tensor(out=t1[:], in0=g[:], in1=t2[:], op=ALU.subtract), reads=[rg], writes=[rg])
                self.op("dve", lambda e: e.tensor_copy(out=fq[:, 2, :], in_=t1[:]), reads=[rg], writes=[rg])
                self.op("dve", lambda e: e.tensor_scalar(out=fk[:], in0=fq[:], scalar1=-1.0, scalar2=None, op0=ALU.mult), reads=[rg], writes=[rg])
                self.dma("sp", lambda e, t0=t0: e.dma_start(out=self.FQd[:, :, t0:t0 + TB], in_=fq[:]), "ag", reads=[rg], writes=[rg])
                self.dma("sp", lambda e, t0=t0: e.dma_start(out=self.FKd[:, :, t0:t0 + TB], in_=fk[:]), "ag", reads=[rg], writes=[rg])
        with self.phase():
            gain = self.tsb("again", [128, 256], F32)
            rg = self.R("again")
            self.dma("sp", lambda e: e.dma_start(out=gain[:], in_=self.w_mix_norm[l:l + 1, 0:256].partition_broadcast(128)), "ag", writes=[rg])
            FQ, FK = self.FQd, self.FKd
            QA = [self.tsb("QA", [70, S], BF16) for _ in range(2)]
            KA = [self.tsb("KA", [70, S], BF16) for _ in range(2)]
            VA = [self.tsb("VA", [128, NT, 65], BF16) for _ in range(2)]
            rH = self.RL("ahead", 2)
            for i in range(2):
                self.op("pool", lambda e, i=i: e.memset(QA[i][64:70, :], 1.0), writes=[rH[i]])
                self.op("pool", lambda e, i=i: e.memset(KA[i][64:70, :], 1.0), writes=[rH[i]])
                self.op("pool", lambda e, i=i: e.memset(VA[i][:, :, 64:65], 1.0), writes=[rH[i]])
            NSP, NPT = 5, 6
            sps = [self.tps("asp", [128, 512], F32) for _ in range(NSP)]
            rsp = self.RLP("asp", NSP)
            pts = [self.tsb("apt", [128, 512], BF16) for _ in range(NPT)]
            rpt = self.RL("apt", NPT)
            Ops = [self.tps("aO", [65, 512], F32) for _ in range(2)]
            rO = self.RLP("aO", 2)
            OT = self.tsb("aOT", [65, 512], F32)
            rOT = self.R("aOT")
            po = self.tps("apo", [128, 4, 65], F32)
            rpo = self.RP("apo")
            rden = self.tsb("arden", [128, 4], F32)
            hn = self.tsb("ahn", [128, 4, 64], F32)
            htmp = self.tsb("ahtmp", [128, 4, 64], F32)
            hss = self.tsb("ahss", [128, 4], F32)
            yb = [self.tsb("ayb", [128, 4, 64], BF16) for _ in range(2)]
            ryb = self.RL("ayb", 2)
            rhn = self.R("ahn")

            def load_head(h):
                i = h % 2
                q = "pool"
                self.dma(q, lambda e: e.dma_start(out=QA[i][0:64, :], in_=self.QT[h * 64:(h + 1) * 64, :]), f"ahd{i}", writes=[rH[i]])
                self.dma(q, lambda e: e.dma_start(out=KA[i][0:64, :], in_=self.KT[h * 64:(h + 1) * 64, :]), f"ahd{i}", writes=[rH[i]])
                for j in range(3):
                    self.dma(q, lambda e, j=j: e.dma_start(out=QA[i][64 + j:65 + j, :], in_=FQ[h:h + 1, j, :]), f"ahd{i}", reads=[rg], writes=[rH[i]])
                    self.dma(q, lambda e, j=j: e.dma_start(out=KA[i][67 + j:68 + j, :], in_=FK[h:h + 1, j, :]), f"ahd{i}", reads=[rg], writes=[rH[i]])
                self.dma(q, lambda e: e.dma_start(out=VA[i][:, :, 0:64], in_=self.AV[:, h * 64:(h + 1) * 64].rearrange("(n p) d -> p n d", p=128)),
                         f"ahd{i}", writes=[rH[i]])

            steps = [(h, qb, kt) for h in range(4) for qb in range(NQ) for kt in range(4 * (qb + 1))]
            nyb = [0]

            def emit_qk(idx):
                h, qb, kt = steps[idx]
                i = h % 2
                c0 = max(kt - 4 * qb, 0) * 128
                sp, pt = idx % NSP, idx % NPT
                self.op("pe", lambda e: e.matmul(sps[sp][:, c0:512], lhsT=KA[i][0:70, kt * 128:(kt + 1) * 128],
                                                 rhs=QA[i][0:70, qb * 512 + c0:(qb + 1) * 512], start=True, stop=True),
                        reads=[rH[i]], writes=[rsp[sp]])
                self.op("act", lambda e: e.activation(out=pts[pt][:, c0:512], in_=sps[sp][:, c0:512], func=AF.Exp),
                        reads=[rsp[sp]], writes=[rpt[pt]])
                if kt - 4 * qb >= 0:
                    self.op("pool", lambda e: e.tensor_tensor(out=pts[pt][:, c0:c0 + 128], in0=pts[pt][:, c0:c0 + 128], in1=self.mask01[:], op=ALU.mult),
                            reads=[rpt[pt], self.rC], writes=[rpt[pt]])

            def emit_pv(idx):
                h, qb, kt = steps[idx]
                i = h % 2
                c0 = max(kt - 4 * qb, 0) * 128
                pt = idx % NPT
                ob = (h * NQ + qb) % 2
                nk = 4 * (qb + 1)
                self.op("pe", lambda e: e.matmul(Ops[ob][:, c0:512], lhsT=VA[i][:, kt, :], rhs=pts[pt][:, c0:512], start=(kt == 0), stop=(kt == nk - 1)),
                        reads=[rH[i], rpt[pt]], writes=[rO[ob]])
                if kt == nk - 1:
                    self.op("dve", lambda e: e.tensor_copy(out=OT[:], in_=Ops[ob][:]), reads=[rO[ob]], writes=[rOT])
                    for tt in range(4):
                        self.op("pe", lambda e, tt=tt: e.transpose(out=po[:, tt, :], in_=OT[0:65, tt * 128:(tt + 1) * 128], identity=self.ident_f[0:65, 0:65]),
                                reads=[rOT, self.rC], writes=[rpo])
                    self.op("dve", lambda e: e.reciprocal(out=rden[:], in_=po[:, :, 64]), reads=[rpo], writes=[rhn])
                    self.op("dve", lambda e: e.tensor_tensor(out=hn[:], in0=po[:, :, 0:64], in1=rden[:].unsqueeze(2).to_broadcast([128, 4, 64]), op=ALU.mult),
                            reads=[rpo, rhn], writes=[rhn])
                    y = nyb[0] % 2
                    nyb[0] += 1
                    self.head_rms("pool", hn[:], 4, gain[:, h * 64:(h + 1) * 64].unsqueeze(1).to_broadcast([128, 4, 64]), yb[y][:], htmp[:], hss[:],
                                  reads=[rhn, rg], writes=[ryb[y]])
                    self.dma("sp", lambda e: e.dma_start(out=self.Y[qb * 512:(qb + 1) * 512, h * 64:(h + 1) * 64].rearrange("(n p) d -> p n d", p=128),
                                                         in_=yb[y][:]), f"ayb{y}", reads=[ryb[y]])
                    if qb == NQ - 1 and h + 2 < 4:
                        load_head(h + 2)

            load_head(0)
            load_head(1)
            SK = 4
            for idx in range(len(steps)):
                emit_qk(idx)
                if idx >= SK:
                    emit_pv(idx - SK)
            for idx in range(max(0, len(steps) - SK), len(steps)):
                emit_pv(idx)

    def phase_sg(self, l):
        S, NT = self.S, self.NT
        with self.phase():
            wst = [self.tsb("gwst", [128, 128], F32) for _ in range(2)]
            rwst = self.RL("gwst", 2)
            pw = self.tps("gpw", [128, 128], F32)
            rpw = self.RP("gpw")
            WT = self.tsb("gWT", [128, 4, 128], BF16)
            SGB = self.tsb("gSGB", [128, 4], F32)
            gain = self.tsb("ggain", [128, 256], F32)
            rW = self.R("gW")
            for h in range(4):
                i = h % 2
                self.dma("sp", lambda e, h=h, i=i: e.dma_start(out=wst[i][:], in_=self.sg_w[l, h, :, :]), f"gwst{i}", writes=[rwst[i]])
                self.op("pe", lambda e, i=i: e.transpose(out=pw[:], in_=wst[i][:], identity=self.ident_f[:]), reads=[rwst[i], self.rC], writes=[rpw])
                self.op("dve", lambda e, h=h: e.tensor_tensor(out=WT[:, h, :], in0=pw[:], in1=self.mask01f[:], op=ALU.mult), reads=[rpw, self.rC], writes=[rW])
            self.dma("sp", lambda e: e.dma_start(out=SGB[:], in_=self.sg_b[l, :, :].rearrange("h t -> t h"), allow_slow_non_contiguous=True), "gw", writes=[rW])
            self.dma("sp", lambda e: e.dma_start(out=gain[:], in_=self.w_mix_norm[l:l + 1, 256:512].partition_broadcast(128)), "gw", writes=[rW])
            su = [self.tsb("gsu", [128, 256], BF16) for _ in range(2)]
            sv = [self.tsb("gsv", [128, 256], BF16) for _ in range(2)]
            rin = self.RL("gin", 2)
            svf = self.tsb("gsvf", [128, 4, 64], F32)
            htmp = self.tsb("ghtmp", [128, 4, 64], F32)
            hss = self.tsb("ghss", [128, 4], F32)
            rsvf = self.R("gsvf")
            vn = [self.tsb("gvn", [128, 4, 64], BF16) for _ in range(2)]
            rvn = self.RL("gvn", 2)
            pm = [self.tps("gpm", [128, 256], F32) for _ in range(2)]
            rpm = self.RLP("gpm", 2)
            yb = [self.tsb("gyb", [128, 256], BF16) for _ in range(2)]
            ryb = self.RL("gyb", 2)
            def load_sg(c):
                i = c % 2
                self.dma("sp", lambda e: e.dma_start(out=su[i][:], in_=self.SU[c * 128:(c + 1) * 128, :]), f"gin{i}", writes=[rin[i]])
                self.dma("sp", lambda e: e.dma_start(out=sv[i][:], in_=self.SV[c * 128:(c + 1) * 128, :]), f"gin{i}", writes=[rin[i]])

            load_sg(0)
            for c in range(NT):
                i = c % 2
                if c + 1 < NT:
                    load_sg(c + 1)
                self.op("pool", lambda e, i=i: e.tensor_copy(out=svf[:], in_=sv[i][:].rearrange("p (h d) -> p h d", d=64)), reads=[rin[i]], writes=[rsvf])
                self.head_rms("pool", svf[:], 4, gain[:].rearrange("p (h d) -> p h d", d=64), vn[i][:], htmp[:], hss[:], reads=[rsvf, rW], writes=[rvn[i]])
                for h in range(4):
                    self.op("pe", lambda e, h=h, i=i: e.matmul(pm[i][:, h * 64:(h + 1) * 64], lhsT=WT[:, h, :], rhs=vn[i][:, h, :], start=True, stop=True),
                            reads=[rW, rvn[i]], writes=[rpm[i]])
                for h in range(4):
                    self.op("dve", lambda e, h=h, i=i: e.scalar_tensor_tensor(out=yb[i][:, h * 64:(h + 1) * 64], in0=pm[i][:, h * 64:(h + 1) * 64],
                                                                              scalar=SGB[:, h:h + 1], in1=su[i][:, h * 64:(h + 1) * 64], op0=ALU.add, op1=ALU.mult),
                            reads=[rpm[i], rin[i], rW], writes=[ryb[i]])
                self.dma("sp", lambda e, c=c, i=i: e.dma_start(out=self.Y[c * 128:(c + 1) * 128, 256:512], in_=yb[i][:]), f"gyb{i}", reads=[ryb[i]])

    def phase_ssd(self, l):
        S, NT = self.S, self.NT
        TB = self.TBK
        with self.phase():
            CW = self.tsb("cCW", [128, 6, 4], F32)
            CB = self.tsb("cCB", [128, 6], F32)
            rcw = self.R("cCW")
            for j in range(4):
                self.dma("sp", lambda e, j=j: e.dma_start(out=CW[:, :, j], in_=self.ssm_conv_w[l, j:j + 1, :].rearrange("o (c p) -> p (o c)", p=128), allow_slow_non_contiguous=True), "ccw", writes=[rcw])
            self.dma("sp", lambda e: e.dma_start(out=CB[:], in_=self.ssm_conv_b[l:l + 1, :].rearrange("o (c p) -> p (o c)", p=128), allow_slow_non_contiguous=True), "ccw", writes=[rcw])
            xin = [self.tsb("cxin", [128, TB + 3], BF16) for _ in range(2)]
            rxin = self.RL("cxin", 2)
            acc = [self.tsb("cacc", [128, TB], F32) for _ in range(2)]
            racc = self.RL("cacc", 2)
            xo = [self.tsb("cxo", [128, TB], BF16) for _ in range(2)]
            rxo = self.RL("cxo", 2)
            n = 0
            for cc in range(6):
                for blk in range(S // TB):
                    i = n % 2
                    n += 1
                    t0 = blk * TB
                    if blk == 0:
                        self.op("pool", lambda e, i=i: e.memset(xin[i][:, 0:3], 0.0), writes=[rxin[i]])
                        self.dma("sp", lambda e, i=i, cc=cc: e.dma_start(out=xin[i][:, 3:TB + 3], in_=self.XBC[cc * 128:(cc + 1) * 128, 0:TB]), f"cxin{i}", writes=[rxin[i]])
                    else:
                        self.dma("sp", lambda e, i=i, cc=cc, t0=t0: e.dma_start(out=xin[i][:, 0:TB + 3], in_=self.XBC[cc * 128:(cc + 1) * 128, t0 - 3:t0 + TB]),
                                 f"cxin{i}", writes=[rxin[i]])
                    eng = "dve"
                    self.op(eng, lambda e, i=i, cc=cc: e.tensor_scalar(out=acc[i][:], in0=xin[i][:, 0:TB], scalar1=CW[:, cc, 0:1], scalar2=None, op0=ALU.mult),
                            reads=[rxin[i], rcw], writes=[racc[i]])
                    for j in range(1, 4):
                        self.op(eng, lambda e, i=i, cc=cc, j=j: e.scalar_tensor_tensor(out=acc[i][:], in0=xin[i][:, j:j + TB], scalar=CW[:, cc, j:j + 1], in1=acc[i][:],
                                                                                     op0=ALU.mult, op1=ALU.add), reads=[rxin[i], rcw], writes=[racc[i]])
                    self.op("act", lambda e, i=i, cc=cc: e.activation(out=xo[i][:], in_=acc[i][:], func=AF.Silu, bias=CB[:, cc:cc + 1]),
                            reads=[racc[i], rcw], writes=[rxo[i]])
                    self.dma("sp", lambda e, i=i, cc=cc, t0=t0: e.dma_start(out=self.XC[cc * 128:(cc + 1) * 128, t0:t0 + TB], in_=xo[i][:]), f"cxo{i}", reads=[rxo[i]])
        with self.phase():
            g = self.tsb("sg_", [4, TB], F32)
            t1 = self.tsb("st1", [4, TB], F32)
            t2 = self.tsb("st2", [4, TB], F32)
            t3 = self.tsb("st3", [4, TB], F32)
            rm = self.tsb("srm", [4, TB], F32)
            dtb = self.tsb("sdtb", [4, 1], F32)
            an = self.tsb("san", [4, 1], F32)
            rg = self.R("sgates")
            self.load_col(dtb[:], self.ssm_dt_bias[l:l + 1, :], "sgt", rg)
            self.load_col(an[:], self.ssm_a_log[l:l + 1, :], "sgt", rg)
            self.op("act", lambda e: e.activation(out=an[:], in_=an[:], func=AF.Exp), reads=[rg], writes=[rg])
            self.op("dve", lambda e: e.tensor_scalar(out=an[:], in0=an[:], scalar1=-1.0, scalar2=None, op0=ALU.mult), reads=[rg], writes=[rg])
            self.op("pool", lambda e: e.memset(rm[:], 1.0), writes=[rg])
            self.op("pool", lambda e: e.memset(rm[:].rearrange("p (n c) -> p n c", c=128)[:, :, 0:1], 0.0), reads=[rg], writes=[rg])
            for blk in range(S // TB):
                t0 = blk * TB
                self.dma("sp", lambda e, t0=t0: e.dma_start(out=g[:], in_=self.GR[4:8, t0:t0 + TB]), "sgt", writes=[rg])
                self.op("dve", lambda e: e.tensor_scalar(out=g[:], in0=g[:], scalar1=dtb[:, 0:1], scalar2=None, op0=ALU.add), reads=[rg], writes=[rg])
                self.op("dve", lambda e: e.scalar_tensor_tensor(out=t1[:], in0=g[:], scalar=-1.0, in1=g[:], op0=ALU.mult, op1=ALU.max), reads=[rg], writes=[rg])
                self.op("act", lambda e: e.activation(out=t1[:], in_=t1[:], func=AF.Exp, scale=-1.0), reads=[rg], writes=[rg])
                self.op("act", lambda e: e.activation(out=t1[:], in_=t1[:], func=AF.Ln, bias=1.0), reads=[rg], writes=[rg])
                self.op("dve", lambda e: e.tensor_scalar_max(out=t2[:], in0=g[:], scalar1=0.0), reads=[rg], writes=[rg])
                self.op("dve", lambda e: e.tensor_tensor(out=g[:], in0=t2[:], in1=t1[:], op=ALU.add), reads=[rg], writes=[rg])
                self.dma("sp", lambda e, t0=t0: e.dma_start(out=self.SROW[4:8, t0:t0 + TB], in_=g[:]), "sgt", reads=[rg], writes=[rg])
                self.op("dve", lambda e: e.tensor_scalar(out=t1[:], in0=g[:], scalar1=an[:, 0:1], scalar2=None, op0=ALU.mult), reads=[rg], writes=[rg])
                self.op("dve", lambda e: e.tensor_tensor_scan(out=t2[:], data0=rm[:], data1=t1[:], initial=0.0, op0=ALU.mult, op1=ALU.add), reads=[rg], writes=[rg])
                self.dma("sp", lambda e, t0=t0: e.dma_start(out=self.SROW[16:20, t0:t0 + TB], in_=t2[:]), "sgt", reads=[rg], writes=[rg])
                v3 = lambda t: t[:].rearrange("p (n c) -> p n c", c=128)
                self.op("dve", lambda e: e.tensor_copy(out=v3(t1), in_=v3(t2)[:, :, 127:128].to_broadcast([4, TB // 128, 128])), reads=[rg], writes=[rg])
                self.op("act", lambda e: e.activation(out=t3[:], in_=t1[:], func=AF.Exp), reads=[rg], writes=[rg])
                self.dma("sp", lambda e, t0=t0: e.dma_start(out=self.SROW[12:16, t0:t0 + TB], in_=t3[:]), "sgt", reads=[rg], writes=[rg])
                self.op("dve", lambda e: e.tensor_tensor(out=t1[:], in0=t1[:], in1=t2[:], op=ALU.subtract), reads=[rg], writes=[rg])
                self.op("act", lambda e: e.activation(out=t1[:], in_=t1[:], func=AF.Exp), reads=[rg], writes=[rg])
                self.op("dve", lambda e: e.tensor_tensor(out=t1[:], in0=t1[:], in1=g[:], op=ALU.mult), reads=[rg], writes=[rg])
                self.dma("sp", lambda e, t0=t0: e.dma_start(out=self.SROW[8:12, t0:t0 + TB], in_=t1[:]), "sgt", reads=[rg], writes=[rg])
                self.op("dve", lambda e: e.tensor_scalar(out=t2[:], in0=t2[:], scalar1=-1.0, scalar2=None, op0=ALU.mult), reads=[rg], writes=[rg])
                self.dma("sp", lambda e, t0=t0: e.dma_start(out=self.SROW[0:4, t0:t0 + TB], in_=t2[:]), "sgt", reads=[rg], writes=[rg])
        with self.phase():
            gain = self.tsb("sgain", [128, 256], F32)
            Dbc = self.tsb("sDbc", [128, 4], F32)
            rk = self.R("sconst")
            self.dma("sp", lambda e: e.dma_start(out=gain[:], in_=self.w_mix_norm[l:l + 1, 512:768].partition_broadcast(128)), "sk", writes=[rk])
            self.dma("sp", lambda e: e.dma_start(out=Dbc[:], in_=self.ssm_d[l:l + 1, :].partition_broadcast(128)), "sk", writes=[rk])
            NBLK = S // 512
            xc4 = [self.tsb("sxc4", [128, 6, 512], BF16) for _ in range(2)]
            rows16 = [self.tsb("srows", [16, 512], F32) for _ in range(2)]
            arow4 = [self.tsb("sarow", [4, 512], F32) for _ in range(2)]
            rblk = self.RL("sblk", 2)
            G2 = 2
            sz = [self.tsb("ssz", [128, 256], BF16) for _ in range(G2)]
            rsz = self.RL("ssz", G2)
            TS = [self.tsb("sTS", [128, 16], F32) for _ in range(G2)]
            rTS = self.RL("sTS", G2)
            tp = self.tps("stp", [128, 4, 128], BF16)
            rtp = self.RP("stp")
            XSB = [self.tsb("sXSB", [128, 4, 128], BF16) for _ in range(G2)]
            rXSB = self.RL("sXSB", G2)
            xdt = [self.tsb("sxdt", [128, 256], BF16) for _ in range(G2)]
            xw = [self.tsb("sxw", [128, 256], BF16) for _ in range(G2)]
            rxd = self.RL("sxd", G2)
            scp = [self.tps("sscp", [128, 2, 128], F32) for _ in range(G2)]
            rscp = self.RLP("sscp", G2)
            Rp = [self.tps("sRp", [128, 128], F32) for _ in range(2)]
            rRp = self.RLP("sRp", 2)
            NR = 8
            seg = [self.tsb("sseg", [128, 128], F32) for _ in range(NR)]
            rseg = self.RL("sseg", NR)
            Mt = [self.tsb("sMt", [128, 128], BF16) for _ in range(NR)]
            rMt = self.RL("sMt", NR)
            Eb = [self.tsb("sEb", [128, 128], BF16) for _ in range(NR)]
            rEb = self.RL("sEb", NR)
            Cp = [self.tsb("sCp", [128, 128], BF16) for _ in range(NR)]
            rCp = self.RL("sCp", NR)
            yps = [self.tps("syps", [128, 256], F32) for _ in range(G2)]
            ryps = self.RLP("syps", G2)
            hps = self.tps("shps", [128, 512], F32)
            rhps = self.RP("shps")
            Hf = self.tsb("sHf", [128, 256], F32)
            rHf = self.R("sHf")
            HTb = [self.tsb("sHTb", [128, 256], BF16) for _ in range(3)]
            rHTb = self.RL("sHTb", 3)
            yf = [self.tsb("syf", [128, 4, 64], F32) for _ in range(G2)]
            dx = [self.tsb("sdx", [128, 4, 64], F32) for _ in range(G2)]
            htmp = [self.tsb("shtmp", [128, 4, 64], F32) for _ in range(G2)]
            hss = [self.tsb("shss", [128, 4], F32) for _ in range(G2)]
            ryf = self.RL("syf", G2)
            yb = [self.tsb("syb", [128, 256], BF16) for _ in range(G2)]
            ryb = self.RL("syb", G2)
            self.op("pool", lambda e: e.memset(Hf[:], 0.0), writes=[rHf])
            self.op("pool", lambda e: e.memset(HTb[0][:], 0.0), writes=[rHTb[0]])
            cnt = [0]

            def load_blk(b):
                bi = b % 2
                t0 = b * 512
                self.dma("sp", lambda e: e.dma_start(out=xc4[bi][:], in_=self.XC[:, t0:t0 + 512].rearrange("(c p) t -> p c t", p=128)), f"sblk{bi}", writes=[rblk[bi]])
                self.dma("sp", lambda e: e.dma_start(out=rows16[bi][:], in_=self.SROW[0:16, t0:t0 + 512]), f"sblk{bi}", writes=[rblk[bi]])
                self.dma("sp", lambda e: e.dma_start(out=arow4[bi][:], in_=self.SROW[16:20, t0:t0 + 512]), f"sblk{bi}", writes=[rblk[bi]])

            def chunk(c):
                i = c % G2
                b = c // 4
                bi = b % 2
                o = (c % 4) * 128
                t0 = c * 128
                if c % 4 == 1 and b + 1 < NBLK:
                    load_blk(b + 1)
                self.dma("sp", lambda e: e.dma_start(out=sz[i][:], in_=self.SZ[t0:t0 + 128, :]), f"ssz{i}", writes=[rsz[i]])
                yield
                self.op("pe", lambda e: e.transpose(out=hps[:, 256:272], in_=rows16[bi][0:16, o:o + 128], identity=self.ident_f[0:16, 0:16]), reads=[rblk[bi], self.rC], writes=[rhps])
                self.op("act", lambda e: e.copy(out=TS[i][:], in_=hps[:, 256:272]), reads=[rhps], writes=[rTS[i]])
                yield
                for j in range(4):
                    self.op("pe", lambda e, j=j: e.transpose(out=tp[:, j, :], in_=xc4[bi][:, j, o:o + 128], identity=self.ident_b[:]), reads=[rblk[bi], self.rC], writes=[rtp])
                self.op("act", lambda e: e.copy(out=XSB[i][:], in_=tp[:]), reads=[rtp], writes=[rXSB[i]])
                yield
                xs4 = XSB[i][:, 0:2, :].rearrange("p g (r d) -> p (g r) d", d=64)
                self.op("pool", lambda e: e.tensor_tensor(out=xdt[i][:].rearrange("p (h d) -> p h d", d=64), in0=xs4, in1=TS[i][:, 4:8].unsqueeze(2).to_broadcast([128, 4, 64]), op=ALU.mult),
                        reads=[rXSB[i], rTS[i]], writes=[rxd[i]])
                yield
                self.op("pool", lambda e: e.tensor_tensor(out=xw[i][:].rearrange("p (h d) -> p h d", d=64), in0=xs4, in1=TS[i][:, 8:12].unsqueeze(2).to_broadcast([128, 4, 64]), op=ALU.mult),
                        reads=[rXSB[i], rTS[i]], writes=[rxd[i]])
                yield
                for g_ in range(2):
                    self.op("pe", lambda e, g_=g_: e.matmul(hps[:, g_ * 128:(g_ + 1) * 128], lhsT=XSB[i][:, 2 + g_, :], rhs=xw[i][:, g_ * 128:(g_ + 1) * 128], start=True, stop=True),
                            reads=[rXSB[i], rxd[i]], writes=[rhps])
                for hd in range(4):
                    self.op("dve", lambda e, hd=hd: e.scalar_tensor_tensor(out=Hf[:, hd * 64:(hd + 1) * 64], in0=Hf[:, hd * 64:(hd + 1) * 64], scalar=TS[i][:, 12 + hd:13 + hd],
                                                                           in1=hps[:, hd * 64:(hd + 1) * 64], op0=ALU.mult, op1=ALU.add), reads=[rhps, rTS[i]], writes=[rHf])
                self.op("pool", lambda e: e.tensor_copy(out=HTb[(c + 1) % 3][:], in_=Hf[:]), reads=[rHf], writes=[rHTb[(c + 1) % 3]])
                yield
                for g_ in range(2):
                    self.op("pe", lambda e, g_=g_: e.matmul(scp[i][:, g_, :], lhsT=xc4[bi][:, 2 + g_, o:o + 128], rhs=xc4[bi][:, 4 + g_, o:o + 128], start=True, stop=True),
                            reads=[rblk[bi]], writes=[rscp[i]])
                yield
                ks = []
                for hd in range(4):
                    k2 = cnt[0] % 2
                    k = cnt[0] % NR
                    cnt[0] += 1
                    ks.append((k2, k))
                    self.op("pe", lambda e, hd=hd, k2=k2: e.matmul(Rp[k2][:], lhsT=self.sel[:, hd, :], rhs=arow4[bi][:, o:o + 128], start=True, stop=True), reads=[rblk[bi], self.rC], writes=[rRp[k2]])
                    self.op("dve", lambda e, hd=hd, k2=k2, k=k: e.scalar_tensor_tensor(out=seg[k][:], in0=Rp[k2][:], scalar=TS[i][:, hd:hd + 1], in1=self.maskb[:], op0=ALU.add, op1=ALU.add),
                            reads=[rRp[k2], rTS[i], self.rC], writes=[rseg[k]])
                    self.op("act", lambda e, k2=k2, k=k: e.activation(out=Eb[k][:], in_=Rp[k2][:], func=AF.Exp), reads=[rRp[k2]], writes=[rEb[k]])
                yield
                for hd in range(4):
                    g_ = hd // 2
                    k2, k = ks[hd]
                    self.op("act", lambda e, k=k: e.activation(out=seg[k][:], in_=seg[k][:], func=AF.Exp), reads=[rseg[k]], writes=[rseg[k]])
                    self.op("pool", lambda e, k=k, g_=g_: e.tensor_tensor(out=Cp[k][:], in0=xc4[bi][:, 4 + g_, o:o + 128], in1=Eb[k][:], op=ALU.mult), reads=[rblk[bi], rEb[k]], writes=[rCp[k]])
                yield
                for hd in range(4):
                    g_ = hd // 2
                    k2, k = ks[hd]
                    self.op("dve", lambda e, k=k, g_=g_: e.tensor_tensor(out=Mt[k][:], in0=scp[i][:, g_, :], in1=seg[k][:], op=ALU.mult), reads=[rscp[i], rseg[k]], writes=[rMt[k]])
                yield
                for hd in range(4):
                    k2, k = ks[hd]
                    self.op("pe", lambda e, hd=hd, k=k: e.matmul(yps[i][:, hd * 64:(hd + 1) * 64], lhsT=Mt[k][:], rhs=xdt[i][:, hd * 64:(hd + 1) * 64], start=True, stop=False),
                            reads=[rMt[k], rxd[i]], writes=[ryps[i]])
                    self.op("pe", lambda e, hd=hd, k=k: e.matmul(yps[i][:, hd * 64:(hd + 1) * 64], lhsT=Cp[k][:], rhs=HTb[c % 3][:, hd * 64:(hd + 1) * 64], start=False, stop=True),
                            reads=[rCp[k], rHTb[c % 3]], writes=[ryps[i]])
                yield
                self.op("pool", lambda e: e.tensor_tensor(out=dx[i][:], in0=xs4, in1=Dbc[:].unsqueeze(2).to_broadcast([128, 4, 64]), op=ALU.mult), reads=[rXSB[i], rk], writes=[ryf[i]])
                yield
                self.op("dve", lambda e: e.tensor_tensor(out=yf[i][:], in0=yps[i][:].rearrange("p (h d) -> p h d", d=64), in1=dx[i][:], op=ALU.add), reads=[ryps[i], ryf[i]], writes=[ryf[i]])
                yield
                self.op("pool", lambda e: e.tensor_tensor(out=yf[i][:], in0=yf[i][:], in1=sz[i][:].rearrange("p (h d) -> p h d", d=64), op=ALU.mult), reads=[rsz[i], ryf[i]], writes=[ryf[i]])
                yield
                self.head_rms("pool", yf[i][:], 4, gain[:].rearrange("p (h d) -> p h d", d=64), yb[i][:].rearrange("p (h d) -> p h d", d=64), htmp[i][:], hss[i][:],
                              reads=[ryf[i], rk], writes=[ryb[i]])
                self.dma("sp", lambda e: e.dma_start(out=self.Y[t0:t0 + 128, 512:768], in_=yb[i][:]), f"syb{i}", reads=[ryb[i]])
                yield

            load_blk(0)
            self.run_window(chunk, NT, G=PIPE_G, skew=9)

    def phase_mlstm(self, l):
        S, NT = self.S, self.NT
        TB = self.TBK
        CPB = TB // 128
        NBK = S // TB
        with self.phase():
            ig = self.tsb("mig", [4, TB], F32)
            lf = self.tsb("mlf", [4, TB], F32)
            t1 = self.tsb("mt1", [4, TB], F32)
            t2 = self.tsb("mt2", [4, TB], F32)
            rm = self.tsb("mrm", [4, TB], F32)
            rneg = self.tsb("mrneg", [4, TB], F32)
            ib = self.tsb("mib", [4, 1], F32)
            fb = self.tsb("mfb", [4, 1], F32)
            AE = self.tsb("mAE", [4, NT], F32)
            CML = self.tsb("mCML", [4, NT], F32)
            MA = self.tsb("mMA", [4, NT], F32)
            MIN = self.tsb("mMIN", [4, NT], F32)
            Q = self.tsb("mQ", [4, NT], F32)
            SCL = self.tsb("mSCL", [4, NT], F32)
            rg = self.R("mgates")
            self.load_col(ib[:], self.mlstm_i_bias[l:l + 1, :], "mgt", rg)
            self.load_col(fb[:], self.mlstm_f_bias[l:l + 1, :], "mgt", rg)
            self.op("pool", lambda e: e.memset(rm[:], 1.0), writes=[rg])
            self.op("pool", lambda e: e.memset(rm[:].rearrange("p (n c) -> p n c", c=128)[:, :, 0:1], 0.0), reads=[rg], writes=[rg])
            self.op("pool", lambda e: e.memset(rneg[:], 0.0), writes=[rg])
            self.op("pool", lambda e: e.memset(rneg[:].rearrange("p (n c) -> p n c", c=128)[:, :, 0:1], NEG), reads=[rg], writes=[rg])
            for blk in range(NBK):
                t0 = blk * TB
                self.dma("sp", lambda e, t0=t0: e.dma_start(out=ig[:], in_=self.GR[8:12, t0:t0 + TB]), "mgt", writes=[rg])
                self.dma("sp", lambda e, t0=t0: e.dma_start(out=lf[:], in_=self.GR[12:16, t0:t0 + TB]), "mgt", writes=[rg])
                self.op("dve", lambda e: e.tensor_scalar(out=ig[:], in0=ig[:], scalar1=ib[:, 0:1], scalar2=None, op0=ALU.add), reads=[rg], writes=[rg])
                self.op("dve", lambda e: e.tensor_scalar(out=lf[:], in0=lf[:], scalar1=fb[:, 0:1], scalar2=None, op0=ALU.add), reads=[rg], writes=[rg])
                self.logsigmoid_rows(lf[:], t1[:], t2[:], rg)
                self.op("dve", lambda e: e.tensor_tensor_scan(out=t1[:], data0=rm[:], data1=lf[:], initial=0.0, op0=ALU.mult, op1=ALU.add), reads=[rg], writes=[rg])
                self.op("dve", lambda e: e.tensor_tensor(out=ig[:], in0=ig[:], in1=t1[:], op=ALU.subtract), reads=[rg], writes=[rg])
                self.op("dve", lambda e: e.tensor_tensor_scan(out=t2[:], data0=rneg[:], data1=ig[:], initial=NEG, op0=ALU.add, op1=ALU.max), reads=[rg], writes=[rg])
                self.op("dve", lambda e, blk=blk: e.tensor_copy(out=AE[:, blk * CPB:(blk + 1) * CPB], in_=t1[:].rearrange("p (n c) -> p n c", c=128)[:, :, 127]), reads=[rg], writes=[rg])
                self.op("dve", lambda e, blk=blk: e.tensor_copy(out=CML[:, blk * CPB:(blk + 1) * CPB], in_=t2[:].rearrange("p (n c) -> p n c", c=128)[:, :, 127]), reads=[rg], writes=[rg])
                self.dma("sp", lambda e, t0=t0: e.dma_start(out=self.MROW[0:4, t0:t0 + TB], in_=t1[:]), "mgt", reads=[rg], writes=[rg])
                self.dma("sp", lambda e, t0=t0: e.dma_start(out=self.MROW[4:8, t0:t0 + TB], in_=ig[:]), "mgt", reads=[rg], writes=[rg])
                self.dma("sp", lambda e, t0=t0: e.dma_start(out=self.MROW[8:12, t0:t0 + TB], in_=t2[:]), "mgt", reads=[rg], writes=[rg])
            self.op("dve", lambda e: e.tensor_tensor_scan(out=MA[:], data0=CML[:], data1=AE[:], initial=NEG, op0=ALU.max, op1=ALU.add), reads=[rg], writes=[rg])
            self.op("pool", lambda e: e.memset(MIN[:, 0:1], NEG), reads=[rg], writes=[rg])
            if NT > 1:
                self.op("dve", lambda e: e.tensor_copy(out=MIN[:, 1:NT], in_=MA[:, 0:NT - 1]), reads=[rg], writes=[rg])
            self.op("dve", lambda e: e.tensor_tensor(out=Q[:], in0=AE[:], in1=MA[:], op=ALU.subtract), reads=[rg], writes=[rg])
            self.op("dve", lambda e: e.tensor_tensor(out=SCL[:], in0=MIN[:], in1=Q[:], op=ALU.add), reads=[rg], writes=[rg])
            self.dma("sp", lambda e: e.dma_start(out=self.MSCL[:, :], in_=SCL[:]), "mgt", reads=[rg], writes=[rg])
            for blk in range(NBK):
                t0 = blk * TB
                a_, b_, cm_ = t1, ig, t2
                self.dma("sp", lambda e, t0=t0: e.dma_start(out=a_[:], in_=self.MROW[0:4, t0:t0 + TB]), "mgt", reads=[rg], writes=[rg])
                self.dma("sp", lambda e, t0=t0: e.dma_start(out=b_[:], in_=self.MROW[4:8, t0:t0 + TB]), "mgt", reads=[rg], writes=[rg])
                self.dma("sp", lambda e, t0=t0: e.dma_start(out=cm_[:], in_=self.MROW[8:12, t0:t0 + TB]), "mgt", reads=[rg], writes=[rg])
                v3 = lambda t: t[:].rearrange("p (n c) -> p n c", c=128)
                bc = lambda t: t[:, blk * CPB:(blk + 1) * CPB].unsqueeze(2).to_broadcast([4, CPB, 128])
                self.op("dve", lambda e, blk=blk: e.tensor_tensor(out=v3(cm_), in0=v3(cm_), in1=MIN[:, blk * CPB:(blk + 1) * CPB].unsqueeze(2).to_broadcast([4, CPB, 128]), op=ALU.max),
                        reads=[rg], writes=[rg])
                self.op("dve", lambda e, blk=blk: e.tensor_tensor(out=v3(lf), in0=MIN[:, blk * CPB:(blk + 1) * CPB].unsqueeze(2).to_broadcast([4, CPB, 128]), in1=v3(cm_), op=ALU.subtract),
                        reads=[rg], writes=[rg])
                self.dma("sp", lambda e, t0=t0: e.dma_start(out=self.MROW[16:20, t0:t0 + TB], in_=lf[:]), "mgt", reads=[rg], writes=[rg])
                self.op("dve", lambda e: e.tensor_tensor(out=a_[:], in0=a_[:], in1=cm_[:], op=ALU.add), reads=[rg], writes=[rg])
                self.op("dve", lambda e: e.tensor_scalar(out=a_[:], in0=a_[:], scalar1=-1.0, scalar2=None, op0=ALU.mult), reads=[rg], writes=[rg])
                self.op("act", lambda e: e.activation(out=a_[:], in_=a_[:], func=AF.Exp), reads=[rg], writes=[rg])
                self.dma("sp", lambda e, t0=t0: e.dma_start(out=self.MROW[0:4, t0:t0 + TB], in_=a_[:]), "mgt", reads=[rg], writes=[rg])
                self.op("dve", lambda e: e.tensor_scalar(out=cm_[:], in0=cm_[:], scalar1=-1.0, scalar2=None, op0=ALU.mult), reads=[rg], writes=[rg])
                self.dma("sp", lambda e, t0=t0: e.dma_start(out=self.MROW[12:16, t0:t0 + TB], in_=cm_[:]), "mgt", reads=[rg], writes=[rg])
                self.op("dve", lambda e, blk=blk: e.tensor_tensor(out=v3(lf), in0=v3(b_), in1=Q[:, blk * CPB:(blk + 1) * CPB].unsqueeze(2).to_broadcast([4, CPB, 128]), op=ALU.add),
                        reads=[rg], writes=[rg])
                self.op("act", lambda e: e.activation(out=lf[:], in_=lf[:], func=AF.Exp), reads=[rg], writes=[rg])
                self.dma("sp", lambda e, t0=t0: e.dma_start(out=self.MROW[8:12, t0:t0 + TB], in_=lf[:]), "mgt", reads=[rg], writes=[rg])
        with self.phase():
            gain = self.tsb("mgain", [128, 256], F32)
            SCB = self.tsb("mSCB", [64, 4, NT], F32)
            rk = self.R("mconst")
            self.dma("sp", lambda e: e.dma_start(out=gain[:], in_=self.w_mix_norm[l:l + 1, 768:1024].partition_broadcast(128)), "mk", writes=[rk])
            for hd in range(4):
                self.dma("sp", lambda e, hd=hd: e.dma_start(out=SCB[:, hd, :], in_=self.MSCL[hd:hd + 1, :].partition_broadcast(64)), "mk", writes=[rk])
            self.op("act", lambda e: e.activation(out=SCB[:], in_=SCB[:], func=AF.Exp), reads=[rk], writes=[rk])
            NBLK = S // 512
            G2 = 2
            mqt4 = [self.tsb("mmqt4", [64, 4, 512], BF16) for _ in range(2)]
            mkt4 = [self.tsb("mmkt4", [64, 4, 512], BF16) for _ in range(2)]
            rows12 = [self.tsb("mrows", [12, 512], F32) for _ in range(2)]
            nmm4 = [self.tsb("mnmm4", [4, 512], F32) for _ in range(2)]
            li4 = [self.tsb("mli4", [4, 512], F32) for _ in range(2)]
            rblk = self.RL("mblk", 2)
            mk = [self.tsb("mmk", [128, 256], BF16) for _ in range(G2)]
            mo = [self.tsb("mmo", [128, 256], BF16) for _ in range(G2)]
            VAUG = [self.tsb("mVAUG", [128, 4, 65], BF16) for _ in range(G2)]
            rin = self.RL("min", G2)
            for i in range(G2):
                self.op("pool", lambda e, i=i: e.memset(VAUG[i][:, :, 64:65], 1.0), writes=[rin[i]])
            TS = [self.tsb("mTS", [128, 12], F32) for _ in range(G2)]
            rTS = self.RL("mTS", G2)
            vw = [self.tsb("mvw", [128, 4, 65], BF16) for _ in range(G2)]
            rvw = self.RL("mvw", G2)
            qk = [self.tps("mqk", [128, 128], F32) for _ in range(2)]
            rqk = self.RLP("mqk", 2)
            Rp = [self.tps("mRp", [128, 128], F32) for _ in range(2)]
            rRp = self.RLP("mRp", 2)
            RI = self.tps("mRI", [64, 128], F32)
            rRI = self.RP("mRI")
            NR = 8
            seg = [self.tsb("mseg", [128, 128], F32) for _ in range(NR)]
            rseg = self.RL("mseg", NR)
            Wt = [self.tsb("mWt", [128, 128], BF16) for _ in range(NR)]
            rWt = self.RL("mWt", NR)
            IR = [self.tsb("mIR", [64, 128], F32) for _ in range(NR)]
            rIR = self.RL("mIR", NR)
            qp = [self.tsb("mqp", [64, 128], BF16) for _ in range(NR)]
            rqp = self.RL("mqp", NR)
            nps = [self.tps("mnps", [128, 4, 65], F32) for _ in range(G2)]
            rnps = self.RLP("mnps", G2)
            cps = self.tps("mcps", [128, 512], F32)
            rcps = self.RP("mcps")
            cpsv = cps[0:64, 0:260].rearrange("p (h d) -> p h d", d=65)
            Cf = self.tsb("mCf", [64, 4, 65], F32)
            rCf = self.R("mCf")
            CTb = [self.tsb("mCTb", [64, 4, 65], BF16) for _ in range(3)]
            rCTb = self.RL("mCTb", 3)
            dd = [self.tsb("mdd", [128, 4], F32) for _ in range(G2)]
            hn = [self.tsb("mhn", [128, 4, 64], F32) for _ in range(G2)]
            hn2 = [self.tsb("mhn2", [128, 4, 64], F32) for _ in range(G2)]
            htmp = [self.tsb("mhtmp", [128, 4, 64], F32) for _ in range(G2)]
            hss = [self.tsb("mhss", [128, 4], F32) for _ in range(G2)]
            rhn = self.RL("mhn", G2)
            yb = [self.tsb("myb", [128, 256], BF16) for _ in range(G2)]
            ryb = self.RL("myb", G2)
            self.op("pool", lambda e: e.memset(Cf[:], 0.0), writes=[rCf])
            self.op("pool", lambda e: e.memset(CTb[0][:], 0.0), writes=[rCTb[0]])
            cnt = [0]

            def load_blk(b):
                bi = b % 2
                t0 = b * 512
                self.dma("sp", lambda e: e.dma_start(out=mqt4[bi][:], in_=self.MQT[:, t0:t0 + 512].rearrange("(h d) t -> d h t", d=64)), f"mblk{bi}", writes=[rblk[bi]])
                self.dma("sp", lambda e: e.dma_start(out=mkt4[bi][:], in_=self.MKT[:, t0:t0 + 512].rearrange("(h d) t -> d h t", d=64)), f"mblk{bi}", writes=[rblk[bi]])
                self.dma("sp", lambda e: e.dma_start(out=rows12[bi][:], in_=self.MROW[0:12, t0:t0 + 512]), f"mblk{bi}", writes=[rblk[bi]])
                self.dma("sp", lambda e: e.dma_start(out=nmm4[bi][:], in_=self.MROW[12:16, t0:t0 + 512]), f"mblk{bi}", writes=[rblk[bi]])
                self.dma("sp", lambda e: e.dma_start(out=li4[bi][:], in_=self.MROW[16:20, t0:t0 + 512]), f"mblk{bi}", writes=[rblk[bi]])

            def chunk(c):
                i = c % G2
                b = c // 4
                bi = b % 2
                o = (c % 4) * 128
                t0 = c * 128
                if c % 4 == 1 and b + 1 < NBLK:
                    load_blk(b + 1)
                self.dma("sp", lambda e: e.dma_start(out=mk[i][:], in_=self.MK[t0:t0 + 128, :]), f"min{i}", writes=[rin[i]])
                self.dma("sp", lambda e: e.dma_start(out=mo[i][:], in_=self.MO[t0:t0 + 128, :]), f"min{i}", writes=[rin[i]])
                self.dma("sp", lambda e: e.dma_start(out=VAUG[i][:, :, 0:64], in_=self.MV[t0:t0 + 128, :].rearrange("t (h d) -> t h d", d=64)), f"min{i}", writes=[rin[i]])
                yield
                self.op("pe", lambda e: e.transpose(out=cps[:, 384:396], in_=rows12[bi][0:12, o:o + 128], identity=self.ident_f[0:12, 0:12]), reads=[rblk[bi], self.rC], writes=[rcps])
                self.op("act", lambda e: e.copy(out=TS[i][:], in_=cps[:, 384:396]), reads=[rcps], writes=[rTS[i]])
                yield
                self.op("pool", lambda e: e.tensor_tensor(out=vw[i][:], in0=VAUG[i][:], in1=TS[i][:, 8:12].unsqueeze(2).to_broadcast([128, 4, 65]), op=ALU.mult),
                        reads=[rin[i], rTS[i]], writes=[rvw[i]])
                yield
                for hd in range(4):
                    self.op("pe", lambda e, hd=hd: e.matmul(cpsv[:, hd, :], lhsT=mk[i][:, hd * 64:(hd + 1) * 64], rhs=vw[i][:, hd, :], start=True, stop=True),
                            reads=[rin[i], rvw[i]], writes=[rcps])
                for hd in range(4):
                    self.op("dve", lambda e, hd=hd: e.scalar_tensor_tensor(out=Cf[:, hd, :], in0=Cf[:, hd, :], scalar=SCB[:, hd, c:c + 1], in1=cpsv[:, hd, :], op0=ALU.mult, op1=ALU.add),
                            reads=[rcps, rk], writes=[rCf])
                self.op("pool", lambda e: e.tensor_copy(out=CTb[(c + 1) % 3][:], in_=Cf[:]), reads=[rCf], writes=[rCTb[(c + 1) % 3]])
                yield
                ks = []
                for hd in range(4):
                    k2 = cnt[0] % 2
                    k = cnt[0] % NR
                    cnt[0] += 1
                    ks.append((k2, k))
                    self.op("pe", lambda e, hd=hd, k2=k2: e.matmul(Rp[k2][:], lhsT=self.sel[:, hd, :], rhs=nmm4[bi][:, o:o + 128], start=True, stop=True), reads=[rblk[bi], self.rC], writes=[rRp[k2]])
                    self.op("dve", lambda e, hd=hd, k2=k2, k=k: e.scalar_tensor_tensor(out=seg[k][:], in0=Rp[k2][:], scalar=TS[i][:, 4 + hd:5 + hd], in1=self.maskb[:], op0=ALU.add, op1=ALU.add),
                            reads=[rRp[k2], rTS[i], self.rC], writes=[rseg[k]])
                    self.op("pe", lambda e, hd=hd: e.matmul(RI[:], lhsT=self.sel[:, hd, 0:64], rhs=li4[bi][:, o:o + 128], start=True, stop=True), reads=[rblk[bi], self.rC], writes=[rRI])
                    self.op("act", lambda e, k=k: e.activation(out=IR[k][:], in_=RI[:], func=AF.Exp), reads=[rRI], writes=[rIR[k]])
                yield
                for hd in range(4):
                    k2, k = ks[hd]
                    self.op("act", lambda e, k=k: e.activation(out=seg[k][:], in_=seg[k][:], func=AF.Exp), reads=[rseg[k]], writes=[rseg[k]])
                    self.op("pool", lambda e, hd=hd, k=k: e.tensor_tensor(out=qp[k][:], in0=mqt4[bi][:, hd, o:o + 128], in1=IR[k][:], op=ALU.mult), reads=[rblk[bi], rIR[k]], writes=[rqp[k]])
                yield
                for hd in range(4):
                    k2, k = ks[hd]
                    self.op("pe", lambda e, hd=hd, k2=k2: e.matmul(qk[k2][:], lhsT=mkt4[bi][:, hd, o:o + 128], rhs=mqt4[bi][:, hd, o:o + 128], start=True, stop=True), reads=[rblk[bi]], writes=[rqk[k2]])
                    self.op("dve", lambda e, k2=k2, k=k: e.tensor_tensor(out=Wt[k][:], in0=qk[k2][:], in1=seg[k][:], op=ALU.mult), reads=[rqk[k2], rseg[k]], writes=[rWt[k]])
                yield
                for hd in range(4):
                    k2, k = ks[hd]
                    self.op("pe", lambda e, hd=hd, k=k: e.matmul(nps[i][:, hd, :], lhsT=Wt[k][:], rhs=VAUG[i][:, hd, :], start=True, stop=False), reads=[rWt[k], rin[i]], writes=[rnps[i]])
                    self.op("pe", lambda e, hd=hd, k=k: e.matmul(nps[i][:, hd, :], lhsT=qp[k][:], rhs=CTb[c % 3][:, hd, :], start=False, stop=True), reads=[rqp[k], rCTb[c % 3]], writes=[rnps[i]])
                yield
                self.op("dve", lambda e: e.tensor_copy(out=hss[i][:], in_=nps[i][:, :, 64]), reads=[rnps[i]], writes=[rhn[i]])
                self.op("dve", lambda e: e.scalar_tensor_tensor(out=dd[i][:], in0=hss[i][:], scalar=-1.0, in1=hss[i][:], op0=ALU.mult, op1=ALU.max), reads=[rhn[i]], writes=[rhn[i]])
                yield
                self.op("dve", lambda e: e.tensor_tensor(out=dd[i][:], in0=dd[i][:], in1=TS[i][:, 0:4], op=ALU.max), reads=[rTS[i], rhn[i]], writes=[rhn[i]])
                self.op("dve", lambda e: e.reciprocal(out=dd[i][:], in_=dd[i][:]), reads=[rhn[i]], writes=[rhn[i]])
                yield
                self.op("dve", lambda e: e.tensor_tensor(out=hn[i][:], in0=nps[i][:, :, 0:64], in1=dd[i][:].unsqueeze(2).to_broadcast([128, 4, 64]), op=ALU.mult), reads=[rnps[i], rhn[i]], writes=[rhn[i]])
                yield
                self.head_rms("pool", hn[i][:], 4, gain[:].rearrange("p (h d) -> p h d", d=64), hn2[i][:], htmp[i][:], hss[i][:], reads=[rhn[i], rk], writes=[rhn[i]])
                yield
                self.op("pool", lambda e: e.tensor_tensor(out=yb[i][:].rearrange("p (h d) -> p h d", d=64), in0=hn2[i][:], in1=mo[i][:].rearrange("p (h d) -> p h d", d=64), op=ALU.mult),
                        reads=[rhn[i], rin[i]], writes=[ryb[i]])
                self.dma("sp", lambda e: e.dma_start(out=self.Y[t0:t0 + 128, 768:1024], in_=yb[i][:]), f"myb{i}", reads=[ryb[i]])
                yield

            load_blk(0)
            self.run_window(chunk, NT, G=PIPE_G, skew=8)

    def phase_c(self, l):
        S, NT = self.S, self.NT
        B, NB, LB = self.MB, self.NB, self.LB
        BIG = 1.0e30
        with self.phase():
            WO = self.tsb("cWO", [128, 8, D], BF16)
            wst = [self.tsb("cwst", [128, 8, 256], F32) for _ in range(2)]
            rwst = self.RL("cwst", 2)
            rW = self.R("cW")
            for pi in range(4):
                i = pi % 2
                self.dma("sp", lambda e, pi=pi, i=i: e.dma_start(out=wst[i][:], in_=self.w_out[l, :, pi * 256:(pi + 1) * 256].rearrange("(k p) n -> p k n", p=128)), f"cwst{i}", writes=[rwst[i]])
                self.op("dve" if i == 0 else "pool", lambda e, pi=pi, i=i: e.tensor_copy(out=WO[:, :, pi * 256:(pi + 1) * 256], in_=wst[i][:]), reads=[rwst[i]], writes=[rW])
            WR = self.tsb("cWR", [128, 8, 36], F32)
            RB = self.tsb("cRB", [128, 36], F32)
            self.dma("sp", lambda e: e.dma_start(out=WR[:, :, 0:4], in_=self.w_rg[l, :, :].rearrange("(k p) n -> p k n", p=128), allow_slow_non_contiguous=True), "cw", writes=[rW])
            self.dma("sp", lambda e: e.dma_start(out=WR[:, :, 4:36], in_=self.w_re[l, :, :].rearrange("(k p) n -> p k n", p=128), allow_slow_non_contiguous=True), "cw", writes=[rW])
            self.dma("sp", lambda e: e.dma_start(out=RB[:, 0:4], in_=self.b_rg[l:l + 1, :].partition_broadcast(128)), "cw", writes=[rW])
            self.dma("sp", lambda e: e.dma_start(out=RB[:, 4:36], in_=self.b_re[l:l + 1, :].partition_broadcast(128)), "cw", writes=[rW])
            LG = self.tsb("cLG", [128, NT, 36], F32)
            rLG = self.R("cLG")
            yt = [self.tsb("cyt", [128, D], BF16) for _ in range(2)]
            xt = [self.tsb("cxt", [128, D], F32) for _ in range(2)]
            rin = self.RL("cin", 2)
            pT = [self.tps("cpT", [128, 4, 128], BF16) for _ in range(2)]
            rpT = self.RLP("cpT", 2)
            yT = [self.tsb("cyT", [128, 8, 128], BF16) for _ in range(2)]
            ryT = self.RL("cyT", 2)
            pm = [self.tps("cpm", [128, 512], F32) for _ in range(2)]
            rpm = self.RLP("cpm", 2)
            x1 = [self.tsb("cx1", [128, D], F32) for _ in range(2)]
            rx1 = self.RL("cx1", 2)
            ss = [self.tsb("css", [128, 1], F32) for _ in range(2)]
            h2f = [self.tsb("ch2f", [128, D], F32) for _ in range(2)]
            rh2f = self.RL("ch2f", 2)
            h2b = [self.tsb("ch2b", [128, D], BF16) for _ in range(2)]
            rh2b = self.RL("ch2b", 2)
            pTf = [self.tps("cpTf", [128, 4, 128], F32) for _ in range(2)]
            rpTf = self.RLP("cpTf", 2)

            pl = self.tps("cpl", [128, 36], F32)
            rpl = self.RP("cpl")
            rsq = self.R("csq")
            h2T2 = [self.tsb("ch2T2", [128, 8, 128], F32) for _ in range(2)]
            rh2T2 = self.RL("ch2T2", 2)
            sq2 = [self.tsb("csq2", [128, D], BF16) for _ in range(2)]
            rsq2 = self.RL("csq2", 2)

            def load_c(ti):
                i = ti % 2
                t0 = ti * 128
                self.dma("sp", lambda e: e.dma_start(out=yt[i][:], in_=self.Y[t0:t0 + 128, :]), f"cin{i}", writes=[rin[i]])
                self.dma("sp", lambda e: e.dma_start(out=xt[i][:], in_=self.X[t0:t0 + 128, :]), f"cin{i}", writes=[rin[i]])

            def tile_c(ti):
                i = ti % 2
                t0 = ti * 128
                load_c(ti)
                yield
                for half in range(2):
                    for j in range(4):
                        kc = half * 4 + j
                        self.op("pe", lambda e, half=half, j=j, kc=kc: e.transpose(out=pT[half][:, j, :], in_=yt[i][:, kc * 128:(kc + 1) * 128], identity=self.ident_b[:]),
                                reads=[rin[i], self.rC], writes=[rpT[half]])
                    if half == 0:
                        self.op("act", lambda e: e.copy(out=yT[i][:, 0:4, :], in_=pT[0][:]), reads=[rpT[0]], writes=[ryT[i]])
                    else:
                        self.op("dve", lambda e: e.tensor_copy(out=yT[i][:, 4:8, :], in_=pT[1][:]), reads=[rpT[1]], writes=[ryT[i]])
                    yield
                for half in range(2):
                    for kc in range(8):
                        self.op("pe", lambda e, half=half, kc=kc: e.matmul(pm[half][:], lhsT=yT[i][:, kc, :], rhs=WO[:, kc, half * 512:(half + 1) * 512], start=(kc == 0), stop=(kc == 7)),
                                reads=[ryT[i], rW], writes=[rpm[half]])
                    self.op("dve", lambda e, half=half: e.tensor_tensor(out=x1[i][:, half * 512:(half + 1) * 512], in0=pm[half][:], in1=self.MOD[:, 2 * D + half * 512:2 * D + (half + 1) * 512], op=ALU.mult),
                            reads=[rpm[half], self.rMOD], writes=[rx1[i]])
                    yield
                self.op("pool", lambda e: e.tensor_tensor(out=x1[i][:], in0=x1[i][:], in1=xt[i][:], op=ALU.add), reads=[rin[i], rx1[i]], writes=[rx1[i]])
                self.dma("sp", lambda e: e.dma_start(out=self.X[t0:t0 + 128, :], in_=x1[i][:]), f"cx1{i}", reads=[rx1[i]])
                yield
                self.op("pool", lambda e: e.memset(ss[i][:], 0.0), writes=[rsq2[i]])
                self.op("act", lambda e: e.activation(out=sq2[i][:], in_=x1[i][:], func=AF.Square, accum_out=ss[i][:]), reads=[rx1[i]], writes=[rsq2[i]])
                yield
                self.op("act", lambda e: e.activation(out=ss[i][:], in_=ss[i][:], func=AF.Sqrt, bias=EPS, scale=1.0 / D), reads=[rsq2[i]], writes=[rsq2[i]])
                self.op("dve", lambda e: e.reciprocal(out=ss[i][:], in_=ss[i][:]), reads=[rsq2[i]], writes=[rsq2[i]])
                yield
                self.op("dve", lambda e: e.scalar_tensor_tensor(out=h2f[i][:], in0=x1[i][:], scalar=ss[i][:, 0:1], in1=self.MOD[:, 4 * D:5 * D], op0=ALU.mult, op1=ALU.mult),
                        reads=[rx1[i], rsq2[i], self.rMOD], writes=[rh2f[i]])
                yield
                self.op("pool", lambda e: e.tensor_tensor(out=h2f[i][:], in0=h2f[i][:], in1=self.MOD[:, 3 * D:4 * D], op=ALU.add), reads=[rh2f[i], self.rMOD], writes=[rh2f[i]])
                yield
                self.op("act", lambda e: e.copy(out=h2b[i][:], in_=h2f[i][:]), reads=[rh2f[i]], writes=[rh2b[i]])
                self.dma("sp", lambda e: e.dma_start(out=self.HS[t0:t0 + 128, :], in_=h2b[i][:]), f"ch2b{i}", reads=[rh2b[i]])
                yield
                for half in range(2):
                    for j in range(4):
                        kc = half * 4 + j
                        self.op("pe", lambda e, half=half, j=j, kc=kc: e.transpose(out=pTf[half][:, j, :], in_=h2f[i][:, kc * 128:(kc + 1) * 128], identity=self.ident_f[:]),
                                reads=[rh2f[i], self.rC], writes=[rpTf[half]])
                    if half == 0:
                        self.op("act", lambda e: e.copy(out=h2T2[i][:, 0:4, :], in_=pTf[0][:]), reads=[rpTf[0]], writes=[rh2T2[i]])
                    else:
                        self.op("dve", lambda e: e.tensor_copy(out=h2T2[i][:, 4:8, :], in_=pTf[1][:]), reads=[rpTf[1]], writes=[rh2T2[i]])
                    yield
                for kc in range(8):
                    self.op("pe", lambda e, kc=kc: e.matmul(pl[:], lhsT=h2T2[i][:, kc, :], rhs=WR[:, kc, :], start=(kc == 0), stop=(kc == 7)), reads=[rh2T2[i], rW], writes=[rpl])
                self.op("dve", lambda e: e.tensor_tensor(out=LG[:, ti, :], in0=pl[:], in1=RB[:], op=ALU.add), reads=[rpl, rW], writes=[rLG])
                yield

            self.run_window(tile_c, NT, G=PIPE_G, skew=7)
            r = rLG
            gmax = self.tsb("rgmax", [128, NT], F32)
            goh = self.tsb("rgoh", [128, NT, 4], F32)
            ge = self.tsb("rge", [128, NT, 4], F32)
            gpr = self.tsb("rgpr", [128, NT], F32)
            em = self.tsb("rem", [128, NT, 32], F32)
            OH1 = self.tsb("rOH1", [128, NT, 32], F32)
            OH2 = self.tsb("rOH2", [128, NT, 32], F32)
            t1 = self.tsb("rt1", [128, NT], F32)
            t2 = self.tsb("rt2", [128, NT], F32)
            GL = LG[:, :, 0:4]
            self.op("dve", lambda e: e.tensor_reduce(out=gmax[:], in_=GL, axis=AX.X, op=ALU.max), reads=[r], writes=[r])
            self.op("dve", lambda e: e.tensor_tensor(out=goh[:], in0=GL, in1=gmax[:].unsqueeze(2).to_broadcast([128, NT, 4]), op=ALU.is_equal), reads=[r], writes=[r])
            self.op("dve", lambda e: e.tensor_tensor(out=ge[:], in0=GL, in1=gmax[:].unsqueeze(2).to_broadcast([128, NT, 4]), op=ALU.subtract), reads=[r], writes=[r])
            self.op("act", lambda e: e.activation(out=ge[:], in_=ge[:], func=AF.Exp), reads=[r], writes=[r])
            self.op("dve", lambda e: e.tensor_reduce(out=gpr[:], in_=ge[:], axis=AX.X, op=ALU.add), reads=[r], writes=[r])
            self.op("dve", lambda e: e.reciprocal(out=gpr[:], in_=gpr[:]), reads=[r], writes=[r])
            self.op("dve", lambda e: e.tensor_scalar(out=goh[:], in0=goh[:], scalar1=BIG, scalar2=-BIG, op0=ALU.mult, op1=ALU.add), reads=[r], writes=[r])
            self.op("dve", lambda e: e.tensor_tensor(out=em[:].rearrange("p n (g j) -> p n g j", j=8), in0=LG[:, :, 4:36].rearrange("p n (g j) -> p n g j", j=8),
                                                     in1=goh[:].unsqueeze(3).to_broadcast([128, NT, 4, 8]), op=ALU.add), reads=[r], writes=[r])
            self.op("dve", lambda e: e.tensor_reduce(out=t1[:], in_=em[:], axis=AX.X, op=ALU.max), reads=[r], writes=[r])
            self.op("dve", lambda e: e.tensor_tensor(out=OH1[:], in0=em[:], in1=t1[:].unsqueeze(2).to_broadcast([128, NT, 32]), op=ALU.is_equal), reads=[r], writes=[r])
            self.op("dve", lambda e: e.scalar_tensor_tensor(out=em[:], in0=OH1[:], scalar=-BIG, in1=em[:], op0=ALU.mult, op1=ALU.add), reads=[r], writes=[r])
            self.op("dve", lambda e: e.tensor_reduce(out=t2[:], in_=em[:], axis=AX.X, op=ALU.max), reads=[r], writes=[r])
            self.op("dve", lambda e: e.tensor_tensor(out=OH2[:], in0=em[:], in1=t2[:].unsqueeze(2).to_broadcast([128, NT, 32]), op=ALU.is_equal), reads=[r], writes=[r])
            self.op("dve", lambda e: e.tensor_tensor(out=t2[:], in0=t2[:], in1=t1[:], op=ALU.subtract), reads=[r], writes=[r])
            self.op("act", lambda e: e.activation(out=t2[:], in_=t2[:], func=AF.Exp), reads=[r], writes=[r])
            self.op("dve", lambda e: e.tensor_scalar_add(out=t1[:], in0=t2[:], scalar1=1.0), reads=[r], writes=[r])
            self.op("dve", lambda e: e.reciprocal(out=t1[:], in_=t1[:]), reads=[r], writes=[r])
            self.op("dve", lambda e: e.tensor_tensor(out=t2[:], in0=t2[:], in1=t1[:], op=ALU.mult), reads=[r], writes=[r])
            self.op("dve", lambda e: e.tensor_tensor(out=self.GATES[:, :, 0], in0=t1[:], in1=gpr[:], op=ALU.mult), reads=[r], writes=[self.rPLAN])
            self.op("dve", lambda e: e.tensor_tensor(out=self.GATES[:, :, 1], in0=t2[:], in1=gpr[:], op=ALU.mult), reads=[r], writes=[self.rPLAN])
            SEL = self.tsb("rSEL", [128, NT * 32], BF16)
            RANK = self.tsb("rRANK", [128, NT, 32], F32)
            PRE = self.tsb("rPRE", [128, NT + 1, 32], F32)
            pr = pm
            rpr = rpm
            self.op("dve", lambda e: e.tensor_tensor(out=SEL[:], in0=OH1[:].rearrange("p n e -> p (n e)"), in1=OH2[:].rearrange("p n e -> p (n e)"), op=ALU.add), reads=[r], writes=[r])
            TOTt = em
            W_ = NT * 32
            for c0 in range(0, W_, 512):
                w = min(512, W_ - c0)
                self.op("pe", lambda e, c0=c0, w=w: e.matmul(pr[0][:, 0:w], lhsT=self.UTs[:], rhs=SEL[:, c0:c0 + w], start=True, stop=True), reads=[r, self.rC], writes=[rpr[0]])
                self.op("dve", lambda e, c0=c0, w=w: e.tensor_copy(out=RANK[:].rearrange("p n e -> p (n e)")[:, c0:c0 + w], in_=pr[0][:, 0:w]), reads=[rpr[0]], writes=[r])
                self.op("pe", lambda e, c0=c0, w=w: e.matmul(pr[1][:, 0:w], lhsT=self.ones_b[:], rhs=SEL[:, c0:c0 + w], start=True, stop=True), reads=[r, self.rC], writes=[rpr[1]])
                self.op("dve", lambda e, c0=c0, w=w: e.tensor_copy(out=TOTt[:].rearrange("p n e -> p (n e)")[:, c0:c0 + w], in_=pr[1][:, 0:w]), reads=[rpr[1]], writes=[r])
            self.op("pool", lambda e: e.memset(PRE[:, 0, :], 0.0), writes=[r])
            for n in range(NT):
                self.op("dve", lambda e, n=n: e.tensor_tensor(out=PRE[:, n + 1, :], in0=PRE[:, n, :], in1=TOTt[:, n, :], op=ALU.add), reads=[r], writes=[r])
            PADf = self.tsb("rPADf", [128, 32], F32)
            PADi = self.tsb("rPADi", [128, 32], I32)
            PEND = self.tsb("rPEND", [128, 32], F32)
            PST = self.tsb("rPST", [128, 32], F32)
            self.op("dve", lambda e: e.tensor_scalar_add(out=PADf[:], in0=PRE[:, NT, :], scalar1=float(B - 1)), reads=[r], writes=[r])
            self.op("dve", lambda e: e.tensor_copy(out=PADi[:], in_=PADf[:]), reads=[r], writes=[r])
            self.op("dve", lambda e: e.tensor_single_scalar(out=PADi[:], in_=PADi[:], scalar=LB, op=ALU.arith_shift_right), reads=[r], writes=[r])
            self.op("dve", lambda e: e.tensor_single_scalar(out=PADi[:], in_=PADi[:], scalar=LB, op=ALU.logical_shift_left), reads=[r], writes=[r])
            self.op("dve", lambda e: e.tensor_copy(out=PADf[:], in_=PADi[:]), reads=[r], writes=[r])
            self.op("dve", lambda e: e.tensor_tensor_scan(out=PEND[:], data0=self.ones_f[:, 0:32], data1=PADf[:], initial=0.0, op0=ALU.mult, op1=ALU.add), reads=[r, self.rC], writes=[r])
            self.op("dve", lambda e: e.tensor_tensor(out=PST[:], in0=PEND[:], in1=PADf[:], op=ALU.subtract), reads=[r], writes=[r])
            self.op("dve", lambda e: e.tensor_tensor(out=RANK[:], in0=RANK[:], in1=PRE[:, 0:NT, :], op=ALU.add), reads=[r], writes=[r])
            self.op("dve", lambda e: e.tensor_tensor(out=RANK[:], in0=RANK[:], in1=PST[:].unsqueeze(1).to_broadcast([128, NT, 32]), op=ALU.add), reads=[r], writes=[r])
            dtmp = self.tsb("rdtmp", [128, NT], F32)
            for k, OH in enumerate((OH1, OH2)):
                self.op("dve", lambda e, OH=OH: e.tensor_tensor(out=em[:], in0=OH[:], in1=RANK[:], op=ALU.mult), reads=[r], writes=[r])
                self.op("dve", lambda e: e.tensor_reduce(out=dtmp[:], in_=em[:], axis=AX.X, op=ALU.add), reads=[r], writes=[r])
                self.op("dve", lambda e, k=k: e.tensor_copy(out=self.DEST[:, :, k], in_=dtmp[:]), reads=[r], writes=[self.rPLAN])
            JBi = self.tsb("rJBi", [128, NB, 32], I32)
            JBf = self.tsb("rJBf", [128, NB, 32], F32)
            EJ = self.tsb("rEJ", [128, NB], F32)
            PKi = self.tsb("rPKi", [128, 8], I32)
            PKf = self.tsb("rPKf", [128, 8], F32)
            IXf = self.tsb("rIXf", [128, NB, 8], F32)
            self.op("pool", lambda e: e.iota(JBi[:], pattern=[[B, NB], [0, 32]], base=0, channel_multiplier=0), writes=[r])
            self.op("dve", lambda e: e.tensor_copy(out=JBf[:], in_=JBi[:]), reads=[r], writes=[r])
            self.op("dve", lambda e: e.tensor_tensor(out=JBf[:], in0=PEND[:].unsqueeze(1).to_broadcast([128, NB, 32]), in1=JBf[:], op=ALU.is_le), reads=[r], writes=[r])
            self.op("dve", lambda e: e.tensor_reduce(out=EJ[:], in_=JBf[:], axis=AX.X, op=ALU.add), reads=[r], writes=[r])
            self.op("dve", lambda e: e.tensor_scalar_min(out=EJ[:], in0=EJ[:], scalar1=31.0), reads=[r], writes=[r])
            self.op("pool", lambda e: e.iota(PKi[:], pattern=[[128, 8]], base=0, channel_multiplier=1), writes=[r])
            self.op("dve", lambda e: e.tensor_copy(out=PKf[:], in_=PKi[:]), reads=[r], writes=[r])
            self.op("dve", lambda e: e.scalar_tensor_tensor(out=IXf[:, :, 0], in0=EJ[:], scalar=128.0, in1=PKf[:, 0:1].to_broadcast([128, NB]), op0=ALU.mult, op1=ALU.add),
                    reads=[r], writes=[r])
            if l > 0:
                self.op("dve", lambda e: e.tensor_scalar_add(out=IXf[:, :, 0], in0=IXf[:, :, 0], scalar1=float(l * 4096)), reads=[r], writes=[r])
            self.op("dve", lambda e: e.tensor_copy(out=self.IDXW[:], in_=IXf[:, :, 0]), reads=[r], writes=[self.rPLAN])
            hs = [self.tsb("rhs", [128, D], BF16) for _ in range(2)]
            rhs_ = self.RL("rhs", 2)
            for ti in range(NT):
                i = ti % 2
                t0 = ti * 128
                self.dma("sp", lambda e, i=i, t0=t0: e.dma_start(out=hs[i][:], in_=self.HS[t0:t0 + 128, :]), f"rhs{i}", writes=[rhs_[i]])
                for k in range(2):
                    self.dma("pool", lambda e, i=i, ti=ti, k=k: e.indirect_dma_start(out=self.XS, out_offset=bass.IndirectOffsetOnAxis(ap=self.DEST[:, ti, k:k + 1], axis=0),
                                                                                      in_=hs[i][:], in_offset=None), f"rsc{i}", reads=[rhs_[i], self.rPLAN])

    def phase_moe(self, l):
        B, NB = self.MB, self.NB
        RT = B // 128
        with self.phase():
            WG = self.tsb("eWG", [128, 8, 512], F32)
            WU = self.tsb("eWU", [128, 8, 512], F32)
            WD = self.tsb("eWD", [128, 4, D], F32)
            rWf = self.RL("eWf", 3)
            WGb = [self.tsb("eWGb", [128, 8, 512], BF16) for _ in range(2)]
            WUb = [self.tsb("eWUb", [128, 8, 512], BF16) for _ in range(2)]
            WDb = [self.tsb("eWDb", [128, 4, D], BF16) for _ in range(2)]
            rWb = [self.RL("eWb", 3) for _ in range(2)]
            xs = [self.tsb("exs", [128, D], BF16) for _ in range(2)]
            rxs = self.RL("exs", 2)
            pT = [self.tps("epT", [128, 4, 128], BF16) for _ in range(2)]
            rpT = self.RLP("epT", 2)
            xT = self.tsb("exT", [128, 8, B], BF16)
            rxT = self.R("exT")
            pg = self.tps("epg", [128, 512], F32)
            pu = self.tps("epu", [128, 512], F32)
            rpg, rpu = self.RP("epg"), self.R("epu")
            sg = self.tsb("esg", [128, B], F32)
            rsg = self.R("esg")
            AT = self.tsb("eAT", [128, 4, B], BF16)
            rAT = self.R("eAT")
            po = [self.tps("epo", [128, 512], F32) for _ in range(2)]
            rpo = self.RLP("epo", 2)
            ob = [self.tsb("eob", [128, D], F32) for _ in range(2)]
            rob = self.RL("eob", 2)
            nx = 0
            no = 0
            xs = [self.tsb("exs2", [128, D], BF16) for _ in range(2 * RT)]
            rxs = self.RL("exs2", 2 * RT)

            def gather_w(j):
                for qi, (Wt_, src_) in enumerate(((WG, self.w_eg), (WU, self.w_eu), (WD, self.w_ed))):
                    self.dma("pool", lambda e, Wt_=Wt_, src_=src_: e.indirect_dma_start(out=Wt_[:].rearrange("p k n -> p (k n)"), out_offset=None, in_=src_,
                                                                                      in_offset=bass.IndirectOffsetOnAxis(ap=self.IDXW[:, j:j + 1], axis=0)),
                             f"eWf{qi}", reads=[self.rPLAN], writes=[rWf[qi]])

            def load_xs(j):
                for rt in range(RT):
                    i = (j % 2) * RT + rt
                    r0 = j * B + rt * 128
                    self.dma("sp", lambda e, i=i, r0=r0: e.dma_start(out=xs[i][:], in_=self.XS[r0:r0 + 128, :]), f"exs{i}", writes=[rxs[i]])

            def cast_w(j):
                w = j % 2
                self.op("dve", lambda e: e.tensor_copy(out=WGb[w][:], in_=WG[:]), reads=[rWf[0]], writes=[rWb[w][0]])
                self.op("act", lambda e: e.copy(out=WUb[w][:], in_=WU[:]), reads=[rWf[1]], writes=[rWb[w][1]])
                self.op("dve", lambda e: e.tensor_copy(out=WDb[w][:], in_=WD[:]), reads=[rWf[2]], writes=[rWb[w][2]])

            gather_w(0)
            load_xs(0)
            cast_w(0)
            if NB > 1:
                gather_w(1)
                load_xs(1)
            for j in range(NB):
                w = j % 2
                for rt in range(RT):
                    i = (j % 2) * RT + rt
                    for half in range(2):
                        for q in range(4):
                            kc = half * 4 + q
                            self.op("pe", lambda e, half=half, q=q, kc=kc, i=i: e.transpose(out=pT[half][:, q, :], in_=xs[i][:, kc * 128:(kc + 1) * 128], identity=self.ident_b[:]),
                                    reads=[rxs[i], self.rC], writes=[rpT[half]])
                        if half == 0:
                            self.op("act", lambda e, rt=rt: e.copy(out=xT[:, 0:4, rt * 128:(rt + 1) * 128], in_=pT[0][:]), reads=[rpT[0]], writes=[rxT])
                        else:
                            self.op("dve", lambda e, rt=rt: e.tensor_copy(out=xT[:, 4:8, rt * 128:(rt + 1) * 128], in_=pT[1][:]), reads=[rpT[1]], writes=[rxT])
                if j + 1 < NB:
                    cast_w(j + 1)
                if j + 2 < NB:
                    gather_w(j + 2)
                    load_xs(j + 2)
                for nch in range(4):
                    for kc in range(8):
                        self.op("pe", lambda e, nch=nch, kc=kc, w=w: e.matmul(pg[:, 0:B], lhsT=WGb[w][:, kc, nch * 128:(nch + 1) * 128], rhs=xT[:, kc, :], start=(kc == 0), stop=(kc == 7)),
                                reads=[rWb[w][0], rxT], writes=[rpg])
                    for kc in range(8):
                        self.op("pe", lambda e, nch=nch, kc=kc, w=w: e.matmul(pu[:, 0:B], lhsT=WUb[w][:, kc, nch * 128:(nch + 1) * 128], rhs=xT[:, kc, :], start=(kc == 0), stop=(kc == 7)),
                                reads=[rWb[w][1], rxT], writes=[rpu])
                    self.op("act", lambda e: e.activation(out=sg[:], in_=pg[:, 0:B], func=AF.Silu), reads=[rpg], writes=[rsg])
                    self.op("dve", lambda e, nch=nch: e.tensor_tensor(out=AT[:, nch, :], in0=pu[:, 0:B], in1=sg[:], op=ALU.mult), reads=[rpu, rsg], writes=[rAT])
                for rt in range(RT):
                    o = no % 2
                    no += 1
                    r0 = j * B + rt * 128
                    for half in range(2):
                        for nch in range(4):
                            self.op("pe", lambda e, half=half, nch=nch, rt=rt, w=w: e.matmul(po[half][:], lhsT=AT[:, nch, rt * 128:(rt + 1) * 128], rhs=WDb[w][:, nch, half * 512:(half + 1) * 512],
                                                                                               start=(nch == 0), stop=(nch == 3)), reads=[rAT, rWb[w][2]], writes=[rpo[half]])
                        if half == 0:
                            self.op("act", lambda e, o=o: e.copy(out=ob[o][:, 0:512], in_=po[0][:]), reads=[rpo[0]], writes=[rob[o]])
                        else:
                            self.op("dve", lambda e, o=o: e.tensor_copy(out=ob[o][:, 512:1024], in_=po[1][:]), reads=[rpo[1]], writes=[rob[o]])
                    self.dma("sp", lambda e, o=o, r0=r0: e.dma_start(out=self.YB[r0:r0 + 128, :], in_=ob[o][:]), f"eob{o}", reads=[rob[o]])

    def phase_comb(self, l, last):
        S, NT = self.S, self.NT
        with self.phase():
            r1 = [self.tsb("br1", [128, D], F32) for _ in range(2)]
            r2 = [self.tsb("br2", [128, D], F32) for _ in range(2)]
            xt = [self.tsb("bxt", [128, D], F32) for _ in range(2)]
            rin = self.RL("bin", 2)
            acc = [self.tsb("bacc", [128, D], F32) for _ in range(2)]
            racc = self.RL("bacc", 2)
            wf = self.tsb("bwf", [128, D], F32)
            ss = self.tsb("bss", [128, 1], F32)
            sq = self.tsb("bsq", [128, D], F32)
            rk = self.R("bk")
            if last:
                self.dma("sp", lambda e: e.dma_start(out=wf[:], in_=self.w_norm_final[0:1, :].partition_broadcast(128)), "bk", writes=[rk])
            def load_b(ti):
                i = ti % 2
                t0 = ti * 128
                self.dma("pool", lambda e: e.indirect_dma_start(out=r1[i][:], out_offset=None, in_=self.YB, in_offset=bass.IndirectOffsetOnAxis(ap=self.DEST[:, ti, 0:1], axis=0)),
                         f"bin{i}", reads=[self.rPLAN], writes=[rin[i]])
                self.dma("pool", lambda e: e.indirect_dma_start(out=r2[i][:], out_offset=None, in_=self.YB, in_offset=bass.IndirectOffsetOnAxis(ap=self.DEST[:, ti, 1:2], axis=0)),
                         f"bin{i}", reads=[self.rPLAN], writes=[rin[i]])
                self.dma("sp", lambda e: e.dma_start(out=xt[i][:], in_=self.X[t0:t0 + 128, :]), f"bin{i}", writes=[rin[i]])

            load_b(0)
            for ti in range(NT):
                i = ti % 2
                t0 = ti * 128
                if ti + 1 < NT:
                    load_b(ti + 1)
                self.op("dve", lambda e, i=i, ti=ti: e.tensor_scalar(out=acc[i][:], in0=r1[i][:], scalar1=self.GATES[:, ti, 0:1], scalar2=None, op0=ALU.mult), reads=[rin[i], self.rPLAN], writes=[racc[i]])
                self.op("dve", lambda e, i=i, ti=ti: e.scalar_tensor_tensor(out=acc[i][:], in0=r2[i][:], scalar=self.GATES[:, ti, 1:2], in1=acc[i][:], op0=ALU.mult, op1=ALU.add),
                        reads=[rin[i], self.rPLAN], writes=[racc[i]])
                self.op("dve", lambda e, i=i: e.tensor_tensor(out=acc[i][:], in0=acc[i][:], in1=self.MOD[:, 5 * D:6 * D], op=ALU.mult), reads=[self.rMOD], writes=[racc[i]])
                self.op("pool", lambda e, i=i: e.tensor_tensor(out=acc[i][:], in0=acc[i][:], in1=xt[i][:], op=ALU.add), reads=[rin[i]], writes=[racc[i]])
                if not last:
                    self.dma("sp", lambda e, i=i, t0=t0: e.dma_start(out=self.X[t0:t0 + 128, :], in_=acc[i][:]), f"bacc{i}", reads=[racc[i]])
                else:
                    if self.debug:
                        self.dma("sp", lambda e, i=i, t0=t0: e.dma_start(out=self.X[t0:t0 + 128, :], in_=acc[i][:]), f"bacc{i}", reads=[racc[i]])
                    self.op("pool", lambda e: e.memset(ss[:], 0.0), writes=[rk])
                    self.op("act", lambda e, i=i: e.activation(out=sq[:], in_=acc[i][:], func=AF.Square, accum_out=ss[:]), reads=[racc[i]], writes=[rk])
                    self.op("act", lambda e: e.activation(out=ss[:], in_=ss[:], func=AF.Sqrt, bias=EPS, scale=1.0 / D), reads=[rk], writes=[rk])
                    self.op("dve", lambda e: e.reciprocal(out=ss[:], in_=ss[:]), reads=[rk], writes=[rk])
                    self.op("dve", lambda e, i=i: e.scalar_tensor_tensor(out=acc[i][:], in0=acc[i][:], scalar=ss[:, 0:1], in1=wf[:], op0=ALU.mult, op1=ALU.mult), reads=[rk], writes=[racc[i]])
                    self.dma("sp", lambda e, i=i, t0=t0: e.dma_start(out=self.out[t0:t0 + 128, :], in_=acc[i][:]), f"bacc{i}", reads=[racc[i]])

    def build(self):
        self._alloc_es = self.es
        self.declare()
        self.consts()
        self.dma("sp", lambda e: e.dma_start(out=self.X, in_=self.x_in), "x0")
        self.barrier()
        ph = self.phases
        for l in range(self.L):
            self.adaln(l)
            if ph is None or "a" in ph:
                self.phase_a(l)
            if ph is None or "attn" in ph:
                self.phase_attn(l)
            if ph is None or "sg" in ph:
                self.phase_sg(l)
            if ph is None or "ssm" in ph:
                self.phase_ssd(l)
            if ph is None or "ml" in ph:
                self.phase_mlstm(l)
            if ph is None or "c" in ph:
                self.phase_c(l)
            if ph is None or "moe" in ph:
                self.phase_moe(l)
                self.phase_comb(l, last=(l == self.L - 1))
        if self.debug:
            self.dbgMOD = self.nc.dram_tensor("dbgMOD", [128, 6 * D], F32, kind="ExternalOutput").ap()
            self.dma("sp", lambda e: e.dma_start(out=self.dbgMOD, in_=self.MOD[:]), "dbg", reads=[self.rMOD])
        self.barrier()
        self.es.close()
        return self.nc


IN_NAMES = ["w_in", "w_out", "w_mix_norm", "attn_f_bias", "sg_w", "sg_b", "ssm_conv_w", "ssm_conv_b", "ssm_dt_bias",
            "ssm_a_log", "ssm_d", "mlstm_i_bias", "mlstm_f_bias", "w_ada", "b_ada", "w_norm1", "w_norm2",
            "w_router_group", "b_router_group", "w_router_expert", "b_router_expert", "w_expert_gate", "w_expert_up",
            "w_expert_down"]


def make_in_map(inputs, b, L):
    m = {"x": np.ascontiguousarray(inputs["x"][b]), "c": np.ascontiguousarray(inputs["c"][b:b + 1])}
    for k in IN_NAMES:
        if k in ("w_expert_gate", "w_expert_up"):
            m[k] = np.ascontiguousarray(inputs[k][:L].reshape(L, 32, 8, 128, 512).transpose(0, 1, 3, 2, 4)).reshape(L * 4096, 4096)
        elif k == "w_expert_down":
            m[k] = np.ascontiguousarray(inputs[k][:L].reshape(L, 32, 4, 128, 1024).transpose(0, 1, 3, 2, 4)).reshape(L * 4096, 4096)
        else:
            m[k] = np.ascontiguousarray(inputs[k][:L])
    m["w_norm_final"] = np.ascontiguousarray(inputs["w_norm_final"].reshape(1, D))
    return m


_CACHE = {}


def kernel(**inputs):
    S = inputs["x"].shape[1]
    L = inputs["w_in"].shape[0]
    nb = inputs["x"].shape[0]
    key = (S, L)
    if key not in _CACHE:
        _CACHE[key] = Builder(S, L).build()
    nc = _CACHE[key]
    inputs = {k: np.asarray(v, dtype=np.float32) for k, v in inputs.items()}
    m0 = make_in_map(inputs, 0, L)
    maps = []
    for b in range(nb):
        mb = dict(m0)
        mb["x"] = np.ascontiguousarray(inputs["x"][b])
        mb["c"] = np.ascontiguousarray(inputs["c"][b:b + 1])
        maps.append(mb)
    in_maps = [maps[c % nb] for c in range(NCORES)]
    res = run_bass_kernel_spmd(nc, in_maps, core_ids=list(range(NCORES)))
    out = np.stack([np.asarray(res.results[b]["out"], dtype=np.float32) for b in range(nb)], axis=0)
    return out
```

```python
import numpy as np
from contextlib import ExitStack
import concourse.bass as bass
import concourse.mybir as mybir
from concourse.bass_utils import run_bass_kernel_spmd

F32 = mybir.dt.float32
BF16 = mybir.dt.bfloat16
I32 = mybir.dt.int32
ALU = mybir.AluOpType
AF = mybir.ActivationFunctionType
AX = mybir.AxisListType

D = 1024
DEPTH = 4
NCORES = 8
EPS = 1e-6
NEG = -1.0e30
SAME_ENGINE_SYNC = True
PIPE_G = 2


class Res:
    __slots__ = ("name", "w", "r", "excl")

    def __init__(self, name=""):
        self.name = name
        self.w = None
        self.r = []
        self.excl = False


class Prog:
    ENG = ("pe", "act", "dve", "pool", "sp")

    def __init__(self, nc, es, same_engine_sync=True):
        self.nc = nc
        self.es = es
        self.same = same_engine_sync
        self.ops = {e: [] for e in self.ENG}
        self.sem = {e: es.enter_context(nc.semaphore("s_" + e)) for e in self.ENG}
        self.cnt = {e: 0 for e in self.ENG}
        self.dsem = {}
        self.waited = {e: {} for e in self.ENG}
        self.nres = 0

    def res(self, name=""):
        self.nres += 1
        return Res(name or f"r{self.nres}")

    def _deps(self, eng, reads, writes):
        deps = []
        for r in reads:
            if r.w is not None:
                deps.append(r.w)
            if r.excl:
                deps.extend(t for t in r.r if not (t[0] == "e" and t[1] == eng))
        for w in writes:
            if w.w is not None:
                deps.append(w.w)
            deps.extend(w.r)
        waits = {}
        for (kind, key, val) in deps:
            if kind == "e":
                if key == eng and (not self.same or eng == "pe"):
                    continue
                sem = self.sem[key]
            else:
                sem = self.dsem[key][0]
            k = id(sem)
            if self.waited[eng].get(k, 0) >= val:
                continue
            if k not in waits or waits[k][1] < val:
                waits[k] = (sem, val)
        for k, (sem, val) in waits.items():
            self.waited[eng][k] = val
        return list(waits.values())

    def _mark(self, token, reads, writes):
        for r in reads:
            r.r.append(token)
        for w in writes:
            w.w = token
            w.r = []

    def op(self, eng, fn, reads=(), writes=()):
        waits = self._deps(eng, reads, writes)
        self.cnt[eng] += 1
        token = ("e", eng, self.cnt[eng])
        self._mark(token, reads, writes)
        self._emit_now(eng, waits, fn, (self.sem[eng], 1))

    def dma(self, eng, fn, key, reads=(), writes=()):
        if key not in self.dsem:
            self.dsem[key] = [self.es.enter_context(self.nc.semaphore("d_" + str(key))), 0]
        waits = self._deps(eng, reads, writes)
        self.dsem[key][1] += 16
        token = ("d", key, self.dsem[key][1])
        self._mark(token, reads, writes)
        self._emit_now(eng, waits, fn, (self.dsem[key][0], 16))

    def final_wait(self, eng, resources):
        waits = self._deps(eng, resources, resources)
        self._emit_now(eng, waits, None, None)

    ENGMAP = {"pe": "tensor", "act": "scalar", "dve": "vector", "pool": "gpsimd", "sp": "sync"}

    def _emit_now(self, e, waits, fn, inc):
        eng = getattr(self.nc, self.ENGMAP[e])
        for sem, val in waits:
            eng.wait_ge(sem, val)
        if fn is not None:
            ins = fn(eng)
            ins.then_inc(inc[0], inc[1])
        self.ninstr = getattr(self, "ninstr", 0) + 1 + len(waits)


C_AQ, C_AK, C_AV, C_AF = 0, 256, 512, 768
C_SU, C_SV = 772, 1028
C_SZ, C_XBC, C_DT = 1284, 1540, 2308
C_MQ, C_MK, C_MV, C_MO, C_MI, C_MF = 2312, 2568, 2824, 3080, 3336, 3340
TM_SEGS = [("su", C_SU), ("sv", C_SV), ("av", C_AV), ("mv", C_MV), ("mk", C_MK), ("sz", C_SZ), ("mo", C_MO)]
CM_SEGS = [("aq", C_AQ, 256), ("ak", C_AK, 256), ("xbc", C_XBC, 768), ("mq", C_MQ, 256), ("mkt", C_MK, 256)]
N_TM = 256 * len(TM_SEGS)
N_CM = 256 * 2 + 768 + 256 * 2
WCOLS = N_TM + N_CM + 16


class Builder:
    def __init__(self, S, nlayers, debug=False, phases=None):
        self.S = S
        self.L = nlayers
        self.debug = debug
        self.phases = phases
        self.NT = S // 128
        self.TBK = min(S, 2048)
        self.nc = bass.Bass("TRN2", target_bir_lowering=False)
        self.es = ExitStack()
        self.P = Prog(self.nc, self.es, same_engine_sync=SAME_ENGINE_SYNC)
        self.dbg_names = []

    def sb(self, name, shape, dt):
        return self.es.enter_context(self.nc.sbuf_tensor(name, shape, dt))

    def ps(self, name, shape, dt):
        return self.es.enter_context(self.nc.psum_tensor(name, shape, dt))

    def din(self, name, shape, dt=F32):
        return self.nc.dram_tensor(name, list(shape), dt, kind="ExternalInput").ap()

    def scr(self, name, shape, dt):
        if self.debug:
            self.dbg_names.append(name)
            return self.nc.dram_tensor(name, list(shape), dt, kind="ExternalOutput").ap()
        return self.nc.dram_tensor(name, list(shape), dt, kind="Internal").ap()

    def R(self, name=""):
        return self.P.res(name)

    def RL(self, name, n):
        return [self.P.res(f"{name}{i}") for i in range(n)]

    def RP(self, name=""):
        r = self.P.res(name)
        r.excl = True
        return r

    def RLP(self, name, n):
        return [self.RP(f"{name}{i}") for i in range(n)]

    def op(self, eng, fn, reads=(), writes=()):
        self.P.op(eng, fn, reads, writes)

    def dma(self, eng, fn, key, reads=(), writes=()):
        self.P.dma(eng, fn, key, reads, writes)

    def barrier(self):
        P = self.P
        allw = [(P.sem[e], P.cnt[e]) for e in P.ENG if P.cnt[e] > 0]
        allw += [(s, c) for (s, c) in P.dsem.values() if c > 0]
        for e in P.ENG:
            waits = []
            for sem, val in allw:
                if P.waited[e].get(id(sem), 0) < val:
                    waits.append((sem, val))
                    P.waited[e][id(sem)] = val
            P._emit_now(e, waits, None, None)

    class Phase:
        def __init__(self, b):
            self.b = b

        def __enter__(self):
            self.saved = self.b.es
            self.b.es_phase = ExitStack()
            self.b._alloc_es = self.b.es_phase
            return self

        def __exit__(self, *a):
            self.b.barrier()
            self.b.es_phase.close()
            self.b._alloc_es = self.b.es
            return False

    def phase(self):
        return Builder.Phase(self)

    def run_window(self, make_gen, n, G=2, skew=0):
        active = []
        nxt = 0
        first = True
        while nxt < n or active:
            while len(active) < G and nxt < n:
                g = make_gen(nxt)
                nxt += 1
                if first and skew > 0 and G > 1:
                    first = False
                    alive = True
                    for _ in range(skew):
                        try:
                            next(g)
                        except StopIteration:
                            alive = False
                            break
                    if alive:
                        active.append(g)
                else:
                    first = False
                    active.append(g)
            for g in list(active):
                try:
                    next(g)
                except StopIteration:
                    active.remove(g)

    def tsb(self, name, shape, dt):
        self._uid = getattr(self, "_uid", 0) + 1
        return self._alloc_es.enter_context(self.nc.sbuf_tensor(f"{name}_{self._uid}", shape, dt))

    def tps(self, name, shape, dt):
        self._uid = getattr(self, "_uid", 0) + 1
        esz = 2 if dt == BF16 else 4
        nfree = int(np.prod(shape[1:]))
        assert nfree * esz <= 2048, (name, shape)
        t = self._alloc_es.enter_context(self.nc.psum_tensor(f"{name}_{self._uid}", [128, 2048 // esz], dt))
        ap = t[0:shape[0], 0:nfree]
        if len(shape) == 3:
            ap = ap.rearrange("p (a b) -> p a b", b=shape[2])
        return ap

    def declare(self):
        S, L = self.S, self.L
        di = self.din
        self.x_in = di("x", [S, D])
        self.c_in = di("c", [1, D])
        self.w_in = di("w_in", [L, D, 3344])
        self.w_out = di("w_out", [L, D, D])
        self.w_mix_norm = di("w_mix_norm", [L, D])
        self.attn_f_bias = di("attn_f_bias", [L, 4])
        self.sg_w = di("sg_w", [L, 4, 128, 128])
        self.sg_b = di("sg_b", [L, 4, 128])
        self.ssm_conv_w = di("ssm_conv_w", [L, 4, 768])
        self.ssm_conv_b = di("ssm_conv_b", [L, 768])
        self.ssm_dt_bias = di("ssm_dt_bias", [L, 4])
        self.ssm_a_log = di("ssm_a_log", [L, 4])
        self.ssm_d = di("ssm_d", [L, 4])
        self.mlstm_i_bias = di("mlstm_i_bias", [L, 4])
        self.mlstm_f_bias = di("mlstm_f_bias", [L, 4])
        self.w_ada = di("w_ada", [L, D, 6 * D])
        self.b_ada = di("b_ada", [L, 6 * D])
        self.w_norm1 = di("w_norm1", [L, D])
        self.w_norm2 = di("w_norm2", [L, D])
        self.w_rg = di("w_router_group", [L, D, 4])
        self.b_rg = di("b_router_group", [L, 4])
        self.w_re = di("w_router_expert", [L, D, 32])
        self.b_re = di("b_router_expert", [L, 32])
        self.w_eg = di("w_expert_gate", [L * 32 * 128, 4096])
        self.w_eu = di("w_expert_up", [L * 32 * 128, 4096])
        self.w_ed = di("w_expert_down", [L * 32 * 128, 4096])
        self.w_norm_final = di("w_norm_final", [1, D])
        self.out = self.nc.dram_tensor("out", [S, D], F32, kind="ExternalOutput").ap()
        sc = self.scr
        self.X = sc("X", [S, D], F32)
        self.QT = sc("QT", [256, S], BF16)
        self.KT = sc("KT", [256, S], BF16)
        self.AV = sc("AV", [S, 256], BF16)
        self.GR = sc("GR", [16, S], F32)
        self.SU = sc("SU", [S, 256], BF16)
        self.SV = sc("SV", [S, 256], BF16)
        self.SZ = sc("SZ", [S, 256], BF16)
        self.XBC = sc("XBC", [768, S], BF16)
        self.XC = sc("XC", [768, S], BF16)
        self.MQT = sc("MQT", [256, S], BF16)
        self.MKT = sc("MKT", [256, S], BF16)
        self.MK = sc("MK", [S, 256], BF16)
        self.MV = sc("MV", [S, 256], BF16)
        self.MO = sc("MO", [S, 256], BF16)
        self.Y = sc("Y", [S, D], BF16)
        self.FQd = sc("FQd", [4, 3, S], BF16)
        self.FKd = sc("FKd", [4, 3, S], BF16)
        self.SROW = sc("SROW", [20, S], F32)
        self.MROW = sc("MROW", [24, S], F32)
        self.MSCL = sc("MSCL", [4, S // 128], F32)
        self.MB = 512 if S >= 2048 else 128
        self.LB = {512: 9, 128: 7}[self.MB]
        self.NB = (2 * S) // self.MB + 32
        self.HS = sc("HS", [S, D], BF16)
        self.XS = sc("XS", [self.NB * self.MB, D], BF16)
        self.YB = sc("YB", [self.NB * self.MB, D], F32)
        NT = self.NT
        self.rX = self.RL("X", NT)
        self.rY = [self.RL(f"Y{m}_", NT) for m in range(4)]
        self.rProj = self.RL("proj", max(1, S // 512))
        self.rXC = self.R("XC")

    def consts(self):
        nc = self.nc
        self.ones_f = self.sb("ones_f", [128, 512], F32)
        self.zeros_f = self.sb("zeros_f", [128, 128], F32)
        self.ident_f = self.sb("ident_f", [128, 128], F32)
        self.ident_b = self.sb("ident_b", [128, 128], BF16)
        self.maskb = self.sb("maskb", [128, 128], F32)
        self.mask01 = self.sb("mask01", [128, 128], BF16)
        self.mask01f = self.sb("mask01f", [128, 128], F32)
        self.ones_b = self.sb("ones_b", [128, 128], BF16)
        self.rC = self.R("consts")
        r = self.rC
        self.op("pool", lambda e: e.memset(self.ones_f[:], 1.0), writes=[r])
        self.op("pool", lambda e: e.memset(self.zeros_f[:], 0.0), writes=[r])
        self.op("pool", lambda e: e.memset(self.ones_b[:], 1.0), writes=[r])
        self.op("pool", lambda e: e.affine_select(out=self.ident_f[:], in_=self.ones_f[:, 0:128], pattern=[[-1, 128]],
                                                   compare_op=ALU.is_equal, fill=0.0, base=0, channel_multiplier=1),
                reads=[r], writes=[r])
        self.op("pool", lambda e: e.tensor_copy(out=self.ident_b[:], in_=self.ident_f[:]), reads=[r], writes=[r])
        self.op("pool", lambda e: e.affine_select(out=self.maskb[:], in_=self.zeros_f[:], pattern=[[1, 128]],
                                                   compare_op=ALU.is_ge, fill=NEG, base=0, channel_multiplier=-1),
                reads=[r], writes=[r])
        self.op("pool", lambda e: e.affine_select(out=self.mask01f[:], in_=self.ones_f[:, 0:128], pattern=[[1, 128]],
                                                   compare_op=ALU.is_ge, fill=0.0, base=0, channel_multiplier=-1),
                reads=[r], writes=[r])
        self.op("pool", lambda e: e.tensor_copy(out=self.mask01[:], in_=self.mask01f[:]), reads=[r], writes=[r])
        self.sel = self.sb("sel", [4, 4, 128], F32)
        self.seln = self.sb("seln", [4, 4, 128], F32)
        self.op("pool", lambda e: e.memset(self.sel[:], 1.0), writes=[r])
        for h in range(4):
            self.op("pool", lambda e, h=h: e.affine_select(out=self.sel[:, h, :], in_=self.sel[:, h, :], pattern=[[0, 128]],
                                                            compare_op=ALU.is_equal, fill=0.0, base=-h, channel_multiplier=1),
                    reads=[r], writes=[r])
        self.op("pool", lambda e: e.tensor_scalar(out=self.seln[:], in0=self.sel[:], scalar1=-1.0, scalar2=None, op0=ALU.mult),
                reads=[r], writes=[r])
        self.sel127 = self.sb("sel127", [128, 128], F32)
        self.op("pool", lambda e: e.affine_select(out=self.sel127[:], in_=self.ones_f[:, 0:128], pattern=[[0, 128]],
                                                   compare_op=ALU.is_equal, fill=0.0, base=-127, channel_multiplier=1),
                reads=[r], writes=[r])
        self.condT = self.sb("condT", [128, 8], F32)
        self.condB = self.sb("condB", [128, 8, 128], F32)
        self.dma("sp", lambda e: e.dma_start(out=self.condT[:], in_=self.c_in.rearrange("o (k p) -> p (o k)", p=128),
                                             allow_slow_non_contiguous=True), "cst", writes=[r])
        self.op("act", lambda e: e.activation(out=self.condT[:], in_=self.condT[:], func=AF.Silu), reads=[r], writes=[r])
        for kc in range(8):
            self.op("dve", lambda e, kc=kc: e.tensor_scalar(out=self.condB[:, kc, :], in0=self.ones_f[:, 0:128],
                                                            scalar1=self.condT[:, kc:kc + 1], scalar2=None, op0=ALU.mult),
                    reads=[r], writes=[r])
        self.UTs = self.sb("UTs", [128, 128], BF16)
        utf = self.sb("utf", [128, 128], F32)
        self.op("pool", lambda e: e.affine_select(out=utf[:], in_=self.ones_f[:, 0:128], pattern=[[1, 128]], compare_op=ALU.is_gt, fill=0.0, base=0, channel_multiplier=-1),
                reads=[r], writes=[r])
        self.op("pool", lambda e: e.tensor_copy(out=self.UTs[:], in_=utf[:]), reads=[r], writes=[r])
        self.GATES = self.sb("GATES", [128, self.NT, 2], F32)
        self.DEST = self.sb("DEST", [128, self.NT, 2], I32)
        self.IDXW = self.sb("IDXW", [128, self.NB], I32)
        self.rPLAN = self.R("plan")
        self.MOD = self.sb("MOD", [128, 6 * D], F32)
        self.rMOD = self.R("MOD")

    def adaln(self, l):
        r = self.rMOD
        with self.phase():
            st = [self.tsb("ada_st", [128, 8, 512], F32) for _ in range(2)]
            rst = self.RL("ada_st", 2)
            bb = [self.tsb("ada_b", [128, 512], F32) for _ in range(2)]
            rbb = self.RL("ada_b", 2)
            pm = [self.tps("ada_ps", [128, 512], F32) for _ in range(2)]
            rpm = self.RLP("ada_ps", 2)
            wn = self.tsb("ada_wn", [128, D], F32)
            rwn = self.R("ada_wn")
            for nb in range(12):
                i = nb % 2
                self.dma("sp", lambda e, nb=nb, i=i: e.dma_start(
                    out=st[i][:], in_=self.w_ada[l, :, nb * 512:(nb + 1) * 512].rearrange("(k p) n -> p k n", p=128)),
                    f"ada_st{i}", writes=[rst[i]])
                self.dma("sp", lambda e, nb=nb, i=i: e.dma_start(
                    out=bb[i][:], in_=self.b_ada[l:l + 1, nb * 512:(nb + 1) * 512].partition_broadcast(128)),
                    f"ada_b{i}", writes=[rbb[i]])
                for kc in range(8):
                    self.op("pe", lambda e, kc=kc, i=i: e.matmul(pm[i][:], lhsT=self.condB[:, kc, :], rhs=st[i][:, kc, :],
                                                                 start=(kc == 0), stop=(kc == 7)),
                            reads=[rst[i], self.rC], writes=[rpm[i]])
                self.op("dve", lambda e, nb=nb, i=i: e.tensor_tensor(out=self.MOD[:, nb * 512:(nb + 1) * 512], in0=pm[i][:],
                                                                     in1=bb[i][:], op=ALU.add),
                        reads=[rpm[i], rbb[i]], writes=[r])
            for j, wnorm in ((1, self.w_norm1), (4, self.w_norm2)):
                self.dma("sp", lambda e, wnorm=wnorm: e.dma_start(out=wn[:], in_=wnorm[l:l + 1, :].partition_broadcast(128)),
                         "ada_wn", writes=[rwn])
                self.op("dve", lambda e, j=j: e.scalar_tensor_tensor(out=self.MOD[:, j * D:(j + 1) * D], in0=self.MOD[:, j * D:(j + 1) * D],
                                                                     scalar=1.0, in1=wn[:], op0=ALU.add, op1=ALU.mult),
                        reads=[rwn], writes=[r])

    def head_rms(self, eng, src, n, gain, out, tmp, ss, reads, writes):
        rt = self.R("hr")
        self.op(eng, lambda e: e.tensor_tensor(out=tmp, in0=src, in1=src, op=ALU.mult), reads=reads, writes=[rt])
        self.op("dve", lambda e: e.tensor_reduce(out=ss, in_=tmp, axis=AX.X, op=ALU.add), reads=[rt], writes=[rt])
        self.op("act", lambda e: e.activation(out=ss, in_=ss, func=AF.Sqrt, bias=EPS, scale=1.0 / 64.0), reads=[rt], writes=[rt])
        self.op("dve", lambda e: e.reciprocal(out=ss, in_=ss), reads=[rt], writes=[rt])
        self.op(eng, lambda e: e.tensor_tensor(out=tmp, in0=src, in1=ss.unsqueeze(2).to_broadcast([128, n, 64]), op=ALU.mult),
                reads=[rt] + list(reads), writes=[rt])
        self.op(eng, lambda e: e.tensor_tensor(out=out, in0=tmp, in1=gain, op=ALU.mult), reads=[rt], writes=writes)

    def phase_a(self, l):
        S, NT = self.S, self.NT
        NB = S // 512
        with self.phase():
            W = self.tsb("W", [128, 8, WCOLS], BF16)
            rW = self.R("W")
            st = [self.tsb("wst", [128, 8, 256], F32) for _ in range(2)]
            rst = self.RL("wst", 2)
            pieces = []
            dst = 0
            for name, c0 in TM_SEGS:
                pieces.append((c0, 256, dst, 0.125 if name == "mk" else 1.0))
                dst += 256
            for name, c0, w in CM_SEGS:
                for o in range(0, w, 256):
                    pieces.append((c0 + o, 256, dst, 0.125 if name in ("aq", "mkt") else 1.0))
                    dst += 256
            for c0 in (C_AF, C_DT, C_MI, C_MF):
                pieces.append((c0, 4, dst, 1.0))
                dst += 4
            assert dst == WCOLS
            for pi, (c0, w, d0, scale) in enumerate(pieces):
                i = pi % 2
                self.dma("sp", lambda e, c0=c0, w=w, i=i: e.dma_start(
                    out=st[i][:, :, 0:w], in_=self.w_in[l, :, c0:c0 + w].rearrange("(k p) n -> p k n", p=128),
                    allow_slow_non_contiguous=(w < 64)), f"wst{i}", writes=[rst[i]])
                self.op("dve" if i == 0 else "pool", lambda e, w=w, d0=d0, i=i, scale=scale: e.tensor_scalar(
                    out=W[:, :, d0:d0 + w], in0=st[i][:, :, 0:w], scalar1=scale, scalar2=None, op0=ALU.mult),
                    reads=[rst[i]], writes=[rW])
            xt = [self.tsb("xt", [128, D], F32) for _ in range(2)]
            rxt = self.RL("xt", 2)
            tmp = [self.tsb("htmp", [128, D], F32) for _ in range(2)]
            rtmp = self.RL("htmp", 2)
            hb = [self.tsb("hb", [128, D], BF16) for _ in range(2)]
            rhb = self.RL("hb", 2)
            ss = [self.tsb("ss", [128, 1], F32) for _ in range(2)]
            hT = [self.tsb("hT", [128, 8, 512], BF16) for _ in range(2)]
            rhT = self.RL("hT", 2)
            pT = [self.tps("pT", [128, 4, 128], BF16) for _ in range(2)]
            rpT = self.RLP("pT", 2)
            pm = [self.tps("pm", [128, 512], F32) for _ in range(4)]
            rpm = self.RLP("pm", 4)
            ost = [self.tsb("ost", [128, 512], BF16) for _ in range(4)]
            rost = self.RL("ost", 4)
            gst = self.tsb("gst", [16, 512], F32)
            rgst = self.R("gst")
            tm_names = [n for n, _ in TM_SEGS]
            tm_dest = {"su": self.SU, "sv": self.SV, "av": self.AV, "mv": self.MV, "mk": self.MK, "sz": self.SZ, "mo": self.MO}
            tm_func = {"su": AF.Gelu_apprx_tanh, "sv": AF.Gelu_apprx_tanh, "sz": AF.Silu, "mo": AF.Sigmoid}
            cm_dest = [(self.QT, 0), (self.QT, 128), (self.KT, 0), (self.KT, 128)] + [(self.XBC, 128 * j) for j in range(6)] + \
                      [(self.MQT, 0), (self.MQT, 128), (self.MKT, 0), (self.MKT, 128)]
            npm = 0
            nost = 0
            ntile = 0

            hb8 = [self.tsb("hb8", [128, D], BF16) for _ in range(8)]
            rhb8 = self.RL("hb8", 8)

            def load_x(ti):
                i = ti % 2
                self.dma("sp", lambda e: e.dma_start(out=xt[i][:], in_=self.X[ti * 128:(ti + 1) * 128, :]), f"xt{i}", writes=[rxt[i]])

            def norm_block(tb):
                for tt in range(4):
                    ti = tb * 4 + tt
                    i = ti % 2
                    hbi = (tb % 2) * 4 + tt
                    if ti + 1 < NT:
                        load_x(ti + 1)
                    self.op("pool", lambda e, i=i: e.memset(ss[i][:], 0.0), writes=[rtmp[i]])
                    self.op("act", lambda e, i=i: e.activation(out=tmp[i][:], in_=xt[i][:], func=AF.Square, accum_out=ss[i][:]),
                            reads=[rxt[i]], writes=[rtmp[i]])
                    self.op("act", lambda e, i=i: e.activation(out=ss[i][:], in_=ss[i][:], func=AF.Sqrt, bias=EPS, scale=1.0 / D),
                            writes=[rtmp[i]])
                    self.op("dve", lambda e, i=i: e.reciprocal(out=ss[i][:], in_=ss[i][:]), writes=[rtmp[i]])
                    self.op("dve", lambda e, i=i: e.scalar_tensor_tensor(out=tmp[i][:], in0=xt[i][:], scalar=ss[i][:, 0:1],
                                                                         in1=self.MOD[:, D:2 * D], op0=ALU.mult, op1=ALU.mult),
                            reads=[rxt[i], self.rMOD], writes=[rtmp[i]])
                    self.op("pool", lambda e, i=i, hbi=hbi: e.tensor_tensor(out=hb8[hbi][:], in0=tmp[i][:], in1=self.MOD[:, 0:D], op=ALU.add),
                            reads=[rtmp[i], self.rMOD], writes=[rhb8[hbi]])

            def transp_block(tb):
                b = tb % 2
                for tt in range(4):
                    hbi = (tb % 2) * 4 + tt
                    for half in range(2):
                        for j in range(4):
                            kc = half * 4 + j
                            self.op("pe", lambda e, half=half, j=j, kc=kc, hbi=hbi: e.transpose(
                                out=pT[half][:, j, :], in_=hb8[hbi][:, kc * 128:(kc + 1) * 128], identity=self.ident_b[:]),
                                reads=[rhb8[hbi], self.rC], writes=[rpT[half]])
                        if half == 0:
                            self.op("act", lambda e, tt=tt, b=b: e.copy(out=hT[b][:, 0:4, tt * 128:(tt + 1) * 128], in_=pT[0][:]),
                                    reads=[rpT[0]], writes=[rhT[b]])
                        else:
                            self.op("dve", lambda e, tt=tt, b=b: e.tensor_copy(out=hT[b][:, 4:8, tt * 128:(tt + 1) * 128], in_=pT[1][:]),
                                    reads=[rpT[1]], writes=[rhT[b]])

            load_x(0)
            norm_block(0)
            transp_block(0)
            for tb in range(NB):
                b = tb % 2
                if tb + 1 < NB:
                    norm_block(tb + 1)
                for cb in range(4):
                    c0 = cb * 512
                    w = min(512, N_TM - c0)
                    for tt in range(4):
                        ti = tb * 4 + tt
                        p = npm % 4
                        npm += 1
                        for kc in range(8):
                            self.op("pe", lambda e, p=p, kc=kc, tt=tt, b=b, c0=c0, w=w: e.matmul(
                                pm[p][:, 0:w], lhsT=hT[b][:, kc, tt * 128:(tt + 1) * 128], rhs=W[:, kc, c0:c0 + w],
                                start=(kc == 0), stop=(kc == 7)), reads=[rhT[b], rW], writes=[rpm[p]])
                        o = nost % 4
                        nost += 1
                        for sgi in range(w // 256):
                            name = tm_names[(c0 + sgi * 256) // 256]
                            if name in tm_func:
                                self.op("act", lambda e, o=o, p=p, sgi=sgi, name=name: e.activation(
                                    out=ost[o][:, sgi * 256:(sgi + 1) * 256], in_=pm[p][:, sgi * 256:(sgi + 1) * 256], func=tm_func[name]),
                                    reads=[rpm[p]], writes=[rost[o]])
                            else:
                                self.op("dve", lambda e, o=o, p=p, sgi=sgi: e.tensor_copy(
                                    out=ost[o][:, sgi * 256:(sgi + 1) * 256], in_=pm[p][:, sgi * 256:(sgi + 1) * 256]),
                                    reads=[rpm[p]], writes=[rost[o]])
                            self.dma("act" if name in tm_func else "sp", lambda e, o=o, sgi=sgi, name=name, ti=ti: e.dma_start(
                                out=tm_dest[name][ti * 128:(ti + 1) * 128, :], in_=ost[o][:, sgi * 256:(sgi + 1) * 256]),
                                f"ost{o}", reads=[rost[o]])
                for ch in range(15):
                    m = 128 if ch < 14 else 16
                    c0 = N_TM + ch * 128
                    p = npm % 4
                    npm += 1
                    for kc in range(8):
                        self.op("pe", lambda e, p=p, kc=kc, b=b, c0=c0, m=m: e.matmul(
                            pm[p][0:m, :], lhsT=W[:, kc, c0:c0 + m], rhs=hT[b][:, kc, :], start=(kc == 0), stop=(kc == 7)),
                            reads=[rhT[b], rW], writes=[rpm[p]])
                    if ch < 14:
                        o = nost % 4
                        nost += 1
                        self.op("dve" if ch % 2 == 0 else "act", lambda e, o=o, p=p, ch=ch: (
                            e.tensor_copy(out=ost[o][:], in_=pm[p][:]) if ch % 2 == 0 else e.copy(out=ost[o][:], in_=pm[p][:])),
                            reads=[rpm[p]], writes=[rost[o]])
                        dt_, r0 = cm_dest[ch]
                        self.dma("sp" if ch % 2 == 0 else "act", lambda e, o=o, dt_=dt_, r0=r0, tb=tb: e.dma_start(
                            out=dt_[r0:r0 + 128, tb * 512:(tb + 1) * 512], in_=ost[o][:]), f"ost{o}", reads=[rost[o]])
                    else:
                        self.op("dve", lambda e, p=p: e.tensor_copy(out=gst[:], in_=pm[p][0:16, :]), reads=[rpm[p]], writes=[rgst])
                        self.dma("sp", lambda e, tb=tb: e.dma_start(out=self.GR[:, tb * 512:(tb + 1) * 512], in_=gst[:]),
                                 "gst", reads=[rgst])
                if tb + 1 < NB:
                    transp_block(tb + 1)

    def logsigmoid_rows(self, g, t1, t2, r):
        self.op("dve", lambda e: e.scalar_tensor_tensor(out=t1, in0=g, scalar=-1.0, in1=g, op0=ALU.mult, op1=ALU.max), reads=[r], writes=[r])
        self.op("act", lambda e: e.activation(out=t1, in_=t1, func=AF.Exp, scale=-1.0), reads=[r], writes=[r])
        self.op("act", lambda e: e.activation(out=t1, in_=t1, func=AF.Ln, bias=1.0), reads=[r], writes=[r])
        self.op("dve", lambda e: e.tensor_scalar_min(out=t2, in0=g, scalar1=0.0), reads=[r], writes=[r])
        self.op("dve", lambda e: e.tensor_tensor(out=g, in0=t2, in1=t1, op=ALU.subtract), reads=[r], writes=[r])

    def load_col(self, dst, src_row, key, r):
        self.dma("sp", lambda e: e.dma_start(out=dst, in_=src_row.rearrange("o h -> h o"), allow_slow_non_contiguous=True), key, writes=[r])

    def phase_attn(self, l):
        S, NT = self.S, self.NT
        NQ = S // 512
        TB = self.TBK
        with self.phase():
            g = self.tsb("ag", [4, TB], F32)
            t1 = self.tsb("at1", [4, TB], F32)
            t2 = self.tsb("at2", [4, TB], F32)
            fb = self.tsb("afb", [4, 1], F32)
            carry = self.tsb("acarry", [4, 1], F32)
            fq = self.tsb("afq", [4, 3, TB], BF16)
            fk = self.tsb("afk", [4, 3, TB], BF16)
            rg = self.R("agates")
            self.load_col(fb[:], self.attn_f_bias[l:l + 1, :], "ag", rg)
            self.op("pool", lambda e: e.memset(carry[:], 0.0), writes=[rg])
            ones_bc = self.ones_f[0:4, 0:1].to_broadcast([4, TB])
            for blk in range(S // TB):
                t0 = blk * TB
                self.dma("sp", lambda e, t0=t0: e.dma_start(out=g[:], in_=self.GR[0:4, t0:t0 + TB]), "ag", writes=[rg])
                self.op("dve", lambda e: e.tensor_scalar(out=g[:], in0=g[:], scalar1=fb[:, 0:1], scalar2=None, op0=ALU.add), reads=[rg], writes=[rg])
                self.logsigmoid_rows(g[:], t1[:], t2[:], rg)
                self.op("dve", lambda e: e.tensor_tensor_scan(out=t1[:], data0=ones_bc, data1=g[:], initial=carry[:, 0:1], op0=ALU.mult, op1=ALU.add),
                        reads=[rg, self.rC], writes=[rg])
                self.op("dve", lambda e: e.tensor_copy(out=carry[:], in_=t1[:, TB - 1:TB]), reads=[rg], writes=[rg])
                self.op("dve", lambda e: e.tensor_copy(out=fq[:, 0, :], in_=t1[:]), reads=[rg], writes=[rg])
                self.op("dve", lambda e: e.tensor_copy(out=t2[:], in_=fq[:, 0, :]), reads=[rg], writes=[rg])
                self.op("dve", lambda e: e.tensor_tensor(out=g[:], in0=t1[:], in1=t2[:], op=ALU.subtract), reads=[rg], writes=[rg])
                self.op("dve", lambda e: e.tensor_copy(out=fq[:, 1, :], in_=g[:]), reads=[rg], writes=[rg])
                self.op("dve", lambda e: e.tensor_copy(out=t2[:], in_=fq[:, 1, :]), reads=[rg], writes=[rg])
                self.op("dve", lambda e: e.tensor_tensor(out=t1[:], in0=g[:], in1=t2[:], op=ALU.subtract), reads=[rg], writes=[rg])
                self.op("dve", lambda e: e.tensor_copy(out=fq[:, 2, :], in_=t1[:]), reads=[rg], writes=[rg])
                self.op("dve", lambda e: e.tensor_scalar(out=fk[:], in0=fq[:], scalar1=-1.0, scalar2=None, op0=ALU.mult), reads=[rg], writes=[rg])
                self.dma("sp", lambda e, t0=t0: e.dma_start(out=self.FQd[:, :, t0:t0 + TB], in_=fq[:]), "ag", reads=[rg], writes=[rg])
                self.dma("sp", lambda e, t0=t0: e.dma_start(out=self.FKd[:, :, t0:t0 + TB], in_=fk[:]), "ag", reads=[rg], writes=[rg])
        with self.phase():
            gain = self.tsb("again", [128, 256], F32)
            rg = self.R("again")
            self.dma("sp", lambda e: e.dma_start(out=gain[:], in_=self.w_mix_norm[l:l + 1, 0:256].partition_broadcast(128)), "ag", writes=[rg])
            FQ, FK = self.FQd, self.FKd
            QA = [self.tsb("QA", [70, S], BF16) for _ in range(2)]
            KA = [self.tsb("KA", [70, S], BF16) for _ in range(2)]
            VA = [self.tsb("VA", [128, NT, 65], BF16) for _ in range(2)]
            rH = self.RL("ahead", 2)
            for i in range(2):
                self.op("pool", lambda e, i=i: e.memset(QA[i][64:70, :], 1.0), writes=[rH[i]])
                self.op("pool", lambda e, i=i: e.memset(KA[i][64:70, :], 1.0), writes=[rH[i]])
                self.op("pool", lambda e, i=i: e.memset(VA[i][:, :, 64:65], 1.0), writes=[rH[i]])
            NSP, NPT = 5, 6
            sps = [self.tps("asp", [128, 512], F32) for _ in range(NSP)]
            rsp = self.RLP("asp", NSP)
            pts = [self.tsb("apt", [128, 512], BF16) for _ in range(NPT)]
            rpt = self.RL("apt", NPT)
            Ops = [self.tps("aO", [65, 512], F32) for _ in range(2)]
            rO = self.RLP("aO", 2)
            OT = self.tsb("aOT", [65, 512], F32)
            rOT = self.R("aOT")
            po = self.tps("apo", [128, 4, 65], F32)
            rpo = self.RP("apo")
            rden = self.tsb("arden", [128, 4], F32)
            hn = self.tsb("ahn", [128, 4, 64], F32)
            htmp = self.tsb("ahtmp", [128, 4, 64], F32)
            hss = self.tsb("ahss", [128, 4], F32)
            yb = [self.tsb("ayb", [128, 4, 64], BF16) for _ in range(2)]
            ryb = self.RL("ayb", 2)
            rhn = self.R("ahn")

            def load_head(h):
                i = h % 2
                q = "pool"
                self.dma(q, lambda e: e.dma_start(out=QA[i][0:64, :], in_=self.QT[h * 64:(h + 1) * 64, :]), f"ahd{i}", writes=[rH[i]])
                self.dma(q, lambda e: e.dma_start(out=KA[i][0:64, :], in_=self.KT[h * 64:(h + 1) * 64, :]), f"ahd{i}", writes=[rH[i]])
                for j in range(3):
                    self.dma(q, lambda e, j=j: e.dma_start(out=QA[i][64 + j:65 + j, :], in_=FQ[h:h + 1, j, :]), f"ahd{i}", reads=[rg], writes=[rH[i]])
                    self.dma(q, lambda e, j=j: e.dma_start(out=KA[i][67 + j:68 + j, :], in_=FK[h:h + 1, j, :]), f"ahd{i}", reads=[rg], writes=[rH[i]])
                self.dma(q, lambda e: e.dma_start(out=VA[i][:, :, 0:64], in_=self.AV[:, h * 64:(h + 1) * 64].rearrange("(n p) d -> p n d", p=128)),
                         f"ahd{i}", writes=[rH[i]])

            steps = [(h, qb, kt) for h in range(4) for qb in range(NQ) for kt in range(4 * (qb + 1))]
            nyb = [0]

            def emit_qk(idx):
                h, qb, kt = steps[idx]
                i = h % 2
                c0 = max(kt - 4 * qb, 0) * 128
                sp, pt = idx % NSP, idx % NPT
                self.op("pe", lambda e: e.matmul(sps[sp][:, c0:512], lhsT=KA[i][0:70, kt * 128:(kt + 1) * 128],
                                                 rhs=QA[i][0:70, qb * 512 + c0:(qb + 1) * 512], start=True, stop=True),
                        reads=[rH[i]], writes=[rsp[sp]])
                self.op("act", lambda e: e.activation(out=pts[pt][:, c0:512], in_=sps[sp][:, c0:512], func=AF.Exp),
                        reads=[rsp[sp]], writes=[rpt[pt]])
                if kt - 4 * qb >= 0:
                    self.op("pool", lambda e: e.tensor_tensor(out=pts[pt][:, c0:c0 + 128], in0=pts[pt][:, c0:c0 + 128], in1=self.mask01[:], op=ALU.mult),
                            reads=[rpt[pt], self.rC], writes=[rpt[pt]])

            def emit_pv(idx):
                h, qb, kt = steps[idx]
                i = h % 2
                c0 = max(kt - 4 * qb, 0) * 128
                pt = idx % NPT
                ob = (h * NQ + qb) % 2
                nk = 4 * (qb + 1)
                self.op("pe", lambda e: e.matmul(Ops[ob][:, c0:512], lhsT=VA[i][:, kt, :], rhs=pts[pt][:, c0:512], start=(kt == 0), stop=(kt == nk - 1)),
                        reads=[rH[i], rpt[pt]], writes=[rO[ob]])
                if kt == nk - 1:
                    self.op("dve", lambda e: e.tensor_copy(out=OT[:], in_=Ops[ob][:]), reads=[rO[ob]], writes=[rOT])
                    for tt in range(4):
                        self.op("pe", lambda e, tt=tt: e.transpose(out=po[:, tt, :], in_=OT[0:65, tt * 128:(tt + 1) * 128], identity=self.ident_f[0:65, 0:65]),
                                reads=[rOT, self.rC], writes=[rpo])
                    self.op("dve", lambda e: e.reciprocal(out=rden[:], in_=po[:, :, 64]), reads=[rpo], writes=[rhn])
                    self.op("dve", lambda e: e.tensor_tensor(out=hn[:], in0=po[:, :, 0:64], in1=rden[:].unsqueeze(2).to_broadcast([128, 4, 64]), op=ALU.mult),
                            reads=[rpo, rhn], writes=[rhn])
                    y = nyb[0] % 2
                    nyb[0] += 1
                    self.head_rms("pool", hn[:], 4, gain[:, h * 64:(h + 1) * 64].unsqueeze(1).to_broadcast([128, 4, 64]), yb[y][:], htmp[:], hss[:],
                                  reads=[rhn, rg], writes=[ryb[y]])
                    self.dma("sp", lambda e: e.dma_start(out=self.Y[qb * 512:(qb + 1) * 512, h * 64:(h + 1) * 64].rearrange("(n p) d -> p n d", p=128),
                                                         in_=yb[y][:]), f"ayb{y}", reads=[ryb[y]])
                    if qb == NQ - 1 and h + 2 < 4:
                        load_head(h + 2)

            load_head(0)
            load_head(1)
            SK = 4
            for idx in range(len(steps)):
                emit_qk(idx)
                if idx >= SK:
                    emit_pv(idx - SK)
            for idx in range(max(0, len(steps) - SK), len(steps)):
                emit_pv(idx)

    def phase_sg(self, l):
        S, NT = self.S, self.NT
        with self.phase():
            wst = [self.tsb("gwst", [128, 128], F32) for _ in range(2)]
            rwst = self.RL("gwst", 2)
            pw = self.tps("gpw", [128, 128], F32)
            rpw = self.RP("gpw")
            WT = self.tsb("gWT", [128, 4, 128], BF16)
            SGB = self.tsb("gSGB", [128, 4], F32)
            gain = self.tsb("ggain", [128, 256], F32)
            rW = self.R("gW")
            for h in range(4):
                i = h % 2
                self.dma("sp", lambda e, h=h, i=i: e.dma_start(out=wst[i][:], in_=self.sg_w[l, h, :, :]), f"gwst{i}", writes=[rwst[i]])
                self.op("pe", lambda e, i=i: e.transpose(out=pw[:], in_=wst[i][:], identity=self.ident_f[:]), reads=[rwst[i], self.rC], writes=[rpw])
                self.op("dve", lambda e, h=h: e.tensor_tensor(out=WT[:, h, :], in0=pw[:], in1=self.mask01f[:], op=ALU.mult), reads=[rpw, self.rC], writes=[rW])
            self.dma("sp", lambda e: e.dma_start(out=SGB[:], in_=self.sg_b[l, :, :].rearrange("h t -> t h"), allow_slow_non_contiguous=True), "gw", writes=[rW])
            self.dma("sp", lambda e: e.dma_start(out=gain[:], in_=self.w_mix_norm[l:l + 1, 256:512].partition_broadcast(128)), "gw", writes=[rW])
            su = [self.tsb("gsu", [128, 256], BF16) for _ in range(2)]
            sv = [self.tsb("gsv", [128, 256], BF16) for _ in range(2)]
            rin = self.RL("gin", 2)
            svf = self.tsb("gsvf", [128, 4, 64], F32)
            htmp = self.tsb("ghtmp", [128, 4, 64], F32)
            hss = self.tsb("ghss", [128, 4], F32)
            rsvf = self.R("gsvf")
            vn = [self.tsb("gvn", [128, 4, 64], BF16) for _ in range(2)]
            rvn = self.RL("gvn", 2)
            pm = [self.tps("gpm", [128, 256], F32) for _ in range(2)]
            rpm = self.RLP("gpm", 2)
            yb = [self.tsb("gyb", [128, 256], BF16) for _ in range(2)]
            ryb = self.RL("gyb", 2)
            def load_sg(c):
                i = c % 2
                self.dma("sp", lambda e: e.dma_start(out=su[i][:], in_=self.SU[c * 128:(c + 1) * 128, :]), f"gin{i}", writes=[rin[i]])
                self.dma("sp", lambda e: e.dma_start(out=sv[i][:], in_=self.SV[c * 128:(c + 1) * 128, :]), f"gin{i}", writes=[rin[i]])

            load_sg(0)
            for c in range(NT):
                i = c % 2
                if c + 1 < NT:
                    load_sg(c + 1)
                self.op("pool", lambda e, i=i: e.tensor_copy(out=svf[:], in_=sv[i][:].rearrange("p (h d) -> p h d", d=64)), reads=[rin[i]], writes=[rsvf])
                self.head_rms("pool", svf[:], 4, gain[:].rearrange("p (h d) -> p h d", d=64), vn[i][:], htmp[:], hss[:], reads=[rsvf, rW], writes=[rvn[i]])
                for h in range(4):
                    self.op("pe", lambda e, h=h, i=i: e.matmul(pm[i][:, h * 64:(h + 1) * 64], lhsT=WT[:, h, :], rhs=vn[i][:, h, :], start=True, stop=True),
                            reads=[rW, rvn[i]], writes=[rpm[i]])
                for h in range(4):
                    self.op("dve", lambda e, h=h, i=i: e.scalar_tensor_tensor(out=yb[i][:, h * 64:(h + 1) * 64], in0=pm[i][:, h * 64:(h + 1) * 64],
                                                                              scalar=SGB[:, h:h + 1], in1=su[i][:, h * 64:(h + 1) * 64], op0=ALU.add, op1=ALU.mult),
                            reads=[rpm[i], rin[i], rW], writes=[ryb[i]])
                self.dma("sp", lambda e, c=c, i=i: e.dma_start(out=self.Y[c * 128:(c + 1) * 128, 256:512], in_=yb[i][:]), f"gyb{i}", reads=[ryb[i]])

    def phase_ssd(self, l):
        S, NT = self.S, self.NT
        TB = self.TBK
        with self.phase():
            CW = self.tsb("cCW", [128, 6, 4], F32)
            CB = self.tsb("cCB", [128, 6], F32)
            rcw = self.R("cCW")
            for j in range(4):
                self.dma("sp", lambda e, j=j: e.dma_start(out=CW[:, :, j], in_=self.ssm_conv_w[l, j:j + 1, :].rearrange("o (c p) -> p (o c)", p=128), allow_slow_non_contiguous=True), "ccw", writes=[rcw])
            self.dma("sp", lambda e: e.dma_start(out=CB[:], in_=self.ssm_conv_b[l:l + 1, :].rearrange("o (c p) -> p (o c)", p=128), allow_slow_non_contiguous=True), "ccw", writes=[rcw])
            xin = [self.tsb("cxin", [128, TB + 3], BF16) for _ in range(2)]
            rxin = self.RL("cxin", 2)
            acc = [self.tsb("cacc", [128, TB], F32) for _ in range(2)]
            racc = self.RL("cacc", 2)
            xo = [self.tsb("cxo", [128, TB], BF16) for _ in range(2)]
            rxo = self.RL("cxo", 2)
            n = 0
            for cc in range(6):
                for blk in range(S // TB):
                    i = n % 2
                    n += 1
                    t0 = blk * TB
                    if blk == 0:
                        self.op("pool", lambda e, i=i: e.memset(xin[i][:, 0:3], 0.0), writes=[rxin[i]])
                        self.dma("sp", lambda e, i=i, cc=cc: e.dma_start(out=xin[i][:, 3:TB + 3], in_=self.XBC[cc * 128:(cc + 1) * 128, 0:TB]), f"cxin{i}", writes=[rxin[i]])
                    else:
                        self.dma("sp", lambda e, i=i, cc=cc, t0=t0: e.dma_start(out=xin[i][:, 0:TB + 3], in_=self.XBC[cc * 128:(cc + 1) * 128, t0 - 3:t0 + TB]),
                                 f"cxin{i}", writes=[rxin[i]])
                    eng = "dve"
                    self.op(eng, lambda e, i=i, cc=cc: e.tensor_scalar(out=acc[i][:], in0=xin[i][:, 0:TB], scalar1=CW[:, cc, 0:1], scalar2=None, op0=ALU.mult),
                            reads=[rxin[i], rcw], writes=[racc[i]])
                    for j in range(1, 4):
                        self.op(eng, lambda e, i=i, cc=cc, j=j: e.scalar_tensor_tensor(out=acc[i][:], in0=xin[i][:, j:j + TB], scalar=CW[:, cc, j:j + 1], in1=acc[i][:],
                                                                                     op0=ALU.mult, op1=ALU.add), reads=[rxin[i], rcw], writes=[racc[i]])
                    self.op("act", lambda e, i=i, cc=cc: e.activation(out=xo[i][:], in_=acc[i][:], func=AF.Silu, bias=CB[:, cc:cc + 1]),
                            reads=[racc[i], rcw], writes=[rxo[i]])
                    self.dma("sp", lambda e, i=i, cc=cc, t0=t0: e.dma_start(out=self.XC[cc * 128:(cc + 1) * 128, t0:t0 + TB], in_=xo[i][:]), f"cxo{i}", reads=[rxo[i]])
        with self.phase():
            g = self.tsb("sg_", [4, TB], F32)
            t1 = self.tsb("st1", [4, TB], F32)
            t2 = self.tsb("st2", [4, TB], F32)
            t3 = self.tsb("st3", [4, TB], F32)
            rm = self.tsb("srm", [4, TB], F32)
            dtb = self.tsb("sdtb", [4, 1], F32)
            an = self.tsb("san", [4, 1], F32)
            rg = self.R("sgates")
            self.load_col(dtb[:], self.ssm_dt_bias[l:l + 1, :], "sgt", rg)
            self.load_col(an[:], self.ssm_a_log[l:l + 1, :], "sgt", rg)
            self.op("act", lambda e: e.activation(out=an[:], in_=an[:], func=AF.Exp), reads=[rg], writes=[rg])
            self.op("dve", lambda e: e.tensor_scalar(out=an[:], in0=an[:], scalar1=-1.0, scalar2=None, op0=ALU.mult), reads=[rg], writes=[rg])
            self.op("pool", lambda e: e.memset(rm[:], 1.0), writes=[rg])
            self.op("pool", lambda e: e.memset(rm[:].rearrange("p (n c) -> p n c", c=128)[:, :, 0:1], 0.0), reads=[rg], writes=[rg])
            for blk in range(S // TB):
                t0 = blk * TB
                self.dma("sp", lambda e, t0=t0: e.dma_start(out=g[:], in_=self.GR[4:8, t0:t0 + TB]), "sgt", writes=[rg])
                self.op("dve", lambda e: e.tensor_scalar(out=g[:], in0=g[:], scalar1=dtb[:, 0:1], scalar2=None, op0=ALU.add), reads=[rg], writes=[rg])
                self.op("dve", lambda e: e.scalar_tensor_tensor(out=t1[:], in0=g[:], scalar=-1.0, in1=g[:], op0=ALU.mult, op1=ALU.max), reads=[rg], writes=[rg])
                self.op("act", lambda e: e.activation(out=t1[:], in_=t1[:], func=AF.Exp, scale=-1.0), reads=[rg], writes=[rg])
                self.op("act", lambda e: e.activation(out=t1[:], in_=t1[:], func=AF.Ln, bias=1.0), reads=[rg], writes=[rg])
                self.op("dve", lambda e: e.tensor_scalar_max(out=t2[:], in0=g[:], scalar1=0.0), reads=[rg], writes=[rg])
                self.op("dve", lambda e: e.tensor_tensor(out=g[:], in0=t2[:], in1=t1[:], op=ALU.add), reads=[rg], writes=[rg])
                self.dma("sp", lambda e, t0=t0: e.dma_start(out=self.SROW[4:8, t0:t0 + TB], in_=g[:]), "sgt", reads=[rg], writes=[rg])
                self.op("dve", lambda e: e.tensor_scalar(out=t1[:], in0=g[:], scalar1=an[:, 0:1], scalar2=None, op0=ALU.mult), reads=[rg], writes=[rg])
                self.op("dve", lambda e: e.tensor_tensor_scan(out=t2[:], data0=rm[:], data1=t1[:], initial=0.0, op0=ALU.mult, op1=ALU.add), reads=[rg], writes=[rg])
                self.dma("sp", lambda e, t0=t0: e.dma_start(out=self.SROW[16:20, t0:t0 + TB], in_=t2[:]), "sgt", reads=[rg], writes=[rg])
                v3 = lambda t: t[:].rearrange("p (n c) -> p n c", c=128)
                self.op("dve", lambda e: e.tensor_copy(out=v3(t1), in_=v3(t2)[:, :, 127:128].to_broadcast([4, TB // 128, 128])), reads=[rg], writes=[rg])
                self.op("act", lambda e: e.activation(out=t3[:], in_=t1[:], func=AF.Exp), reads=[rg], writes=[rg])
                self.dma("sp", lambda e, t0=t0: e.dma_start(out=self.SROW[12:16, t0:t0 + TB], in_=t3[:]), "sgt", reads=[rg], writes=[rg])
                self.op("dve", lambda e: e.tensor_tensor(out=t1[:], in0=t1[:], in1=t2[:], op=ALU.subtract), reads=[rg], writes=[rg])
                self.op("act", lambda e: e.activation(out=t1[:], in_=t1[:], func=AF.Exp), reads=[rg], writes=[rg])
                self.op("dve", lambda e: e.tensor_tensor(out=t1[:], in0=t1[:], in1=g[:], op=ALU.mult), reads=[rg], writes=[rg])
                self.dma("sp", lambda e, t0=t0: e.dma_start(out=self.SROW[8:12, t0:t0 + TB], in_=t1[:]), "sgt", reads=[rg], writes=[rg])
                self.op("dve", lambda e: e.tensor_scalar(out=t2[:], in0=t2[:], scalar1=-1.0, scalar2=None, op0=ALU.mult), reads=[rg], writes=[rg])
                self.dma("sp", lambda e, t0=t0: e.dma_start(out=self.SROW[0:4, t0:t0 + TB], in_=t2[:]), "sgt", reads=[rg], writes=[rg])
        with self.phase():
            gain = self.tsb("sgain", [128, 256], F32)
            Dbc = self.tsb("sDbc", [128, 4], F32)
            rk = self.R("sconst")
            self.dma("sp", lambda e: e.dma_start(out=gain[:], in_=self.w_mix_norm[l:l + 1, 512:768].partition_broadcast(128)), "sk", writes=[rk])
            self.dma("sp", lambda e: e.dma_start(out=Dbc[:], in_=self.ssm_d[l:l + 1, :].partition_broadcast(128)), "sk", writes=[rk])
            NBLK = S // 512
            xc4 = [self.tsb("sxc4", [128, 6, 512], BF16) for _ in range(2)]
            rows16 = [self.tsb("srows", [16, 512], F32) for _ in range(2)]
            arow4 = [self.tsb("sarow", [4, 512], F32) for _ in range(2)]
            rblk = self.RL("sblk", 2)
            G2 = 2
            sz = [self.tsb("ssz", [128, 256], BF16) for _ in range(G2)]
            rsz = self.RL("ssz", G2)
            TS = [self.tsb("sTS", [128, 16], F32) for _ in range(G2)]
            rTS = self.RL("sTS", G2)
            tp = self.tps("stp", [128, 4, 128], BF16)
            rtp = self.RP("stp")
            XSB = [self.tsb("sXSB", [128, 4, 128], BF16) for _ in range(G2)]
            rXSB = self.RL("sXSB", G2)
            xdt = [self.tsb("sxdt", [128, 256], BF16) for _ in range(G2)]
            xw = [self.tsb("sxw", [128, 256], BF16) for _ in range(G2)]
            rxd = self.RL("sxd", G2)
            scp = [self.tps("sscp", [128, 2, 128], F32) for _ in range(G2)]
            rscp = self.RLP("sscp", G2)
            Rp = [self.tps("sRp", [128, 128], F32) for _ in range(2)]
            rRp = self.RLP("sRp", 2)
            NR = 8
            seg = [self.tsb("sseg", [128, 128], F32) for _ in range(NR)]
            rseg = self.RL("sseg", NR)
            Mt = [self.tsb("sMt", [128, 128], BF16) for _ in range(NR)]
            rMt = self.RL("sMt", NR)
            Eb = [self.tsb("sEb", [128, 128], BF16) for _ in range(NR)]
            rEb = self.RL("sEb", NR)
            Cp = [self.tsb("sCp", [128, 128], BF16) for _ in range(NR)]
            rCp = self.RL("sCp", NR)
            yps = [self.tps("syps", [128, 256], F32) for _ in range(G2)]
            ryps = self.RLP("syps", G2)
            hps = self.tps("shps", [128, 512], F32)
            rhps = self.RP("shps")
            Hf = self.tsb("sHf", [128, 256], F32)
            rHf = self.R("sHf")
            HTb = [self.tsb("sHTb", [128, 256], BF16) for _ in range(3)]
            rHTb = self.RL("sHTb", 3)
            yf = [self.tsb("syf", [128, 4, 64], F32) for _ in range(G2)]
            dx = [self.tsb("sdx", [128, 4, 64], F32) for _ in range(G2)]
            htmp = [self.tsb("shtmp", [128, 4, 64], F32) for _ in range(G2)]
            hss = [self.tsb("shss", [128, 4], F32) for _ in range(G2)]
            ryf = self.RL("syf", G2)
            yb = [self.tsb("syb", [128, 256], BF16) for _ in range(G2)]
            ryb = self.RL("syb", G2)
            self.op("pool", lambda e: e.memset(Hf[:], 0.0), writes=[rHf])
            self.op("pool", lambda e: e.memset(HTb[0][:], 0.0), writes=[rHTb[0]])
            cnt = [0]

            def load_blk(b):
                bi = b % 2
                t0 = b * 512
                self.dma("sp", lambda e: e.dma_start(out=xc4[bi][:], in_=self.XC[:, t0:t0 + 512].rearrange("(c p) t -> p c t", p=128)), f"sblk{bi}", writes=[rblk[bi]])
                self.dma("sp", lambda e: e.dma_start(out=rows16[bi][:], in_=self.SROW[0:16, t0:t0 + 512]), f"sblk{bi}", writes=[rblk[bi]])
                self.dma("sp", lambda e: e.dma_start(out=arow4[bi][:], in_=self.SROW[16:20, t0:t0 + 512]), f"sblk{bi}", writes=[rblk[bi]])

            def chunk(c):
                i = c % G2
                b = c // 4
                bi = b % 2
                o = (c % 4) * 128
                t0 = c * 128
                if c % 4 == 1 and b + 1 < NBLK:
                    load_blk(b + 1)
                self.dma("sp", lambda e: e.dma_start(out=sz[i][:], in_=self.SZ[t0:t0 + 128, :]), f"ssz{i}", writes=[rsz[i]])
                yield
                self.op("pe", lambda e: e.transpose(out=hps[:, 256:272], in_=rows16[bi][0:16, o:o + 128], identity=self.ident_f[0:16, 0:16]), reads=[rblk[bi], self.rC], writes=[rhps])
                self.op("act", lambda e: e.copy(out=TS[i][:], in_=hps[:, 256:272]), reads=[rhps], writes=[rTS[i]])
                yield
                for j in range(4):
                    self.op("pe", lambda e, j=j: e.transpose(out=tp[:, j, :], in_=xc4[bi][:, j, o:o + 128], identity=self.ident_b[:]), reads=[rblk[bi], self.rC], writes=[rtp])
                self.op("act", lambda e: e.copy(out=XSB[i][:], in_=tp[:]), reads=[rtp], writes=[rXSB[i]])
                yield
                xs4 = XSB[i][:, 0:2, :].rearrange("p g (r d) -> p (g r) d", d=64)
                self.op("pool", lambda e: e.tensor_tensor(out=xdt[i][:].rearrange("p (h d) -> p h d", d=64), in0=xs4, in1=TS[i][:, 4:8].unsqueeze(2).to_broadcast([128, 4, 64]), op=ALU.mult),
                        reads=[rXSB[i], rTS[i]], writes=[rxd[i]])
                yield
                self.op("pool", lambda e: e.tensor_tensor(out=xw[i][:].rearrange("p (h d) -> p h d", d=64), in0=xs4, in1=TS[i][:, 8:12].unsqueeze(2).to_broadcast([128, 4, 64]), op=ALU.mult),
                        reads=[rXSB[i], rTS[i]], writes=[rxd[i]])
                yield
                for g_ in range(2):
                    self.op("pe", lambda e, g_=g_: e.matmul(hps[:, g_ * 128:(g_ + 1) * 128], lhsT=XSB[i][:, 2 + g_, :], rhs=xw[i][:, g_ * 128:(g_ + 1) * 128], start=True, stop=True),
                            reads=[rXSB[i], rxd[i]], writes=[rhps])
                for hd in range(4):
                    self.op("dve", lambda e, hd=hd: e.scalar_tensor_tensor(out=Hf[:, hd * 64:(hd + 1) * 64], in0=Hf[:, hd * 64:(hd + 1) * 64], scalar=TS[i][:, 12 + hd:13 + hd],
                                                                           in1=hps[:, hd * 64:(hd + 1) * 64], op0=ALU.mult, op1=ALU.add), reads=[rhps, rTS[i]], writes=[rHf])
                self.op("pool", lambda e: e.tensor_copy(out=HTb[(c + 1) % 3][:], in_=Hf[:]), reads=[rHf], writes=[rHTb[(c + 1) % 3]])
                yield
                for g_ in range(2):
                    self.op("pe", lambda e, g_=g_: e.matmul(scp[i][:, g_, :], lhsT=xc4[bi][:, 2 + g_, o:o + 128], rhs=xc4[bi][:, 4 + g_, o:o + 128], start=True, stop=True),
                            reads=[rblk[bi]], writes=[rscp[i]])
                yield
                ks = []
                for hd in range(4):
                    k2 = cnt[0] % 2
                    k = cnt[0] % NR
                    cnt[0] += 1
                    ks.append((k2, k))
                    self.op("pe", lambda e, hd=hd, k2=k2: e.matmul(Rp[k2][:], lhsT=self.sel[:, hd, :], rhs=arow4[bi][:, o:o + 128], start=True, stop=True), reads=[rblk[bi], self.rC], writes=[rRp[k2]])
                    self.op("dve", lambda e, hd=hd, k2=k2, k=k: e.scalar_tensor_tensor(out=seg[k][:], in0=Rp[k2][:], scalar=TS[i][:, hd:hd + 1], in1=self.maskb[:], op0=ALU.add, op1=ALU.add),
                            reads=[rRp[k2], rTS[i], self.rC], writes=[rseg[k]])
                    self.op("act", lambda e, k2=k2, k=k: e.activation(out=Eb[k][:], in_=Rp[k2][:], func=AF.Exp), reads=[rRp[k2]], writes=[rEb[k]])
                yield
                for hd in range(4):
                    g_ = hd // 2
                    k2, k = ks[hd]
                    self.op("act", lambda e, k=k: e.activation(out=seg[k][:], in_=seg[k][:], func=AF.Exp), reads=[rseg[k]], writes=[rseg[k]])
                    self.op("pool", lambda e, k=k, g_=g_: e.tensor_tensor(out=Cp[k][:], in0=xc4[bi][:, 4 + g_, o:o + 128], in1=Eb[k][:], op=ALU.mult), reads=[rblk[bi], rEb[k]], writes=[rCp[k]])
                yield
                for hd in range(4):
                    g_ = hd // 2
                    k2, k = ks[hd]
                    self.op("dve", lambda e, k=k, g_=g_: e.tensor_tensor(out=Mt[k][:], in0=scp[i][:, g_, :], in1=seg[k][:], op=ALU.mult), reads=[rscp[i], rseg[k]], writes=[rMt[k]])
                yield
                for hd in range(4):
                    k2, k = ks[hd]
                    self.op("pe", lambda e, hd=hd, k=k: e.matmul(yps[i][:, hd * 64:(hd + 1) * 64], lhsT=Mt[k][:], rhs=xdt[i][:, hd * 64:(hd + 1) * 64], start=True, stop=False),
                            reads=[rMt[k], rxd[i]], writes=[ryps[i]])
                    self.op("pe", lambda e, hd=hd, k=k: e.matmul(yps[i][:, hd * 64:(hd + 1) * 64], lhsT=Cp[k][:], rhs=HTb[c % 3][:, hd * 64:(hd + 1) * 64], start=False, stop=True),
                            reads=[rCp[k], rHTb[c % 3]], writes=[ryps[i]])
                yield
                self.op("pool", lambda e: e.tensor_tensor(out=dx[i][:], in0=xs4, in1=Dbc[:].unsqueeze(2).to_broadcast([128, 4, 64]), op=ALU.mult), reads=[rXSB[i], rk], writes=[ryf[i]])
                yield
                self.op("dve", lambda e: e.tensor_tensor(out=yf[i][:], in0=yps[i][:].rearrange("p (h d) -> p h d", d=64), in1=dx[i][:], op=ALU.add), reads=[ryps[i], ryf[i]], writes=[ryf[i]])
                yield
                self.op("pool", lambda e: e.tensor_tensor(out=yf[i][:], in0=yf[i][:], in1=sz[i][:].rearrange("p (h d) -> p h d", d=64), op=ALU.mult), reads=[rsz[i], ryf[i]], writes=[ryf[i]])
                yield
                self.head_rms("pool", yf[i][:], 4, gain[:].rearrange("p (h d) -> p h d", d=64), yb[i][:].rearrange("p (h d) -> p h d", d=64), htmp[i][:], hss[i][:],
                              reads=[ryf[i], rk], writes=[ryb[i]])
                self.dma("sp", lambda e: e.dma_start(out=self.Y[t0:t0 + 128, 512:768], in_=yb[i][:]), f"syb{i}", reads=[ryb[i]])
                yield

            load_blk(0)
            self.run_window(chunk, NT, G=PIPE_G, skew=9)

    def phase_mlstm(self, l):
        S, NT = self.S, self.NT
        TB = self.TBK
        CPB = TB // 128
        NBK = S // TB
        with self.phase():
            ig = self.tsb("mig", [4, TB], F32)
            lf = self.tsb("mlf", [4, TB], F32)
            t1 = self.tsb("mt1", [4, TB], F32)
            t2 = self.tsb("mt2", [4, TB], F32)
            rm = self.tsb("mrm", [4, TB], F32)
            rneg = self.tsb("mrneg", [4, TB], F32)
            ib = self.tsb("mib", [4, 1], F32)
            fb = self.tsb("mfb", [4, 1], F32)
            AE = self.tsb("mAE", [4, NT], F32)
            CML = self.tsb("mCML", [4, NT], F32)
            MA = self.tsb("mMA", [4, NT], F32)
            MIN = self.tsb("mMIN", [4, NT], F32)
            Q = self.tsb("mQ", [4, NT], F32)
            SCL = self.tsb("mSCL", [4, NT], F32)
            rg = self.R("mgates")
            self.load_col(ib[:], self.mlstm_i_bias[l:l + 1, :], "mgt", rg)
            self.load_col(fb[:], self.mlstm_f_bias[l:l + 1, :], "mgt", rg)
            self.op("pool", lambda e: e.memset(rm[:], 1.0), writes=[rg])
            self.op("pool", lambda e: e.memset(rm[:].rearrange("p (n c) -> p n c", c=128)[:, :, 0:1], 0.0), reads=[rg], writes=[rg])
            self.op("pool", lambda e: e.memset(rneg[:], 0.0), writes=[rg])
            self.op("pool", lambda e: e.memset(rneg[:].rearrange("p (n c) -> p n c", c=128)[:, :, 0:1], NEG), reads=[rg], writes=[rg])
            for blk in range(NBK):
                t0 = blk * TB
                self.dma("sp", lambda e, t0=t0: e.dma_start(out=ig[:], in_=self.GR[8:12, t0:t0 + TB]), "mgt", writes=[rg])
                self.dma("sp", lambda e, t0=t0: e.dma_start(out=lf[:], in_=self.GR[12:16, t0:t0 + TB]), "mgt", writes=[rg])
                self.op("dve", lambda e: e.tensor_scalar(out=ig[:], in0=ig[:], scalar1=ib[:, 0:1], scalar2=None, op0=ALU.add), reads=[rg], writes=[rg])
                self.op("dve", lambda e: e.tensor_scalar(out=lf[:], in0=lf[:], scalar1=fb[:, 0:1], scalar2=None, op0=ALU.add), reads=[rg], writes=[rg])
                self.logsigmoid_rows(lf[:], t1[:], t2[:], rg)
                self.op("dve", lambda e: e.tensor_tensor_scan(out=t1[:], data0=rm[:], data1=lf[:], initial=0.0, op0=ALU.mult, op1=ALU.add), reads=[rg], writes=[rg])
                self.op("dve", lambda e: e.tensor_tensor(out=ig[:], in0=ig[:], in1=t1[:], op=ALU.subtract), reads=[rg], writes=[rg])
                self.op("dve", lambda e: e.tensor_tensor_scan(out=t2[:], data0=rneg[:], data1=ig[:], initial=NEG, op0=ALU.add, op1=ALU.max), reads=[rg], writes=[rg])
                self.op("dve", lambda e, blk=blk: e.tensor_copy(out=AE[:, blk * CPB:(blk + 1) * CPB], in_=t1[:].rearrange("p (n c) -> p n c", c=128)[:, :, 127]), reads=[rg], writes=[rg])
                self.op("dve", lambda e, blk=blk: e.tensor_copy(out=CML[:, blk * CPB:(blk + 1) * CPB], in_=t2[:].rearrange("p (n c) -> p n c", c=128)[:, :, 127]), reads=[rg], writes=[rg])
                self.dma("sp", lambda e, t0=t0: e.dma_start(out=self.MROW[0:4, t0:t0 + TB], in_=t1[:]), "mgt", reads=[rg], writes=[rg])
                self.dma("sp", lambda e, t0=t0: e.dma_start(out=self.MROW[4:8, t0:t0 + TB], in_=ig[:]), "mgt", reads=[rg], writes=[rg])
                self.dma("sp", lambda e, t0=t0: e.dma_start(out=self.MROW[8:12, t0:t0 + TB], in_=t2[:]), "mgt", reads=[rg], writes=[rg])
            self.op("dve", lambda e: e.tensor_tensor_scan(out=MA[:], data0=CML[:], data1=AE[:], initial=NEG, op0=ALU.max, op1=ALU.add), reads=[rg], writes=[rg])
            self.op("pool", lambda e: e.memset(MIN[:, 0:1], NEG), reads=[rg], writes=[rg])
            if NT > 1:
                self.op("dve", lambda e: e.tensor_copy(out=MIN[:, 1:NT], in_=MA[:, 0:NT - 1]), reads=[rg], writes=[rg])
            self.op("dve", lambda e: e.tensor_tensor(out=Q[:], in0=AE[:], in1=MA[:], op=ALU.subtract), reads=[rg], writes=[rg])
            self.op("dve", lambda e: e.tensor_tensor(out=SCL[:], in0=MIN[:], in1=Q[:], op=ALU.add), reads=[rg], writes=[rg])
            self.dma("sp", lambda e: e.dma_start(out=self.MSCL[:, :], in_=SCL[:]), "mgt", reads=[rg], writes=[rg])
            for blk in range(NBK):
                t0 = blk * TB
                a_, b_, cm_ = t1, ig, t2
                self.dma("sp", lambda e, t0=t0: e.dma_start(out=a_[:], in_=self.MROW[0:4, t0:t0 + TB]), "mgt", reads=[rg], writes=[rg])
                self.dma("sp", lambda e, t0=t0: e.dma_start(out=b_[:], in_=self.MROW[4:8, t0:t0 + TB]), "mgt", reads=[rg], writes=[rg])
                self.dma("sp", lambda e, t0=t0: e.dma_start(out=cm_[:], in_=self.MROW[8:12, t0:t0 + TB]), "mgt", reads=[rg], writes=[rg])
                v3 = lambda t: t[:].rearrange("p (n c) -> p n c", c=128)
                bc = lambda t: t[:, blk * CPB:(blk + 1) * CPB].unsqueeze(2).to_broadcast([4, CPB, 128])
                self.op("dve", lambda e, blk=blk: e.tensor_tensor(out=v3(cm_), in0=v3(cm_), in1=MIN[:, blk * CPB:(blk + 1) * CPB].unsqueeze(2).to_broadcast([4, CPB, 128]), op=ALU.max),
                        reads=[rg], writes=[rg])
                self.op("dve", lambda e, blk=blk: e.tensor_tensor(out=v3(lf), in0=MIN[:, blk * CPB:(blk + 1) * CPB].unsqueeze(2).to_broadcast([4, CPB, 128]), in1=v3(cm_), op=ALU.subtract),
                        reads=[rg], writes=[rg])
                self.dma("sp", lambda e, t0=t0: e.dma_start(out=self.MROW[16:20, t0:t0 + TB], in_=lf[:]), "mgt", reads=[rg], writes=[rg])
                self.op("dve", lambda e: e.tensor_tensor(out=a_[:], in0=a_[:], in1=cm_[:], op=ALU.add), reads=[rg], writes=[rg])
                self.op("dve", lambda e: e.tensor_scalar(out=a_[:], in0=a_[:], scalar1=-1.0, scalar2=None, op0=ALU.mult), reads=[rg], writes=[rg])
                self.op("act", lambda e: e.activation(out=a_[:], in_=a_[:], func=AF.Exp), reads=[rg], writes=[rg])
                self.dma("sp", lambda e, t0=t0: e.dma_start(out=self.MROW[0:4, t0:t0 + TB], in_=a_[:]), "mgt", reads=[rg], writes=[rg])
                self.op("dve", lambda e: e.tensor_scalar(out=cm_[:], in0=cm_[:], scalar1=-1.0, scalar2=None, op0=ALU.mult), reads=[rg], writes=[rg])
                self.dma("sp", lambda e, t0=t0: e.dma_start(out=self.MROW[12:16, t0:t0 + TB], in_=cm_[:]), "mgt", reads=[rg], writes=[rg])
                self.op("dve", lambda e, blk=blk: e.tensor_tensor(out=v3(lf), in0=v3(b_), in1=Q[:, blk * CPB:(blk + 1) * CPB].unsqueeze(2).to_broadcast([4, CPB, 128]), op=ALU.add),
                        reads=[rg], writes=[rg])
                self.op("act", lambda e: e.activation(out=lf[:], in_=lf[:], func=AF.Exp), reads=[rg], writes=[rg])
                self.dma("sp", lambda e, t0=t0: e.dma_start(out=self.MROW[8:12, t0:t0 + TB], in_=lf[:]), "mgt", reads=[rg], writes=[rg])
        with self.phase():
            gain = self.tsb("mgain", [128, 256], F32)
            SCB = self.tsb("mSCB", [64, 4, NT], F32)
            rk = self.R("mconst")
            self.dma("sp", lambda e: e.dma_start(out=gain[:], in_=self.w_mix_norm[l:l + 1, 768:1024].partition_broadcast(128)), "mk", writes=[rk])
            for hd in range(4):
                self.dma("sp", lambda e, hd=hd: e.dma_start(out=SCB[:, hd, :], in_=self.MSCL[hd:hd + 1, :].partition_broadcast(64)), "mk", writes=[rk])
            self.op("act", lambda e: e.activation(out=SCB[:], in_=SCB[:], func=AF.Exp), reads=[rk], writes=[rk])
            NBLK = S // 512
            G2 = 2
            mqt4 = [self.tsb("mmqt4", [64, 4, 512], BF16) for _ in range(2)]
            mkt4 = [self.tsb("mmkt4", [64, 4, 512], BF16) for _ in range(2)]
            rows12 = [self.tsb("mrows", [12, 512], F32) for _ in range(2)]
            nmm4 = [self.tsb("mnmm4", [4, 512], F32) for _ in range(2)]
            li4 = [self.tsb("mli4", [4, 512], F32) for _ in range(2)]
            rblk = self.RL("mblk", 2)
            mk = [self.tsb("mmk", [128, 256], BF16) for _ in range(G2)]
            mo = [self.tsb("mmo", [128, 256], BF16) for _ in range(G2)]
            VAUG = [self.tsb("mVAUG", [128, 4, 65], BF16) for _ in range(G2)]
            rin = self.RL("min", G2)
            for i in range(G2):
                self.op("pool", lambda e, i=i: e.memset(VAUG[i][:, :, 64:65], 1.0), writes=[rin[i]])
            TS = [self.tsb("mTS", [128, 12], F32) for _ in range(G2)]
            rTS = self.RL("mTS", G2)
            vw = [self.tsb("mvw", [128, 4, 65], BF16) for _ in range(G2)]
            rvw = self.RL("mvw", G2)
            qk = [self.tps("mqk", [128, 128], F32) for _ in range(2)]
            rqk = self.RLP("mqk", 2)
            Rp = [self.tps("mRp", [128, 128], F32) for _ in range(2)]
            rRp = self.RLP("mRp", 2)
            RI = self.tps("mRI", [64, 128], F32)
            rRI = self.RP("mRI")
            NR = 8
            seg = [self.tsb("mseg", [128, 128], F32) for _ in range(NR)]
            rseg = self.RL("mseg", NR)
            Wt = [self.tsb("mWt", [128, 128], BF16) for _ in range(NR)]
            rWt = self.RL("mWt", NR)
            IR = [self.tsb("mIR", [64, 128], F32) for _ in range(NR)]
            rIR = self.RL("mIR", NR)
            qp = [self.tsb("mqp", [64, 128], BF16) for _ in range(NR)]
            rqp = self.RL("mqp", NR)
            nps = [self.tps("mnps", [128, 4, 65], F32) for _ in range(G2)]
            rnps = self.RLP("mnps", G2)
            cps = self.tps("mcps", [128, 512], F32)
            rcps = self.RP("mcps")
            cpsv = cps[0:64, 0:260].rearrange("p (h d) -> p h d", d=65)
            Cf = self.tsb("mCf", [64, 4, 65], F32)
            rCf = self.R("mCf")
            CTb = [self.tsb("mCTb", [64, 4, 65], BF16) for _ in range(3)]
            rCTb = self.RL("mCTb", 3)
            dd = [self.tsb("mdd", [128, 4], F32) for _ in range(G2)]
            hn = [self.tsb("mhn", [128, 4, 64], F32) for _ in range(G2)]
            hn2 = [self.tsb("mhn2", [128, 4, 64], F32) for _ in range(G2)]
            htmp = [self.tsb("mhtmp", [128, 4, 64], F32) for _ in range(G2)]
            hss = [self.tsb("mhss", [128, 4], F32) for _ in range(G2)]
            rhn = self.RL("mhn", G2)
            yb = [self.tsb("myb", [128, 256], BF16) for _ in range(G2)]
            ryb = self.RL("myb", G2)
            self.op("pool", lambda e: e.memset(Cf[:], 0.0), writes=[rCf])
            self.op("pool", lambda e: e.memset(CTb[0][:], 0.0), writes=[rCTb[0]])
            cnt = [0]

            def load_blk(b):
                bi = b % 2
                t0 = b * 512
                self.dma("sp", lambda e: e.dma_start(out=mqt4[bi][:], in_=self.MQT[:, t0:t0 + 512].rearrange("(h d) t -> d h t", d=64)), f"mblk{bi}", writes=[rblk[bi]])
                self.dma("sp", lambda e: e.dma_start(out=mkt4[bi][:], in_=self.MKT[:, t0:t0 + 512].rearrange("(h d) t -> d h t", d=64)), f"mblk{bi}", writes=[rblk[bi]])
                self.dma("sp", lambda e: e.dma_start(out=rows12[bi][:], in_=self.MROW[0:12, t0:t0 + 512]), f"mblk{bi}", writes=[rblk[bi]])
                self.dma("sp", lambda e: e.dma_start(out=nmm4[bi][:], in_=self.MROW[12:16, t0:t0 + 512]), f"mblk{bi}", writes=[rblk[bi]])
                self.dma("sp", lambda e: e.dma_start(out=li4[bi][:], in_=self.MROW[16:20, t0:t0 + 512]), f"mblk{bi}", writes=[rblk[bi]])

            def chunk(c):
                i = c % G2
                b = c // 4
                bi = b % 2
                o = (c % 4) * 128
                t0 = c * 128
                if c % 4 == 1 and b + 1 < NBLK:
                    load_blk(b + 1)
                self.dma("sp", lambda e: e.dma_start(out=mk[i][:], in_=self.MK[t0:t0 + 128, :]), f"min{i}", writes=[rin[i]])
                self.dma("sp", lambda e: e.dma_start(out=mo[i][:], in_=self.MO[t0:t0 + 128, :]), f"min{i}", writes=[rin[i]])
                self.dma("sp", lambda e: e.dma_start(out=VAUG[i][:, :, 0:64], in_=self.MV[t0:t0 + 128, :].rearrange("t (h d) -> t h d", d=64)), f"min{i}", writes=[rin[i]])
                yield
                self.op("pe", lambda e: e.transpose(out=cps[:, 384:396], in_=rows12[bi][0:12, o:o + 128], identity=self.ident_f[0:12, 0:12]), reads=[rblk[bi], self.rC], writes=[rcps])
                self.op("act", lambda e: e.copy(out=TS[i][:], in_=cps[:, 384:396]), reads=[rcps], writes=[rTS[i]])
                yield
                self.op("pool", lambda e: e.tensor_tensor(out=vw[i][:], in0=VAUG[i][:], in1=TS[i][:, 8:12].unsqueeze(2).to_broadcast([128, 4, 65]), op=ALU.mult),
                        reads=[rin[i], rTS[i]], writes=[rvw[i]])
                yield
                for hd in range(4):
                    self.op("pe", lambda e, hd=hd: e.matmul(cpsv[:, hd, :], lhsT=mk[i][:, hd * 64:(hd + 1) * 64], rhs=vw[i][:, hd, :], start=True, stop=True),
                            reads=[rin[i], rvw[i]], writes=[rcps])
                for hd in range(4):
                    self.op("dve", lambda e, hd=hd: e.scalar_tensor_tensor(out=Cf[:, hd, :], in0=Cf[:, hd, :], scalar=SCB[:, hd, c:c + 1], in1=cpsv[:, hd, :], op0=ALU.mult, op1=ALU.add),
                            reads=[rcps, rk], writes=[rCf])
                self.op("pool", lambda e: e.tensor_copy(out=CTb[(c + 1) % 3][:], in_=Cf[:]), reads=[rCf], writes=[rCTb[(c + 1) % 3]])
                yield
                ks = []
                for hd in range(4):
                    k2 = cnt[0] % 2
                    k = cnt[0] % NR
                    cnt[0] += 1
                    ks.append((k2, k))
                    self.op("pe", lambda e, hd=hd, k2=k2: e.matmul(Rp[k2][:], lhsT=self.sel[:, hd, :], rhs=nmm4[bi][:, o:o + 128], start=True, stop=True), reads=[rblk[bi], self.rC], writes=[rRp[k2]])
                    self.op("dve", lambda e, hd=hd, k2=k2, k=k: e.scalar_tensor_tensor(out=seg[k][:], in0=Rp[k2][:], scalar=TS[i][:, 4 + hd:5 + hd], in1=self.maskb[:], op0=ALU.add, op1=ALU.add),
                            reads=[rRp[k2], rTS[i], self.rC], writes=[rseg[k]])
                    self.op("pe", lambda e, hd=hd: e.matmul(RI[:], lhsT=self.sel[:, hd, 0:64], rhs=li4[bi][:, o:o + 128], start=True, stop=True), reads=[rblk[bi], self.rC], writes=[rRI])
                    self.op("act", lambda e, k=k: e.activation(out=IR[k][:], in_=RI[:], func=AF.Exp), reads=[rRI], writes=[rIR[k]])
                yield
                for hd in range(4):
                    k2, k = ks[hd]
                    self.op("act", lambda e, k=k: e.activation(out=seg[k][:], in_=seg[k][:], func=AF.Exp), reads=[rseg[k]], writes=[rseg[k]])
                    self.op("pool", lambda e, hd=hd, k=k: e.tensor_tensor(out=qp[k][:], in0=mqt4[bi][:, hd, o:o + 128], in1=IR[k][:], op=ALU.mult), reads=[rblk[bi], rIR[k]], writes=[rqp[k]])
                yield
                for hd in range(4):
                    k2, k = ks[hd]
                    self.op("pe", lambda e, hd=hd, k2=k2: e.matmul(qk[k2][:], lhsT=mkt4[bi][:, hd, o:o + 128], rhs=mqt4[bi][:, hd, o:o + 128], start=True, stop=True), reads=[rblk[bi]], writes=[rqk[k2]])
                    self.op("dve", lambda e, k2=k2, k=k: e.tensor_tensor(out=Wt[k][:], in0=qk[k2][:], in1=seg[k][:], op=ALU.mult), reads=[rqk[k2], rseg[k]], writes=[rWt[k]])
                yield
                for hd in range(4):
                    k2, k = ks[hd]
                    self.op("pe", lambda e, hd=hd, k=k: e.matmul(nps[i][:, hd, :], lhsT=Wt[k][:], rhs=VAUG[i][:, hd, :], start=True, stop=False), reads=[rWt[k], rin[i]], writes=[rnps[i]])
                    self.op("pe", lambda e, hd=hd, k=k: e.matmul(nps[i][:, hd, :], lhsT=qp[k][:], rhs=CTb[c % 3][:, hd, :], start=False, stop=True), reads=[rqp[k], rCTb[c % 3]], writes=[rnps[i]])
                yield
                self.op("dve", lambda e: e.tensor_copy(out=hss[i][:], in_=nps[i][:, :, 64]), reads=[rnps[i]], writes=[rhn[i]])
                self.op("dve", lambda e: e.scalar_tensor_tensor(out=dd[i][:], in0=hss[i][:], scalar=-1.0, in1=hss[i][:], op0=ALU.mult, op1=ALU.max), reads=[rhn[i]], writes=[rhn[i]])
                yield
                self.op("dve", lambda e: e.tensor_tensor(out=dd[i][:], in0=dd[i][:], in1=TS[i][:, 0:4], op=ALU.max), reads=[rTS[i], rhn[i]], writes=[rhn[i]])
                self.op("dve", lambda e: e.reciprocal(out=dd[i][:], in_=dd[i][:]), reads=[rhn[i]], writes=[rhn[i]])
                yield
                self.op("dve", lambda e: e.tensor_tensor(out=hn[i][:], in0=nps[i][:, :, 0:64], in1=dd[i][:].unsqueeze(2).to_broadcast([128, 4, 64]), op=ALU.mult), reads=[rnps[i], rhn[i]], writes=[rhn[i]])
                yield
                self.head_rms("pool", hn[i][:], 4, gain[:].rearrange("p (h d) -> p h d", d=64), hn2[i][:], htmp[i][:], hss[i][:], reads=[rhn[i], rk], writes=[rhn[i]])
                yield
                self.op("pool", lambda e: e.tensor_tensor(out=yb[i][:].rearrange("p (h d) -> p h d", d=64), in0=hn2[i][:], in1=mo[i][:].rearrange("p (h d) -> p h d", d=64), op=ALU.mult),
                        reads=[rhn[i], rin[i]], writes=[ryb[i]])
                self.dma("sp", lambda e: e.dma_start(out=self.Y[t0:t0 + 128, 768:1024], in_=yb[i][:]), f"myb{i}", reads=[ryb[i]])
                yield

            load_blk(0)
            self.run_window(chunk, NT, G=PIPE_G, skew=8)

    def phase_c(self, l):
        S, NT = self.S, self.NT
        B, NB, LB = self.MB, self.NB, self.LB
        BIG = 1.0e30
        with self.phase():
            WO = self.tsb("cWO", [128, 8, D], BF16)
            wst = [self.tsb("cwst", [128, 8, 256], F32) for _ in range(2)]
            rwst = self.RL("cwst", 2)
            rW = self.R("cW")
            for pi in range(4):
                i = pi % 2
                self.dma("sp", lambda e, pi=pi, i=i: e.dma_start(out=wst[i][:], in_=self.w_out[l, :, pi * 256:(pi + 1) * 256].rearrange("(k p) n -> p k n", p=128)), f"cwst{i}", writes=[rwst[i]])
                self.op("dve" if i == 0 else "pool", lambda e, pi=pi, i=i: e.tensor_copy(out=WO[:, :, pi * 256:(pi + 1) * 256], in_=wst[i][:]), reads=[rwst[i]], writes=[rW])
            WR = self.tsb("cWR", [128, 8, 36], F32)
            RB = self.tsb("cRB", [128, 36], F32)
            self.dma("sp", lambda e: e.dma_start(out=WR[:, :, 0:4], in_=self.w_rg[l, :, :].rearrange("(k p) n -> p k n", p=128), allow_slow_non_contiguous=True), "cw", writes=[rW])
            self.dma("sp", lambda e: e.dma_start(out=WR[:, :, 4:36], in_=self.w_re[l, :, :].rearrange("(k p) n -> p k n", p=128), allow_slow_non_contiguous=True), "cw", writes=[rW])
            self.dma("sp", lambda e: e.dma_start(out=RB[:, 0:4], in_=self.b_rg[l:l + 1, :].partition_broadcast(128)), "cw", writes=[rW])
            self.dma("sp", lambda e: e.dma_start(out=RB[:, 4:36], in_=self.b_re[l:l + 1, :].partition_broadcast(128)), "cw", writes=[rW])
            LG = self.tsb("cLG", [128, NT, 36], F32)
            rLG = self.R("cLG")
            yt = [self.tsb("cyt", [128, D], BF16) for _ in range(2)]
            xt = [self.tsb("cxt", [128, D], F32) for _ in range(2)]
            rin = self.RL("cin", 2)
            pT = [self.tps("cpT", [128, 4, 128], BF16) for _ in range(2)]
            rpT = self.RLP("cpT", 2)
            yT = [self.tsb("cyT", [128, 8, 128], BF16) for _ in range(2)]
            ryT = self.RL("cyT", 2)
            pm = [self.tps("cpm", [128, 512], F32) for _ in range(2)]
            rpm = self.RLP("cpm", 2)
            x1 = [self.tsb("cx1", [128, D], F32) for _ in range(2)]
            rx1 = self.RL("cx1", 2)
            ss = [self.tsb("css", [128, 1], F32) for _ in range(2)]
            h2f = [self.tsb("ch2f", [128, D], F32) for _ in range(2)]
            rh2f = self.RL("ch2f", 2)
            h2b = [self.tsb("ch2b", [128, D], BF16) for _ in range(2)]
            rh2b = self.RL("ch2b", 2)
            pTf = [self.tps("cpTf", [128, 4, 128], F32) for _ in range(2)]
            rpTf = self.RLP("cpTf", 2)

            pl = self.tps("cpl", [128, 36], F32)
            rpl = self.RP("cpl")
            rsq = self.R("csq")
            h2T2 = [self.tsb("ch2T2", [128, 8, 128], F32) for _ in range(2)]
            rh2T2 = self.RL("ch2T2", 2)
            sq2 = [self.tsb("csq2", [128, D], BF16) for _ in range(2)]
            rsq2 = self.RL("csq2", 2)

            def load_c(ti):
                i = ti % 2
                t0 = ti * 128
                self.dma("sp", lambda e: e.dma_start(out=yt[i][:], in_=self.Y[t0:t0 + 128, :]), f"cin{i}", writes=[rin[i]])
                self.dma("sp", lambda e: e.dma_start(out=xt[i][:], in_=self.X[t0:t0 + 128, :]), f"cin{i}", writes=[rin[i]])

            def tile_c(ti):
                i = ti % 2
                t0 = ti * 128
                load_c(ti)
                yield
                for half in range(2):
                    for j in range(4):
                        kc = half * 4 + j
                        self.op("pe", lambda e, half=half, j=j, kc=kc: e.transpose(out=pT[half][:, j, :], in_=yt[i][:, kc * 128:(kc + 1) * 128], identity=self.ident_b[:]),
                                reads=[rin[i], self.rC], writes=[rpT[half]])
                    if half == 0:
                        self.op("act", lambda e: e.copy(out=yT[i][:, 0:4, :], in_=pT[0][:]), reads=[rpT[0]], writes=[ryT[i]])
                    else:
                        self.op("dve", lambda e: e.tensor_copy(out=yT[i][:, 4:8, :], in_=pT[1][:]), reads=[rpT[1]], writes=[ryT[i]])
                    yield
                for half in range(2):
                    for kc in range(8):
                        self.op("pe", lambda e, half=half, kc=kc: e.matmul(pm[half][:], lhsT=yT[i][:, kc, :], rhs=WO[:, kc, half * 512:(half + 1) * 512], start=(kc == 0), stop=(kc == 7)),
                                reads=[ryT[i], rW], writes=[rpm[half]])
                    self.op("dve", lambda e, half=half: e.tensor_tensor(out=x1[i][:, half * 512:(half + 1) * 512], in0=pm[half][:], in1=self.MOD[:, 2 * D + half * 512:2 * D + (half + 1) * 512], op=ALU.mult),
                            reads=[rpm[half], self.rMOD], writes=[rx1[i]])
                    yield
                self.op("pool", lambda e: e.tensor_tensor(out=x1[i][:], in0=x1[i][:], in1=xt[i][:], op=ALU.add), reads=[rin[i], rx1[i]], writes=[rx1[i]])
                self.dma("sp", lambda e: e.dma_start(out=self.X[t0:t0 + 128, :], in_=x1[i][:]), f"cx1{i}", reads=[rx1[i]])
                yield
                self.op("pool", lambda e: e.memset(ss[i][:], 0.0), writes=[rsq2[i]])
                self.op("act", lambda e: e.activation(out=sq2[i][:], in_=x1[i][:], func=AF.Square, accum_out=ss[i][:]), reads=[rx1[i]], writes=[rsq2[i]])
                yield
                self.op("act", lambda e: e.activation(out=ss[i][:], in_=ss[i][:], func=AF.Sqrt, bias=EPS, scale=1.0 / D), reads=[rsq2[i]], writes=[rsq2[i]])
                self.op("dve", lambda e: e.reciprocal(out=ss[i][:], in_=ss[i][:]), reads=[rsq2[i]], writes=[rsq2[i]])
                yield
                self.op("dve", lambda e: e.scalar_tensor_tensor(out=h2f[i][:], in0=x1[i][:], scalar=ss[i][:, 0:1], in1=self.MOD[:, 4 * D:5 * D], op0=ALU.mult, op1=ALU.mult),
                        reads=[rx1[i], rsq2[i], self.rMOD], writes=[rh2f[i]])
                yield
                self.op("pool", lambda e: e.tensor_tensor(out=h2f[i][:], in0=h2f[i][:], in1=self.MOD[:, 3 * D:4 * D], op=ALU.add), reads=[rh2f[i], self.rMOD], writes=[rh2f[i]])
                yield
                self.op("act", lambda e: e.copy(out=h2b[i][:], in_=h2f[i][:]), reads=[rh2f[i]], writes=[rh2b[i]])
                self.dma("sp", lambda e: e.dma_start(out=self.HS[t0:t0 + 128, :], in_=h2b[i][:]), f"ch2b{i}", reads=[rh2b[i]])
                yield
                for half in range(2):
                    for j in range(4):
                        kc = half * 4 + j
                        self.op("pe", lambda e, half=half, j=j, kc=kc: e.transpose(out=pTf[half][:, j, :], in_=h2f[i][:, kc * 128:(kc + 1) * 128], identity=self.ident_f[:]),
                                reads=[rh2f[i], self.rC], writes=[rpTf[half]])
                    if half == 0:
                        self.op("act", lambda e: e.copy(out=h2T2[i][:, 0:4, :], in_=pTf[0][:]), reads=[rpTf[0]], writes=[rh2T2[i]])
                    else:
                        self.op("dve", lambda e: e.tensor_copy(out=h2T2[i][:, 4:8, :], in_=pTf[1][:]), reads=[rpTf[1]], writes=[rh2T2[i]])
                    yield
                for kc in range(8):
                    self.op("pe", lambda e, kc=kc: e.matmul(pl[:], lhsT=h2T2[i][:, kc, :], rhs=WR[:, kc, :], start=(kc == 0), stop=(kc == 7)), reads=[rh2T2[i], rW], writes=[rpl])
                self.op("dve", lambda e: e.tensor_tensor(out=LG[:, ti, :], in0=pl[:], in1=RB[:], op=ALU.add), reads=[rpl, rW], writes=[rLG])
                yield

            self.run_window(tile_c, NT, G=PIPE_G, skew=7)
            r = rLG
            gmax = self.tsb("rgmax", [128, NT], F32)
            goh = self.tsb("rgoh", [128, NT, 4], F32)
            ge = self.tsb("rge", [128, NT, 4], F32)
            gpr = self.tsb("rgpr", [128, NT], F32)
            em = self.tsb("rem", [128, NT, 32], F32)
            OH1 = self.tsb("rOH1", [128, NT, 32], F32)
            OH2 = self.tsb("rOH2", [128, NT, 32], F32)
            t1 = self.tsb("rt1", [128, NT], F32)
            t2 = self.tsb("rt2", [128, NT], F32)
            GL = LG[:, :, 0:4]
            self.op("dve", lambda e: e.tensor_reduce(out=gmax[:], in_=GL, axis=AX.X, op=ALU.max), reads=[r], writes=[r])
            self.op("dve", lambda e: e.tensor_tensor(out=goh[:], in0=GL, in1=gmax[:].unsqueeze(2).to_broadcast([128, NT, 4]), op=ALU.is_equal), reads=[r], writes=[r])
            self.op("dve", lambda e: e.tensor_tensor(out=ge[:], in0=GL, in1=gmax[:].unsqueeze(2).to_broadcast([128, NT, 4]), op=ALU.subtract), reads=[r], writes=[r])
            self.op("act", lambda e: e.activation(out=ge[:], in_=ge[:], func=AF.Exp), reads=[r], writes=[r])
            self.op("dve", lambda e: e.tensor_reduce(out=gpr[:], in_=ge[:], axis=AX.X, op=ALU.add), reads=[r], writes=[r])
            self.op("dve", lambda e: e.reciprocal(out=gpr[:], in_=gpr[:]), reads=[r], writes=[r])
            self.op("dve", lambda e: e.tensor_scalar(out=goh[:], in0=goh[:], scalar1=BIG, scalar2=-BIG, op0=ALU.mult, op1=ALU.add), reads=[r], writes=[r])
            self.op("dve", lambda e: e.tensor_tensor(out=em[:].rearrange("p n (g j) -> p n g j", j=8), in0=LG[:, :, 4:36].rearrange("p n (g j) -> p n g j", j=8),
                                                     in1=goh[:].unsqueeze(3).to_broadcast([128, NT, 4, 8]), op=ALU.add), reads=[r], writes=[r])
            self.op("dve", lambda e: e.tensor_reduce(out=t1[:], in_=em[:], axis=AX.X, op=ALU.max), reads=[r], writes=[r])
            self.op("dve", lambda e: e.tensor_tensor(out=OH1[:], in0=em[:], in1=t1[:].unsqueeze(2).to_broadcast([128, NT, 32]), op=ALU.is_equal), reads=[r], writes=[r])
            self.op("dve", lambda e: e.scalar_tensor_tensor(out=em[:], in0=OH1[:], scalar=-BIG, in1=em[:], op0=ALU.mult, op1=ALU.add), reads=[r], writes=[r])
            self.op("dve", lambda e: e.tensor_reduce(out=t2[:], in_=em[:], axis=AX.X, op=ALU.max), reads=[r], writes=[r])
            self.op("dve", lambda e: e.tensor_tensor(out=OH2[:], in0=em[:], in1=t2[:].unsqueeze(2).to_broadcast([128, NT, 32]), op=ALU.is_equal), reads=[r], writes=[r])
            self.op("dve", lambda e: e.tensor_tensor(out=t2[:], in0=t2[:], in1=t1[:], op=ALU.subtract), reads=[r], writes=[r])
            self.op("act", lambda e: e.activation(out=t2[:], in_=t2[:], func=AF.Exp), reads=[r], writes=[r])
            self.op("dve", lambda e: e.tensor_scalar_add(out=t1[:], in0=t2[:], scalar1=1.0), reads=[r], writes=[r])
            self.op("dve", lambda e: e.reciprocal(out=t1[:], in_=t1[:]), reads=[r], writes=[r])
            self.op("dve", lambda e: e.tensor_tensor(out=t2[:], in0=t2[:], in1=t1[:], op=ALU.mult), reads=[r], writes=[r])
            self.op("dve", lambda e: e.tensor_tensor(out=self.GATES[:, :, 0], in0=t1[:], in1=gpr[:], op=ALU.mult), reads=[r], writes=[self.rPLAN])
            self.op("dve", lambda e: e.tensor_tensor(out=self.GATES[:, :, 1], in0=t2[:], in1=gpr[:], op=ALU.mult), reads=[r], writes=[self.rPLAN])
            SEL = self.tsb("rSEL", [128, NT * 32], BF16)
            RANK = self.tsb("rRANK", [128, NT, 32], F32)
            PRE = self.tsb("rPRE", [128, NT + 1, 32], F32)
            pr = pm
            rpr = rpm
            self.op("dve", lambda e: e.tensor_tensor(out=SEL[:], in0=OH1[:].rearrange("p n e -> p (n e)"), in1=OH2[:].rearrange("p n e -> p (n e)"), op=ALU.add), reads=[r], writes=[r])
            TOTt = em
            W_ = NT * 32
            for c0 in range(0, W_, 512):
                w = min(512, W_ - c0)
                self.op("pe", lambda e, c0=c0, w=w: e.matmul(pr[0][:, 0:w], lhsT=self.UTs[:], rhs=SEL[:, c0:c0 + w], start=True, stop=True), reads=[r, self.rC], writes=[rpr[0]])
                self.op("dve", lambda e, c0=c0, w=w: e.tensor_copy(out=RANK[:].rearrange("p n e -> p (n e)")[:, c0:c0 + w], in_=pr[0][:, 0:w]), reads=[rpr[0]], writes=[r])
                self.op("pe", lambda e, c0=c0, w=w: e.matmul(pr[1][:, 0:w], lhsT=self.ones_b[:], rhs=SEL[:, c0:c0 + w], start=True, stop=True), reads=[r, self.rC], writes=[rpr[1]])
                self.op("dve", lambda e, c0=c0, w=w: e.tensor_copy(out=TOTt[:].rearrange("p n e -> p (n e)")[:, c0:c0 + w], in_=pr[1][:, 0:w]), reads=[rpr[1]], writes=[r])
            self.op("pool", lambda e: e.memset(PRE[:, 0, :], 0.0), writes=[r])
            for n in range(NT):
                self.op("dve", lambda e, n=n: e.tensor_tensor(out=PRE[:, n + 1, :], in0=PRE[:, n, :], in1=TOTt[:, n, :], op=ALU.add), reads=[r], writes=[r])
            PADf = self.tsb("rPADf", [128, 32], F32)
            PADi = self.tsb("rPADi", [128, 32], I32)
            PEND = self.tsb("rPEND", [128, 32], F32)
            PST = self.tsb("rPST", [128, 32], F32)
            self.op("dve", lambda e: e.tensor_scalar_add(out=PADf[:], in0=PRE[:, NT, :], scalar1=float(B - 1)), reads=[r], writes=[r])
            self.op("dve", lambda e: e.tensor_copy(out=PADi[:], in_=PADf[:]), reads=[r], writes=[r])
            self.op("dve", lambda e: e.tensor_single_scalar(out=PADi[:], in_=PADi[:], scalar=LB, op=ALU.arith_shift_right), reads=[r], writes=[r])
            self.op("dve", lambda e: e.tensor_single_scalar(out=PADi[:], in_=PADi[:], scalar=LB, op=ALU.logical_shift_left), reads=[r], writes=[r])
            self.op("dve", lambda e: e.tensor_copy(out=PADf[:], in_=PADi[:]), reads=[r], writes=[r])
            self.op("dve", lambda e: e.tensor_tensor_scan(out=PEND[:], data0=self.ones_f[:, 0:32], data1=PADf[:], initial=0.0, op0=ALU.mult, op1=ALU.add), reads=[r, self.rC], writes=[r])
            self.op("dve", lambda e: e.tensor_tensor(out=PST[:], in0=PEND[:], in1=PADf[:], op=ALU.subtract), reads=[r], writes=[r])
            self.op("dve", lambda e: e.tensor_tensor(out=RANK[:], in0=RANK[:], in1=PRE[:, 0:NT, :], op=ALU.add), reads=[r], writes=[r])
            self.op("dve", lambda e: e.tensor_tensor(out=RANK[:], in0=RANK[:], in1=PST[:].unsqueeze(1).to_broadcast([128, NT, 32]), op=ALU.add), reads=[r], writes=[r])
            dtmp = self.tsb("rdtmp", [128, NT], F32)
            for k, OH in enumerate((OH1, OH2)):
                self.op("dve", lambda e, OH=OH: e.tensor_tensor(out=em[:], in0=OH[:], in1=RANK[:], op=ALU.mult), reads=[r], writes=[r])
                self.op("dve", lambda e: e.tensor_reduce(out=dtmp[:], in_=em[:], axis=AX.X, op=ALU.add), reads=[r], writes=[r])
                self.op("dve", lambda e, k=k: e.tensor_copy(out=self.DEST[:, :, k], in_=dtmp[:]), reads=[r], writes=[self.rPLAN])
            JBi = self.tsb("rJBi", [128, NB, 32], I32)
            JBf = self.tsb("rJBf", [128, NB, 32], F32)
            EJ = self.tsb("rEJ", [128, NB], F32)
            PKi = self.tsb("rPKi", [128, 8], I32)
            PKf = self.tsb("rPKf", [128, 8], F32)
            IXf = self.tsb("rIXf", [128, NB, 8], F32)
            self.op("pool", lambda e: e.iota(JBi[:], pattern=[[B, NB], [0, 32]], base=0, channel_multiplier=0), writes=[r])
            self.op("dve", lambda e: e.tensor_copy(out=JBf[:], in_=JBi[:]), reads=[r], writes=[r])
            self.op("dve", lambda e: e.tensor_tensor(out=JBf[:], in0=PEND[:].unsqueeze(1).to_broadcast([128, NB, 32]), in1=JBf[:], op=ALU.is_le), reads=[r], writes=[r])
            self.op("dve", lambda e: e.tensor_reduce(out=EJ[:], in_=JBf[:], axis=AX.X, op=ALU.add), reads=[r], writes=[r])
            self.op("dve", lambda e: e.tensor_scalar_min(out=EJ[:], in0=EJ[:], scalar1=31.0), reads=[r], writes=[r])
            self.op("pool", lambda e: e.iota(PKi[:], pattern=[[128, 8]], base=0, channel_multiplier=1), writes=[r])
            self.op("dve", lambda e: e.tensor_copy(out=PKf[:], in_=PKi[:]), reads=[r], writes=[r])
            self.op("dve", lambda e: e.scalar_tensor_tensor(out=IXf[:, :, 0], in0=EJ[:], scalar=128.0, in1=PKf[:, 0:1].to_broadcast([128, NB]), op0=ALU.mult, op1=ALU.add),
                    reads=[r], writes=[r])
            if l > 0:
                self.op("dve", lambda e: e.tensor_scalar_add(out=IXf[:, :, 0], in0=IXf[:, :, 0], scalar1=float(l * 4096)), reads=[r], writes=[r])
            self.op("dve", lambda e: e.tensor_copy(out=self.IDXW[:], in_=IXf[:, :, 0]), reads=[r], writes=[self.rPLAN])
            hs = [self.tsb("rhs", [128, D], BF16) for _ in range(2)]
            rhs_ = self.RL("rhs", 2)
            for ti in range(NT):
                i = ti % 2
                t0 = ti * 128
                self.dma("sp", lambda e, i=i, t0=t0: e.dma_start(out=hs[i][:], in_=self.HS[t0:t0 + 128, :]), f"rhs{i}", writes=[rhs_[i]])
                for k in range(2):
                    self.dma("pool", lambda e, i=i, ti=ti, k=k: e.indirect_dma_start(out=self.XS, out_offset=bass.IndirectOffsetOnAxis(ap=self.DEST[:, ti, k:k + 1], axis=0),
                                                                                      in_=hs[i][:], in_offset=None), f"rsc{i}", reads=[rhs_[i], self.rPLAN])

    def phase_moe(self, l):
        B, NB = self.MB, self.NB
        RT = B // 128
        with self.phase():
            WG = self.tsb("eWG", [128, 8, 512], F32)
            WU = self.tsb("eWU", [128, 8, 512], F32)
            WD = self.tsb("eWD", [128, 4, D], F32)
            rWf = self.RL("eWf", 3)
            WGb = [self.tsb("eWGb", [128, 8, 512], BF16) for _ in range(2)]
            WUb = [self.tsb("eWUb", [128, 8, 512], BF16) for _ in range(2)]
            WDb = [self.tsb("eWDb", [128, 4, D], BF16) for _ in range(2)]
            rWb = [self.RL("eWb", 3) for _ in range(2)]
            xs = [self.tsb("exs", [128, D], BF16) for _ in range(2)]
            rxs = self.RL("exs", 2)
            pT = [self.tps("epT", [128, 4, 128], BF16) for _ in range(2)]
            rpT = self.RLP("epT", 2)
            xT = self.tsb("exT", [128, 8, B], BF16)
            rxT = self.R("exT")
            pg = self.tps("epg", [128, 512], F32)
            pu = self.tps("epu", [128, 512], F32)
            rpg, rpu = self.RP("epg"), self.R("epu")
            sg = self.tsb("esg", [128, B], F32)
            rsg = self.R("esg")
            AT = self.tsb("eAT", [128, 4, B], BF16)
            rAT = self.R("eAT")
            po = [self.tps("epo", [128, 512], F32) for _ in range(2)]
            rpo = self.RLP("epo", 2)
            ob = [self.tsb("eob", [128, D], F32) for _ in range(2)]
            rob = self.RL("eob", 2)
            nx = 0
            no = 0
            xs = [self.tsb("exs2", [128, D], BF16) for _ in range(2 * RT)]
            rxs = self.RL("exs2", 2 * RT)

            def gather_w(j):
                for qi, (Wt_, src_) in enumerate(((WG, self.w_eg), (WU, self.w_eu), (WD, self.w_ed))):
                    self.dma("pool", lambda e, Wt_=Wt_, src_=src_: e.indirect_dma_start(out=Wt_[:].rearrange("p k n -> p (k n)"), out_offset=None, in_=src_,
                                                                                      in_offset=bass.IndirectOffsetOnAxis(ap=self.IDXW[:, j:j + 1], axis=0)),
                             f"eWf{qi}", reads=[self.rPLAN], writes=[rWf[qi]])

            def load_xs(j):
                for rt in range(RT):
                    i = (j % 2) * RT + rt
                    r0 = j * B + rt * 128
                    self.dma("sp", lambda e, i=i, r0=r0: e.dma_start(out=xs[i][:], in_=self.XS[r0:r0 + 128, :]), f"exs{i}", writes=[rxs[i]])

            def cast_w(j):
                w = j % 2
                self.op("dve", lambda e: e.tensor_copy(out=WGb[w][:], in_=WG[:]), reads=[rWf[0]], writes=[rWb[w][0]])
                self.op("act", lambda e: e.copy(out=WUb[w][:], in_=WU[:]), reads=[rWf[1]], writes=[rWb[w][1]])
                self.op("dve", lambda e: e.tensor_copy(out=WDb[w][:], in_=WD[:]), reads=[rWf[2]], writes=[rWb[w][2]])

            gather_w(0)
            load_xs(0)
            cast_w(0)
            if NB > 1:
                gather_w(1)
                load_xs(1)
            for j in range(NB):
                w = j % 2
                for rt in range(RT):
                    i = (j % 2) * RT + rt
                    for half in range(2):
                        for q in range(4):
                            kc = half * 4 + q
                            self.op("pe", lambda e, half=half, q=q, kc=kc, i=i: e.transpose(out=pT[half][:, q, :], in_=xs[i][:, kc * 128:(kc + 1) * 128], identity=self.ident_b[:]),
                                    reads=[rxs[i], self.rC], writes=[rpT[half]])
                        if half == 0:
                            self.op("act", lambda e, rt=rt: e.copy(out=xT[:, 0:4, rt * 128:(rt + 1) * 128], in_=pT[0][:]), reads=[rpT[0]], writes=[rxT])
                        else:
                            self.op("dve", lambda e, rt=rt: e.tensor_copy(out=xT[:, 4:8, rt * 128:(rt + 1) * 128], in_=pT[1][:]), reads=[rpT[1]], writes=[rxT])
                if j + 1 < NB:
                    cast_w(j + 1)
                if j + 2 < NB:
                    gather_w(j + 2)
                    load_xs(j + 2)
                for nch in range(4):
                    for kc in range(8):
                        self.op("pe", lambda e, nch=nch, kc=kc, w=w: e.matmul(pg[:, 0:B], lhsT=WGb[w][:, kc, nch * 128:(nch + 1) * 128], rhs=xT[:, kc, :], start=(kc == 0), stop=(kc == 7)),
                                reads=[rWb[w][0], rxT], writes=[rpg])
                    for kc in range(8):
                        self.op("pe", lambda e, nch=nch, kc=kc, w=w: e.matmul(pu[:, 0:B], lhsT=WUb[w][:, kc, nch * 128:(nch + 1) * 128], rhs=xT[:, kc, :], start=(kc == 0), stop=(kc == 7)),
                                reads=[rWb[w][1], rxT], writes=[rpu])
                    self.op("act", lambda e: e.activation(out=sg[:], in_=pg[:, 0:B], func=AF.Silu), reads=[rpg], writes=[rsg])
                    self.op("dve", lambda e, nch=nch: e.tensor_tensor(out=AT[:, nch, :], in0=pu[:, 0:B], in1=sg[:], op=ALU.mult), reads=[rpu, rsg], writes=[rAT])
                for rt in range(RT):
                    o = no % 2
                    no += 1
                    r0 = j * B + rt * 128
                    for half in range(2):
                        for nch in range(4):
                            self.op("pe", lambda e, half=half, nch=nch, rt=rt, w=w: e.matmul(po[half][:], lhsT=AT[:, nch, rt * 128:(rt + 1) * 128], rhs=WDb[w][:, nch, half * 512:(half + 1) * 512],
                                                                                               start=(nch == 0), stop=(nch == 3)), reads=[rAT, rWb[w][2]], writes=[rpo[half]])
                        if half == 0:
                            self.op("act", lambda e, o=o: e.copy(out=ob[o][:, 0:512], in_=po[0][:]), reads=[rpo[0]], writes=[rob[o]])
                        else:
                            self.op("dve", lambda e, o=o: e.tensor_copy(out=ob[o][:, 512:1024], in_=po[1][:]), reads=[rpo[1]], writes=[rob[o]])
                    self.dma("sp", lambda e, o=o, r0=r0: e.dma_start(out=self.YB[r0:r0 + 128, :], in_=ob[o][:]), f"eob{o}", reads=[rob[o]])

    def phase_comb(self, l, last):
        S, NT = self.S, self.NT
        with self.phase():
            r1 = [self.tsb("br1", [128, D], F32) for _ in range(2)]
            r2 = [self.tsb("br2", [128, D], F32) for _ in range(2)]
            xt = [self.tsb("bxt", [128, D], F32) for _ in range(2)]
            rin = self.RL("bin", 2)
            acc = [self.tsb("bacc", [128, D], F32) for _ in range(2)]
            racc = self.RL("bacc", 2)
            wf = self.tsb("bwf", [128, D], F32)
            ss = self.tsb("bss", [128, 1], F32)
            sq = self.tsb("bsq", [128, D], F32)
            rk = self.R("bk")
            if last:
                self.dma("sp", lambda e: e.dma_start(out=wf[:], in_=self.w_norm_final[0:1, :].partition_broadcast(128)), "bk", writes=[rk])
            def load_b(ti):
                i = ti % 2
                t0 = ti * 128
                self.dma("pool", lambda e: e.indirect_dma_start(out=r1[i][:], out_offset=None, in_=self.YB, in_offset=bass.IndirectOffsetOnAxis(ap=self.DEST[:, ti, 0:1], axis=0)),
                         f"bin{i}", reads=[self.rPLAN], writes=[rin[i]])
                self.dma("pool", lambda e: e.indirect_dma_start(out=r2[i][:], out_offset=None, in_=self.YB, in_offset=bass.IndirectOffsetOnAxis(ap=self.DEST[:, ti, 1:2], axis=0)),
                         f"bin{i}", reads=[self.rPLAN], writes=[rin[i]])
                self.dma("sp", lambda e: e.dma_start(out=xt[i][:], in_=self.X[t0:t0 + 128, :]), f"bin{i}", writes=[rin[i]])

            load_b(0)
            for ti in range(NT):
                i = ti % 2
                t0 = ti * 128
                if ti + 1 < NT:
                    load_b(ti + 1)
                self.op("dve", lambda e, i=i, ti=ti: e.tensor_scalar(out=acc[i][:], in0=r1[i][:], scalar1=self.GATES[:, ti, 0:1], scalar2=None, op0=ALU.mult), reads=[rin[i], self.rPLAN], writes=[racc[i]])
                self.op("dve", lambda e, i=i, ti=ti: e.scalar_tensor_tensor(out=acc[i][:], in0=r2[i][:], scalar=self.GATES[:, ti, 1:2], in1=acc[i][:], op0=ALU.mult, op1=ALU.add),
                        reads=[rin[i], self.rPLAN], writes=[racc[i]])
                self.op("pool", lambda e, i=i: e.tensor_tensor(out=acc[i][:], in0=acc[i][:], in1=self.MOD[:, 5 * D:6 * D], op=ALU.mult), reads=[self.rMOD], writes=[racc[i]])
                self.op("pool", lambda e, i=i: e.tensor_tensor(out=acc[i][:], in0=acc[i][:], in1=xt[i][:], op=ALU.add), reads=[rin[i]], writes=[racc[i]])
                if not last:
                    self.dma("sp", lambda e, i=i, t0=t0: e.dma_start(out=self.X[t0:t0 + 128, :], in_=acc[i][:]), f"bacc{i}", reads=[racc[i]])
                else:
                    if self.debug:
                        self.dma("sp", lambda e, i=i, t0=t0: e.dma_start(out=self.X[t0:t0 + 128, :], in_=acc[i][:]), f"bacc{i}", reads=[racc[i]])
                    self.op("pool", lambda e: e.memset(ss[:], 0.0), writes=[rk])
                    self.op("act", lambda e, i=i: e.activation(out=sq[:], in_=acc[i][:], func=AF.Square, accum_out=ss[:]), reads=[racc[i]], writes=[rk])
                    self.op("act", lambda e: e.activation(out=ss[:], in_=ss[:], func=AF.Sqrt, bias=EPS, scale=1.0 / D), reads=[rk], writes=[rk])
                    self.op("dve", lambda e: e.reciprocal(out=ss[:], in_=ss[:]), reads=[rk], writes=[rk])
                    self.op("dve", lambda e, i=i: e.scalar_tensor_tensor(out=acc[i][:], in0=acc[i][:], scalar=ss[:, 0:1], in1=wf[:], op0=ALU.mult, op1=ALU.mult), reads=[rk], writes=[racc[i]])
                    self.dma("sp", lambda e, i=i, t0=t0: e.dma_start(out=self.out[t0:t0 + 128, :], in_=acc[i][:]), f"bacc{i}", reads=[racc[i]])

    def build(self):
        self._alloc_es = self.es
        self.declare()
        self.consts()
        self.dma("sp", lambda e: e.dma_start(out=self.X, in_=self.x_in), "x0")
        self.barrier()
        ph = self.phases
        for l in range(self.L):
            self.adaln(l)
            if ph is None or "a" in ph:
                self.phase_a(l)
            if ph is None or "attn" in ph:
                self.phase_attn(l)
            if ph is None or "sg" in ph:
                self.phase_sg(l)
            if ph is None or "ssm" in ph:
                self.phase_ssd(l)
            if ph is None or "ml" in ph:
                self.phase_mlstm(l)
            if ph is None or "c" in ph:
                self.phase_c(l)
            if ph is None or "moe" in ph:
                self.phase_moe(l)
                self.phase_comb(l, last=(l == self.L - 1))
        if self.debug:
            self.dbgMOD = self.nc.dram_tensor("dbgMOD", [128, 6 * D], F32, kind="ExternalOutput").ap()
            self.dma("sp", lambda e: e.dma_start(out=self.dbgMOD, in_=self.MOD[:]), "dbg", reads=[self.rMOD])
        self.barrier()
        self.es.close()
        return self.nc


IN_NAMES = ["w_in", "w_out", "w_mix_norm", "attn_f_bias", "sg_w", "sg_b", "ssm_conv_w", "ssm_conv_b", "ssm_dt_bias",
            "ssm_a_log", "ssm_d", "mlstm_i_bias", "mlstm_f_bias", "w_ada", "b_ada", "w_norm1", "w_norm2",
            "w_router_group", "b_router_group", "w_router_expert", "b_router_expert", "w_expert_gate", "w_expert_up",
            "w_expert_down"]


def make_in_map(inputs, b, L):
    m = {"x": np.ascontiguousarray(inputs["x"][b]), "c": np.ascontiguousarray(inputs["c"][b:b + 1])}
    for k in IN_NAMES:
        if k in ("w_expert_gate", "w_expert_up"):
            m[k] = np.ascontiguousarray(inputs[k][:L].reshape(L, 32, 8, 128, 512).transpose(0, 1, 3, 2, 4)).reshape(L * 4096, 4096)
        elif k == "w_expert_down":
            m[k] = np.ascontiguousarray(inputs[k][:L].reshape(L, 32, 4, 128, 1024).transpose(0, 1, 3, 2, 4)).reshape(L * 4096, 4096)
        else:
            m[k] = np.ascontiguousarray(inputs[k][:L])
    m["w_norm_final"] = np.ascontiguousarray(inputs["w_norm_final"].reshape(1, D))
    return m


_CACHE = {}


def kernel(**inputs):
    S = inputs["x"].shape[1]
    L = inputs["w_in"].shape[0]
    nb = inputs["x"].shape[0]
    key = (S, L)
    if key not in _CACHE:
        _CACHE[key] = Builder(S, L).build()
    nc = _CACHE[key]
    inputs = {k: np.asarray(v, dtype=np.float32) for k, v in inputs.items()}
    m0 = make_in_map(inputs, 0, L)
    maps = []
    for b in range(nb):
        mb = dict(m0)
        mb["x"] = np.ascontiguousarray(inputs["x"][b])
        mb["c"] = np.ascontiguousarray(inputs["c"][b:b + 1])
        maps.append(mb)
    in_maps = [maps[c % nb] for c in range(NCORES)]
    res = run_bass_kernel_spmd(nc, in_maps, core_ids=list(range(NCORES)))
    out = np.stack([np.asarray(res.results[b]["out"], dtype=np.float32) for b in range(nb)], axis=0)
    return out
```
